# Optimizing a Trainium2 kernel written in Bass

```python
import jax, jax.numpy as jnp
from jax import lax
import numpy as np

D_MODEL = 2048
BATCH = 1
SEQ = 8192
DEPTH = 1

MLA_HEADS = 16
MLA_Q_RANK = 512
MLA_KV_RANK = 512
MLA_NOPE = 128
MLA_ROPE = 64
MLA_V = 128
MLA_QK_DIM = MLA_NOPE + MLA_ROPE
ROPE_THETA = 10000.0
FOX_HEADS = 16
FOX_HEAD_DIM = 128
FOX_FORGET_BIAS = 2.0
MLA_WIDTH = MLA_HEADS * MLA_V
FOX_WIDTH = FOX_HEADS * FOX_HEAD_DIM
MIX_WIDTH = MLA_WIDTH + FOX_WIDTH
Q_BLOCK = 128
PEER_HEADS = 8
PEER_QUERY_DIM = 256
PEER_HALF = PEER_QUERY_DIM // 2
PEER_N_KEYS = 128
PEER_N_EXPERTS = PEER_N_KEYS * PEER_N_KEYS
PEER_TOPK = 16
PEER_BLOCK = 128
NORM_EPS = 1e-6

IN_SPLIT_SIZES = (MLA_Q_RANK, MLA_KV_RANK, MLA_ROPE,
                  FOX_WIDTH, FOX_WIDTH, FOX_WIDTH, FOX_HEADS,
                  MLA_WIDTH, FOX_WIDTH)
IN_WIDTH = sum(IN_SPLIT_SIZES)
IN_SPLIT_POINTS = tuple(sum(IN_SPLIT_SIZES[:i + 1]) for i in range(len(IN_SPLIT_SIZES) - 1))

kernel_name = "hybrid_mla_fox_peer_block"


def rms_norm(x, g):
    xf = x.astype(jnp.float32)
    y = xf * lax.rsqrt(jnp.mean(xf * xf, axis=-1, keepdims=True) + NORM_EPS)
    return (y * g.astype(jnp.float32)).astype(x.dtype)


def rope(x, positions):
    half = x.shape[-1] // 2
    inv_freq = ROPE_THETA ** (-jnp.arange(half, dtype=jnp.float32) / half)
    ang = positions.astype(jnp.float32)[:, :, None, None] * inv_freq
    cos, sin = jnp.cos(ang), jnp.sin(ang)
    xf = x.astype(jnp.float32)
    x1, x2 = xf[..., :half], xf[..., half:]
    return jnp.concatenate([x1 * cos - x2 * sin, x1 * sin + x2 * cos], axis=-1).astype(x.dtype)


def causal_block_attention(q, k, v, log_forget_cum=None):
    B, S, H, Dk = q.shape
    Dv = v.shape[-1]
    nb = S // Q_BLOCK
    scale = Dk ** -0.5
    kf = k.astype(jnp.float32)
    vf = v.astype(jnp.float32)
    key_pos = jnp.arange(S, dtype=jnp.int32)
    q_blocks = q.reshape(B, nb, Q_BLOCK, H, Dk).swapaxes(0, 1)
    starts = jnp.arange(nb, dtype=jnp.int32) * Q_BLOCK
    if log_forget_cum is None:
        xs = (q_blocks, starts)
    else:
        cum_keys = log_forget_cum.astype(jnp.float32).transpose(0, 2, 1)
        cum_blocks = log_forget_cum.astype(jnp.float32).reshape(B, nb, Q_BLOCK, H).swapaxes(0, 1)
        xs = (q_blocks, starts, cum_blocks)

    def one_block(args):
        q_blk, start = args[0], args[1]
        s = jnp.einsum('bqhd,bkhd->bhqk', q_blk.astype(jnp.float32), kf) * scale
        if log_forget_cum is not None:
            cum_q = args[2].transpose(0, 2, 1)
            s = s + cum_q[..., :, None] - cum_keys[:, :, None, :]
        q_pos = start + jnp.arange(Q_BLOCK, dtype=jnp.int32)
        mask = key_pos[None, :] <= q_pos[:, None]
        s = jnp.where(mask, s, -jnp.inf)
        p = jax.nn.softmax(s, axis=-1)
        return jnp.einsum('bhqk,bkhd->bqhd', p, vf).astype(v.dtype)

    out = lax.map(one_block, xs)
    return out.swapaxes(0, 1).reshape(B, S, H, Dv)


def peer_ffn(h, w_q, sub_keys, u, v):
    B, S, D = h.shape
    T = B * S
    ht = h.reshape(T, D)
    q = jnp.einsum('td,de->te', ht, w_q).reshape(T, PEER_HEADS, 2, PEER_HALF).astype(jnp.float32)
    scores = jnp.einsum('thcd,hcnd->thcn', q, sub_keys.astype(jnp.float32))
    s1, i1 = lax.top_k(scores[:, :, 0], PEER_TOPK)
    s2, i2 = lax.top_k(scores[:, :, 1], PEER_TOPK)
    cand_scores = (s1[..., :, None] + s2[..., None, :]).reshape(T, PEER_HEADS, PEER_TOPK * PEER_TOPK)
    cand_idx = (i1[..., :, None] * PEER_N_KEYS + i2[..., None, :]).reshape(T, PEER_HEADS, PEER_TOPK * PEER_TOPK)
    top_scores, top_pos = lax.top_k(cand_scores, PEER_TOPK)
    experts = jnp.take_along_axis(cand_idx, top_pos, axis=-1)
    gates = jax.nn.softmax(top_scores, axis=-1)

    nb = T // PEER_BLOCK
    xs = (ht.reshape(nb, PEER_BLOCK, D),
          experts.reshape(nb, PEER_BLOCK, PEER_HEADS, PEER_TOPK),
          gates.reshape(nb, PEER_BLOCK, PEER_HEADS, PEER_TOPK))

    def apply_block(args):
        xb, eb, gb = args
        ub = u[eb].astype(jnp.float32)
        a = jnp.einsum('td,thkd->thk', xb.astype(jnp.float32), ub)
        w = gb * jax.nn.gelu(a, approximate=False)
        vb = v[eb].astype(jnp.float32)
        return jnp.einsum('thk,thkd->td', w, vb).astype(h.dtype)

    out = lax.map(apply_block, xs)
    return out.reshape(B, S, D)


def setup_inputs(seed: int = 0) -> dict:
    key = jax.random.key(seed)
    ks = jax.random.split(key, 24)
    f32 = jnp.float32

    def normal(k, shape, scale):
        return jax.random.normal(k, shape, f32) * scale

    def gain(k, shape):
        return 1.0 + 0.02 * jax.random.normal(k, shape, f32)

    L = DEPTH
    return {
        "x": jax.random.normal(ks[0], (BATCH, SEQ, D_MODEL), f32),
        "positions": jnp.broadcast_to(jnp.arange(SEQ, dtype=jnp.int32)[None, :], (BATCH, SEQ)),
        "mix_norm_g": gain(ks[1], (L, D_MODEL)),
        "w_in": normal(ks[2], (L, D_MODEL, IN_WIDTH), D_MODEL ** -0.5),
        "b_forget": FOX_FORGET_BIAS + 0.1 * jax.random.normal(ks[3], (L, FOX_HEADS), f32),
        "b_gate": normal(ks[4], (L, MIX_WIDTH), 0.02),
        "mla_q_latent_g": gain(ks[5], (L, MLA_Q_RANK)),
        "w_q_up": normal(ks[6], (L, MLA_Q_RANK, MLA_HEADS * MLA_QK_DIM), MLA_Q_RANK ** -0.5),
        "mla_kv_latent_g": gain(ks[7], (L, MLA_KV_RANK)),
        "w_kv_up": normal(ks[8], (L, MLA_KV_RANK, MLA_HEADS * (MLA_NOPE + MLA_V)), MLA_KV_RANK ** -0.5),
        "mla_q_norm_g": gain(ks[9], (L, MLA_QK_DIM)),
        "mla_k_norm_g": gain(ks[10], (L, MLA_QK_DIM)),
        "fox_q_norm_g": gain(ks[11], (L, FOX_HEAD_DIM)),
        "fox_k_norm_g": gain(ks[12], (L, FOX_HEAD_DIM)),
        "w_out": normal(ks[13], (L, MIX_WIDTH, D_MODEL), MIX_WIDTH ** -0.5),
        "ffn_norm_g": gain(ks[14], (L, D_MODEL)),
        "w_peer_q": normal(ks[15], (L, D_MODEL, PEER_HEADS * PEER_QUERY_DIM), D_MODEL ** -0.5),
        "peer_sub_keys": normal(ks[16], (L, PEER_HEADS, 2, PEER_N_KEYS, PEER_HALF), PEER_HALF ** -0.5),
        "peer_u": normal(ks[17], (L, PEER_N_EXPERTS, D_MODEL), D_MODEL ** -0.5),
        "peer_v": normal(ks[18], (L, PEER_N_EXPERTS, D_MODEL), PEER_HEADS ** -0.5),
    }


def reference(x, positions, mix_norm_g, w_in, b_forget, b_gate, mla_q_latent_g, w_q_up,
              mla_kv_latent_g, w_kv_up, mla_q_norm_g, mla_k_norm_g, fox_q_norm_g, fox_k_norm_g,
              w_out, ffn_norm_g, w_peer_q, peer_sub_keys, peer_u, peer_v):
    B, S, _ = x.shape
    for layer in range(DEPTH):
        h = rms_norm(x, mix_norm_g[layer])
        proj = jnp.einsum('bsd,de->bse', h, w_in[layer])
        (c_q, c_kv, k_pe, fq, fk, fv, f_logit, gate_a, gate_b) = jnp.split(proj, IN_SPLIT_POINTS, axis=-1)

        q_a = jnp.einsum('bsr,re->bse', rms_norm(c_q, mla_q_latent_g[layer]), w_q_up[layer])
        q_a = q_a.reshape(B, S, MLA_HEADS, MLA_QK_DIM)
        kv = jnp.einsum('bsr,re->bse', rms_norm(c_kv, mla_kv_latent_g[layer]), w_kv_up[layer])
        kv = kv.reshape(B, S, MLA_HEADS, MLA_NOPE + MLA_V)
        k_nope, v_a = kv[..., :MLA_NOPE], kv[..., MLA_NOPE:]
        k_pe_h = jnp.broadcast_to(k_pe[:, :, None, :], (B, S, MLA_HEADS, MLA_ROPE))
        k_a = jnp.concatenate([k_nope, k_pe_h], axis=-1)
        q_a = rms_norm(q_a, mla_q_norm_g[layer])
        k_a = rms_norm(k_a, mla_k_norm_g[layer])
        q_a = jnp.concatenate([q_a[..., :MLA_NOPE], rope(q_a[..., MLA_NOPE:], positions)], axis=-1)
        k_a = jnp.concatenate([k_a[..., :MLA_NOPE], rope(k_a[..., MLA_NOPE:], positions)], axis=-1)
        y_a = causal_block_attention(q_a, k_a, v_a).reshape(B, S, MLA_WIDTH)

        q_b = rms_norm(fq.reshape(B, S, FOX_HEADS, FOX_HEAD_DIM), fox_q_norm_g[layer])
        k_b = rms_norm(fk.reshape(B, S, FOX_HEADS, FOX_HEAD_DIM), fox_k_norm_g[layer])
        v_b = fv.reshape(B, S, FOX_HEADS, FOX_HEAD_DIM)
        log_f = jax.nn.log_sigmoid((f_logit + b_forget[layer]).astype(jnp.float32))
        cum_log_f = jnp.cumsum(log_f, axis=1)
        y_b = causal_block_attention(q_b, k_b, v_b, cum_log_f).reshape(B, S, FOX_WIDTH)

        gates = jax.nn.sigmoid(jnp.concatenate([gate_a, gate_b], axis=-1) + b_gate[layer])
        merged = jnp.concatenate([y_a, y_b], axis=-1) * gates
        x = x + jnp.einsum('bse,ed->bsd', merged, w_out[layer])

        h2 = rms_norm(x, ffn_norm_g[layer])
        x = x + peer_ffn(h2, w_peer_q[layer], peer_sub_keys[layer], peer_u[layer], peer_v[layer])
    return x
```

```python
import math
import numpy as np
from contextlib import ExitStack
import concourse.bass as bass
import concourse.mybir as mybir
from concourse.bass_utils import run_bass_kernel_spmd

F32 = mybir.dt.float32
BF16 = mybir.dt.bfloat16
I32 = mybir.dt.int32
U32 = mybir.dt.uint32
ALU = mybir.AluOpType
AF = mybir.ActivationFunctionType
AX = mybir.AxisListType

NDS = 40
ARENA_BYTES = 206 * 1024


class Res:
    __slots__ = ("name", "w", "rs")

    def __init__(self, name=""):
        self.name = name
        self.w = None
        self.rs = {}


class KB:
    ENG = ("pe", "act", "dve", "pool", "sp")

    def __init__(self):
        self.nc = bass.Bass("TRN2", target_bir_lowering=False)
        self.es = ExitStack()
        self.q = {e: [] for e in self.ENG}
        self.cnt = {e: 0 for e in self.ENG}
        self.seen = {e: {} for e in self.ENG}
        self.sem = {e: self.es.enter_context(self.nc.semaphore("s_" + e)) for e in self.ENG}
        self.dsems = [self.es.enter_context(self.nc.semaphore("d%d" % i)) for i in range(NDS)]
        self.dcnt = [0] * NDS
        self.dnext = 0
        self.nuniq = 0
        self.big = self.es.enter_context(self.nc.sbuf_tensor("big", [128, ARENA_BYTES // 2], BF16))
        self.top = 0
        self.pbank = [self.es.enter_context(self.nc.psum_tensor("pbank%d" % i, [128, 512], F32)) for i in range(8)]

    def al(self, shape, dt, name=None, parts=None):
        esz = {F32: 4, I32: 4, U32: 4, BF16: 2}[dt]
        n = 1
        for d in shape[1:]:
            n *= d
        nbytes = (n * esz + 63) // 64 * 64
        assert self.top + nbytes <= ARENA_BYTES, ("arena overflow", self.top, nbytes, name)
        o = self.top // 2
        self.top += nbytes
        v = self.big[0:shape[0], o:o + (n * esz) // 2]
        if dt != BF16:
            v = v.bitcast(dt)
        if len(shape) == 3:
            v = v.rearrange("p (a b) -> p a b", a=shape[1])
        elif len(shape) == 4:
            v = v.rearrange("p (a b c) -> p a b c", a=shape[1], b=shape[2])
        return v

    def barrier(self):
        toks = [(o, self.sem[o], self.cnt[o]) for o in self.ENG if self.cnt[o] > 0]
        toks += [("d%d" % i, self.dsems[i], self.dcnt[i]) for i in range(NDS) if self.dcnt[i] > 0]
        for e in self.ENG:
            self._waits(e, toks)

    def arena_reset(self):
        self.barrier()
        self.top = 0

    def mark(self):
        return self.top

    def release(self, m):
        self.barrier()
        self.top = m

    def sb(self, shape, dt, name=None):
        self.nuniq += 1
        return self.es.enter_context(self.nc.sbuf_tensor(name or ("sb%d" % self.nuniq), list(shape), dt))

    def ps(self, shape, dt=F32, name=None):
        self.nuniq += 1
        return self.es.enter_context(self.nc.psum_tensor(name or ("ps%d" % self.nuniq), list(shape), dt))

    def dram(self, name, shape, dt, kind="Internal"):
        return self.nc.dram_tensor(name, list(shape), dt, kind=kind)

    def _collect(self, r, w):
        toks = []
        for x in r:
            toks.append(x.w)
        for x in w:
            toks.append(x.w)
            toks.extend(x.rs.values())
        return toks

    def _waits(self, eng, toks):
        need = {}
        seen = self.seen[eng]
        for t in toks:
            if t is None:
                continue
            k, h, v = t
            if seen.get(k, 0) >= v:
                continue
            if k not in need or need[k][1] < v:
                need[k] = (h, v)
        for k, (h, v) in need.items():
            seen[k] = v
            self.q[eng].append(lambda e, h=h, v=v: e.wait_ge(h, v))

    def _record(self, tok, r, w):
        k = tok[0]
        for x in r:
            old = x.rs.get(k)
            if old is None or old[2] < tok[2]:
                x.rs[k] = tok
        for x in w:
            x.w = tok
            x.rs = {}

    def op(self, eng, fn, r=(), w=(), pe_acc=False):
        toks = self._collect(r, w)
        if pe_acc:
            toks = [t for t in toks if t is None or t[0] != eng]
        self._waits(eng, toks)
        self.cnt[eng] += 1
        n = self.cnt[eng]
        h = self.sem[eng]
        self.q[eng].append(lambda e: fn(e).then_inc(h, 1))
        tok = (eng, h, n)
        self._record(tok, r, w)
        return tok

    def dma(self, qe, out, in_, r=(), w=(), fn=None, **kw):
        toks = self._collect(r, w)
        i = self.dnext
        self.dnext = (i + 1) % NDS
        key = "d%d" % i
        h = self.dsems[i]
        if self.dcnt[i] > 0:
            toks.append((key, h, self.dcnt[i]))
        self._waits(qe, toks)
        self.dcnt[i] += 16
        v = self.dcnt[i]
        if fn is None:
            self.q[qe].append(lambda e: e.dma_start(out=out, in_=in_, **kw).then_inc(h, 16))
        else:
            self.q[qe].append(lambda e: fn(e).then_inc(h, 16))
        tok = (key, h, v)
        self._record(tok, r, w)
        return tok

    def wait_all_dma(self, eng="sp"):
        toks = [("d%d" % i, self.dsems[i], self.dcnt[i]) for i in range(NDS) if self.dcnt[i] > 0]
        self._waits(eng, toks)

    def build(self):
        self.wait_all_dma("sp")
        nc = self.nc
        q = self.q
        with nc.Block() as block:
            @block.tensor
            def _(e):
                for f in q["pe"]:
                    f(e)

            @block.scalar
            def _(e):
                for f in q["act"]:
                    f(e)

            @block.vector
            def _(e):
                for f in q["dve"]:
                    f(e)

            @block.gpsimd
            def _(e):
                for f in q["pool"]:
                    f(e)

            @block.sync
            def _(e):
                for f in q["sp"]:
                    f(e)
        self.es.close()
        return nc


D = 2048
NCOLS = 2370
QL, KL, QN, KN, FQ, FK, BF, BG, IF, NV = 0, 512, 1024, 1216, 1408, 1536, 1664, 1666, 2178, 2210
EPS = 1e-6
NC = 8


def build_A(kb, S, io, R_mt=None, stage=3):
    NT = S // 128
    TPC = S // NC
    x, w_in, w_qup, w_kvup, gmix, gvd, posd = io["x"], io["w_in"], io["w_qup"], io["w_kvup"], io["gmix"], io["gv"], io["pos"]
    qkg, vd, mt = io["qkg"], io["vd"], io["mt"]
    R_qkg, R_vd = Res("qkg"), Res("vd")
    if R_mt is None:
        R_mt = Res("mt")

    V = lambda fn, r=(), w=(): kb.op("dve", fn, r, w)
    A = lambda fn, r=(), w=(): kb.op("act", fn, r, w)
    P = lambda fn, r=(), w=(): kb.op("pool", fn, r, w)
    T = lambda fn, r=(), w=(): kb.op("pe", fn, r, w, pe_acc=True)

    identf = kb.al([128, 128], F32, "identf")
    ident = kb.al([128, 128], BF16, "ident")
    trif = kb.al([128, 128], F32, "trif")
    mtri = kb.al([128, 128], BF16, "mtri")
    onesf = kb.al([128, 128], F32, "onesf")
    onesb = kb.al([128, 128], BF16, "onesb")
    nhalf = kb.al([128, 8], F32, "nhalf")
    R_c = Res("consts")
    P(lambda e: e.memset(identf[:], 0.0), w=[R_c])
    P(lambda e: e.affine_select(out=identf[:], in_=identf[:], pattern=[[-1, 128]], compare_op=ALU.not_equal,
                                fill=1.0, base=0, channel_multiplier=1), r=[R_c], w=[R_c])
    P(lambda e: e.memset(onesf[:], 1.0), w=[R_c])
    P(lambda e: e.affine_select(out=trif[:], in_=onesf[:], pattern=[[1, 128]], compare_op=ALU.is_ge,
                                fill=0.0, base=0, channel_multiplier=-1), r=[R_c], w=[R_c])
    P(lambda e: e.memset(nhalf[:], -0.5), w=[R_c])
    V(lambda e: e.tensor_copy(out=ident[:], in_=identf[:]), r=[R_c], w=[R_c])
    V(lambda e: e.tensor_copy(out=mtri[:], in_=trif[:]), r=[R_c], w=[R_c])
    V(lambda e: e.tensor_copy(out=onesb[:], in_=onesf[:]), r=[R_c], w=[R_c])

    arena = kb.al([128, 16 * NCOLS], BF16, "arena")
    R_ar = Res("arena")
    wb = arena[:, :].rearrange("p (c n) -> p c n", c=16)
    w_in_v = w_in.rearrange("(c p) n -> p c n", p=128)
    for c in range(16):
        kb.dma("pool", wb[:, c, :], w_in_v[:, c, :], w=[R_ar], max_dma_last_dim=4096)
    wq = kb.al([128, 4, 384], BF16, "wq")
    wkv = kb.al([128, 4, 512], BF16, "wkv")
    R_wq = Res("wq")
    kb.dma("pool", wq[:], w_qup.rearrange("(c p) n -> p c n", p=128), w=[R_wq])
    kb.dma("pool", wkv[:], w_kvup.rearrange("(c p) n -> p c n", p=128), w=[R_wq])
    gT = kb.al([128, 16], F32, "gT")
    gv = kb.al([128, NV], F32, "gv_sb")
    R_g = Res("gains")
    kb.dma("sp", gT[:], gmix.rearrange("(c p) -> p c", p=128), w=[R_g], allow_slow_non_contiguous=True)
    kb.dma("sp", gv[:], gvd, w=[R_g])
    V(lambda e: e.tensor_scalar(out=gv[:, QN:QN + 192], in0=gv[:, QN:QN + 192], scalar1=192 ** -0.5, scalar2=None, op0=ALU.mult), r=[R_g], w=[R_g])
    V(lambda e: e.tensor_scalar(out=gv[:, FQ:FQ + 128], in0=gv[:, FQ:FQ + 128], scalar1=128 ** -0.5, scalar2=None, op0=ALU.mult), r=[R_g], w=[R_g])

    flog = kb.al([128, 2, NT], F32, "flog")
    R_fl = Res("flog")
    mark_p1 = kb.mark()
    sin_t = kb.al([128, NT, 32], F32, "sin_t")
    cos_t = kb.al([128, NT, 32], F32, "cos_t")
    posi = kb.al([128, NT], I32, "posi")
    posf = kb.al([128, NT], F32, "posf")
    mark_r = kb.mark()
    ang = kb.al([128, NT, 32], F32, "ang")
    tq = kb.al([128, NT, 32], F32, "tq")
    tki = kb.al([128, NT, 32], I32, "tki")
    R_rp = Res("rope")
    kb.dma("sp", posi[:], posd, w=[R_rp])
    V(lambda e: e.tensor_copy(out=posf[:], in_=posi[:]), r=[R_rp], w=[R_rp])
    V(lambda e: e.tensor_tensor(out=ang[:], in0=posf[:, :].unsqueeze(2).to_broadcast([128, NT, 32]),
                                in1=gv[:, IF:IF + 32].unsqueeze(1).to_broadcast([128, NT, 32]), op=ALU.mult), r=[R_rp, R_g], w=[R_rp])
    for dst, shift in ((sin_t, 0.0), (cos_t, math.pi / 2)):
        V(lambda e, shift=shift: e.tensor_scalar(out=tq[:], in0=ang[:], scalar1=shift, scalar2=1.0 / (2 * math.pi), op0=ALU.add, op1=ALU.mult), r=[R_rp], w=[R_rp])
        V(lambda e: e.tensor_copy(out=tki[:], in_=tq[:]), r=[R_rp], w=[R_rp])
        V(lambda e: e.tensor_copy(out=tq[:], in_=tki[:]), r=[R_rp], w=[R_rp])
        V(lambda e: e.tensor_scalar(out=tq[:], in0=tq[:], scalar1=-2 * math.pi, scalar2=None, op0=ALU.mult), r=[R_rp], w=[R_rp])
        V(lambda e, shift=shift: e.scalar_tensor_tensor(out=tq[:], in0=ang[:], scalar=shift, in1=tq[:], op0=ALU.add, op1=ALU.add), r=[R_rp], w=[R_rp])
        V(lambda e, dst=dst: e.tensor_scalar(out=dst[:], in0=tq[:], scalar1=math.pi, scalar2=-2 * math.pi, op0=ALU.is_gt, op1=ALU.mult), r=[R_rp], w=[R_rp])
        V(lambda e, dst=dst: e.tensor_tensor(out=tq[:], in0=tq[:], in1=dst[:], op=ALU.add), r=[R_rp], w=[R_rp])
        A(lambda e, dst=dst: e.activation(out=dst[:], in_=tq[:], func=AF.Sin), r=[R_rp], w=[R_rp])

    kb.release(mark_r)
    pb = kb.pbank[0:7]
    R_pb = [Res("pb%d" % i) for i in range(7)]
    pT = kb.pbank[7][:, :].bitcast(BF16)
    R_pT = Res("pT")
    pTv = pT[:, :].rearrange("p (c t) -> p c t", c=8)

    xt = [kb.al([128, D], F32, "xt%d" % i) for i in range(2)]
    R_xt = [Res("xt0"), Res("xt1")]
    junk = kb.al([128, D], BF16, "junk")
    R_junk = Res("junk")
    xn = kb.al([128, D], BF16, "xn")
    R_xn = Res("xn")
    hT = kb.al([128, 16, 128], BF16, "hT")
    R_hT = Res("hT")
    st = kb.al([128, 16], F32, "st")
    rs = kb.al([128, 16], F32, "rs")
    R_st = Res("st")
    lat = kb.al([128, 2, 512], BF16, "lat")
    R_lat = Res("lat")
    latT = kb.al([128, 8, 128], BF16, "latT")
    R_latT = Res("latT")
    src14 = kb.al([128, 14, 128], BF16, "src14")
    R_s14 = Res("src14")
    pe_in = kb.al([128, 4, 64], F32, "pe_in")
    pe_t = kb.al([128, 4, 4, 32], F32, "pe_t")
    R_pe = Res("pe")
    gtmp = kb.al([128, 512], F32, "gtmp")
    R_gt = Res("gtmp")
    out14 = kb.al([128, 14, 128], BF16, "out14")
    R_o14 = Res("out14")
    vab = kb.al([128, 4, 128], BF16, "vab")
    R_vab = Res("vab")

    def rstd(cols, inv_n):
        a, b = cols
        V(lambda e: e.tensor_scalar(out=rs[:, a:b], in0=st[:, a:b], scalar1=inv_n, scalar2=EPS, op0=ALU.mult, op1=ALU.add), r=[R_st], w=[R_st])
        P(lambda e: e.tensor_tensor(out=rs[:, a:b], in0=rs[:, a:b], in1=nhalf[:, 0:b - a], op=ALU.pow), r=[R_st, R_c], w=[R_st])

    banks = [(0, 512), (512, 1024), (1024, 1346), (1346, 1858), (1858, 2370)]
    kb.dma("sp", xt[0][:], x[0:128, :], w=[R_xt[0]])
    for t in range(NT):
        b = t % 2
        if t + 1 < NT:
            kb.dma("sp", xt[1 - b][:], x[(t + 1) * 128:(t + 2) * 128, :], w=[R_xt[1 - b]])
        A(lambda e, b=b: e.activation(out=junk[:], in_=xt[b][:], func=AF.Square, accum_out=st[:, 0:1]), r=[R_xt[b]], w=[R_junk, R_st])
        rstd((0, 1), 1.0 / D)
        V(lambda e, b=b: e.tensor_scalar(out=xn[:], in0=xt[b][:], scalar1=rs[:, 0:1], scalar2=None, op0=ALU.mult), r=[R_xt[b], R_st], w=[R_xn])
        for half in range(2):
            for c in range(8):
                cc = half * 8 + c
                T(lambda e, c=c, cc=cc: e.transpose(out=pT[:, c * 128:(c + 1) * 128], in_=xn[:, cc * 128:(cc + 1) * 128], identity=ident[:]),
                  r=[R_xn, R_c], w=[R_pT])
            V(lambda e, half=half: e.tensor_tensor(out=hT[:, half * 8:half * 8 + 8, :], in0=pTv,
                                                   in1=gT[:, half * 8:half * 8 + 8].unsqueeze(2).to_broadcast([128, 8, 128]), op=ALU.mult),
              r=[R_pT, R_g], w=[R_hT])
        for j, (c0, c1) in enumerate(banks):
            for c in range(16):
                T(lambda e, j=j, c=c, c0=c0, c1=c1: e.matmul(out=pb[j][:, 0:c1 - c0], lhsT=hT[:, c, :], rhs=wb[:, c, c0:c1], start=(c == 0), stop=(c == 15)),
                  r=[R_hT, R_ar], w=[R_pb[j]])
        A(lambda e: e.activation(out=junk[:, 0:512], in_=pb[0][:], func=AF.Square, accum_out=st[:, 0:1]), r=[R_pb[0]], w=[R_junk, R_st])
        A(lambda e: e.activation(out=junk[:, 0:512], in_=pb[1][:], func=AF.Square, accum_out=st[:, 1:2]), r=[R_pb[1]], w=[R_junk, R_st])
        rstd((0, 2), 1.0 / 512)
        for i, off in ((0, QL), (1, KL)):
            V(lambda e, i=i, off=off: e.scalar_tensor_tensor(out=lat[:, i, :], in0=pb[i][:], scalar=rs[:, i:i + 1], in1=gv[:, off:off + 512], op0=ALU.mult, op1=ALU.mult),
              r=[R_pb[i], R_st, R_g], w=[R_lat])
        for i in range(2):
            for c in range(4):
                T(lambda e, i=i, c=c: e.transpose(out=pT[:, (i * 4 + c) * 128:(i * 4 + c + 1) * 128], in_=lat[:, i, c * 128:(c + 1) * 128], identity=ident[:]),
                  r=[R_lat, R_c], w=[R_pT])
        A(lambda e: e.copy(out=latT[:], in_=pTv), r=[R_pT], w=[R_latT])
        for c in range(4):
            T(lambda e, c=c: e.matmul(out=pb[5][:, 0:384], lhsT=latT[:, c, :], rhs=wq[:, c, :], start=(c == 0), stop=(c == 3)), r=[R_latT, R_wq], w=[R_pb[5]])
        for c in range(4):
            T(lambda e, c=c: e.matmul(out=pb[6][:, 0:512], lhsT=latT[:, 4 + c, :], rhs=wkv[:, c, :], start=(c == 0), stop=(c == 3)), r=[R_latT, R_wq], w=[R_pb[6]])
        for h in range(2):
            A(lambda e, h=h: e.activation(out=junk[:, 0:192], in_=pb[5][:, h * 192:(h + 1) * 192], func=AF.Square, accum_out=st[:, h:h + 1]), r=[R_pb[5]], w=[R_junk, R_st])
            A(lambda e, h=h: e.activation(out=junk[:, 0:128], in_=pb[6][:, h * 256:h * 256 + 128], func=AF.Square, accum_out=st[:, 2 + h:3 + h]), r=[R_pb[6]], w=[R_junk, R_st])
        A(lambda e: e.activation(out=junk[:, 0:64], in_=pb[2][:, 0:64], func=AF.Square, accum_out=st[:, 4:5]), r=[R_pb[2]], w=[R_junk, R_st])
        for j in range(4):
            A(lambda e, j=j: e.activation(out=junk[:, 0:128], in_=pb[3][:, j * 128:(j + 1) * 128], func=AF.Square, accum_out=st[:, 8 + j:9 + j]), r=[R_pb[3]], w=[R_junk, R_st])
        V(lambda e: e.tensor_scalar(out=st[:, 2:4], in0=st[:, 2:4], scalar1=st[:, 4:5], scalar2=None, op0=ALU.add), r=[R_st], w=[R_st])
        rstd((0, 4), 1.0 / 192)
        rstd((8, 12), 1.0 / 128)
        for h in range(2):
            V(lambda e, h=h: e.scalar_tensor_tensor(out=src14[:, h, :], in0=pb[5][:, h * 192:h * 192 + 128], scalar=rs[:, h:h + 1], in1=gv[:, QN:QN + 128], op0=ALU.mult, op1=ALU.mult),
              r=[R_pb[5], R_st, R_g], w=[R_s14])
            V(lambda e, h=h: e.scalar_tensor_tensor(out=pe_in[:, h, :], in0=pb[5][:, h * 192 + 128:h * 192 + 192], scalar=rs[:, h:h + 1], in1=gv[:, QN + 128:QN + 192], op0=ALU.mult, op1=ALU.mult),
              r=[R_pb[5], R_st, R_g], w=[R_pe])
            V(lambda e, h=h: e.scalar_tensor_tensor(out=src14[:, 3 + h, :], in0=pb[6][:, h * 256:h * 256 + 128], scalar=rs[:, 2 + h:3 + h], in1=gv[:, KN:KN + 128], op0=ALU.mult, op1=ALU.mult),
              r=[R_pb[6], R_st, R_g], w=[R_s14])
            V(lambda e, h=h: e.scalar_tensor_tensor(out=pe_in[:, 2 + h, :], in0=pb[2][:, 0:64], scalar=rs[:, 2 + h:3 + h], in1=gv[:, KN + 128:KN + 192], op0=ALU.mult, op1=ALU.mult),
              r=[R_pb[2], R_st, R_g], w=[R_pe])
        cs = cos_t[:, t, :].unsqueeze(1).to_broadcast([128, 4, 32])
        sn = sin_t[:, t, :].unsqueeze(1).to_broadcast([128, 4, 32])
        x1 = pe_in[:, :, 0:32]
        x2 = pe_in[:, :, 32:64]
        V(lambda e, cs=cs: e.tensor_tensor(out=pe_t[:, 0], in0=x1, in1=cs, op=ALU.mult), r=[R_pe, R_rp], w=[R_pe])
        V(lambda e, sn=sn: e.tensor_tensor(out=pe_t[:, 1], in0=x2, in1=sn, op=ALU.mult), r=[R_pe, R_rp], w=[R_pe])
        V(lambda e, sn=sn: e.tensor_tensor(out=pe_t[:, 2], in0=x1, in1=sn, op=ALU.mult), r=[R_pe, R_rp], w=[R_pe])
        V(lambda e, cs=cs: e.tensor_tensor(out=pe_t[:, 3], in0=x2, in1=cs, op=ALU.mult), r=[R_pe, R_rp], w=[R_pe])
        for blk, h0 in ((2, 0), (5, 2)):
            dv = src14[:, blk, :].rearrange("p (h two i) -> p h two i", h=2, two=2)
            V(lambda e, dv=dv, h0=h0: e.tensor_tensor(out=dv[:, :, 0, :], in0=pe_t[:, 0, h0:h0 + 2, :], in1=pe_t[:, 1, h0:h0 + 2, :], op=ALU.subtract), r=[R_pe], w=[R_s14])
            V(lambda e, dv=dv, h0=h0: e.tensor_tensor(out=dv[:, :, 1, :], in0=pe_t[:, 2, h0:h0 + 2, :], in1=pe_t[:, 3, h0:h0 + 2, :], op=ALU.add), r=[R_pe], w=[R_s14])
        for j in range(4):
            off = FQ if j < 2 else FK
            V(lambda e, j=j, off=off: e.scalar_tensor_tensor(out=src14[:, 6 + j, :], in0=pb[3][:, j * 128:(j + 1) * 128], scalar=rs[:, 8 + j:9 + j], in1=gv[:, off:off + 128], op0=ALU.mult, op1=ALU.mult),
              r=[R_pb[3], R_st, R_g], w=[R_s14])
        A(lambda e: e.copy(out=vab[:, 0:2, :], in_=pb[6][:, :].rearrange("p (h two d) -> p h two d", h=2, two=2)[:, :, 1, :]), r=[R_pb[6]], w=[R_vab])
        A(lambda e: e.copy(out=vab[:, 2:4, :], in_=pb[2][:, 64:320].rearrange("p (h d) -> p h d", h=2)), r=[R_pb[2]], w=[R_vab])
        A(lambda e, t=t: e.copy(out=flog[:, :, t], in_=pb[2][:, 320:322]), r=[R_pb[2]], w=[R_fl])
        V(lambda e: e.tensor_tensor(out=gtmp[:], in0=pb[4][:], in1=gv[:, BG:BG + 512], op=ALU.add), r=[R_pb[4], R_g], w=[R_gt])
        A(lambda e: e.activation(out=src14[:, 10:14, :], in_=gtmp[:].rearrange("p (b d) -> p b d", b=4), func=AF.Sigmoid), r=[R_gt], w=[R_s14])
        for c in range(8):
            T(lambda e, c=c: e.transpose(out=pT[:, c * 128:(c + 1) * 128], in_=src14[:, c, :], identity=ident[:]), r=[R_s14, R_c], w=[R_pT])
        V(lambda e: e.tensor_copy(out=out14[:, 0:8, :], in_=pTv), r=[R_pT], w=[R_o14])
        for c in range(6):
            T(lambda e, c=c: e.transpose(out=pT[:, c * 128:(c + 1) * 128], in_=src14[:, 8 + c, :], identity=ident[:]), r=[R_s14, R_c], w=[R_pT])
        V(lambda e: e.tensor_copy(out=out14[:, 8:14, :], in_=pTv[:, 0:6, :]), r=[R_pT], w=[R_o14])
        for b0 in range(0, 14, 4):
            b1 = min(14, b0 + 4)
            kb.dma("sp", qkg[b0:b1, :, t * 128:(t + 1) * 128].rearrange("b p s -> p b s"), out14[:, b0:b1, :], r=[R_o14], w=[R_qkg])
        kb.dma("sp", vd[t * 128:(t + 1) * 128, :], vab[:].rearrange("p b d -> p (b d)"), r=[R_vab], w=[R_vd])

    if stage < 2:
        return R_mt
    kb.release(mark_p1)
    lz = kb.al([128, 2, NT], F32, "lz")
    cw = kb.al([128, 2, NT], F32, "cw")
    cend = kb.al([128, 2, NT], F32, "cend")
    onesr = kb.al([128, NT], F32, "onesr")
    R_cs = Res("cums")
    V(lambda e: e.tensor_tensor(out=lz[:], in0=flog[:], in1=gv[:, BF:BF + 2].unsqueeze(2).to_broadcast([128, 2, NT]), op=ALU.add), r=[R_fl, R_g], w=[R_cs])
    A(lambda e: e.activation(out=lz[:], in_=lz[:], func=AF.Exp, scale=-1.0), r=[R_cs], w=[R_cs])
    A(lambda e: e.activation(out=lz[:], in_=lz[:], func=AF.Ln, bias=1.0), r=[R_cs], w=[R_cs])
    lz2 = lz[:].rearrange("p h j -> p (h j)")
    T(lambda e: e.matmul(out=pb[0][:, 0:2 * NT], lhsT=trif[:], rhs=lz2, start=True, stop=True), r=[R_cs, R_c], w=[R_pb[0]])
    T(lambda e: e.matmul(out=pb[1][:, 0:2 * NT], lhsT=onesf[:], rhs=lz2, start=True, stop=True), r=[R_cs, R_c], w=[R_pb[1]])
    V(lambda e: e.tensor_copy(out=cw[:].rearrange("p h j -> p (h j)"), in_=pb[1][:, 0:2 * NT]), r=[R_pb[1]], w=[R_cs])
    P(lambda e: e.memset(onesr[:], 1.0), w=[R_cs])
    for h in range(2):
        V(lambda e, h=h: e.tensor_tensor_scan(out=cend[:, h, :], data0=onesr[:], data1=cw[:, h, :], initial=0.0, op0=ALU.mult, op1=ALU.add), r=[R_cs], w=[R_cs])
    V(lambda e: e.tensor_tensor(out=cw[:], in0=cend[:], in1=cw[:], op=ALU.subtract), r=[R_cs], w=[R_cs])
    V(lambda e: e.tensor_tensor(out=cw[:].rearrange("p h j -> p (h j)"), in0=cw[:].rearrange("p h j -> p (h j)"), in1=pb[0][:, 0:2 * NT], op=ALU.add), r=[R_cs, R_pb[0]], w=[R_cs])
    bt = kb.al([128, NT, NT], F32, "bt")
    R_bt = Res("bt")

    if stage < 3:
        return R_mt
    kn = arena[:, 0:S]
    kpe = arena[0:64, S:2 * S]
    vt = arena[:, 2 * S:3 * S].rearrange("p (j d) -> p j d", d=128)
    NQ = S // 512
    qn_t = [kb.al([128, 512], BF16, "qn%d" % i) for i in range(2)]
    qpe_t = [kb.al([64, 512], BF16, "qpe%d" % i) for i in range(2)]
    g_t = [kb.al([128, 512], BF16, "gt%d" % i) for i in range(2)]
    R_q = [Res("q0"), Res("q1")]
    pt_t = [kb.al([128, 512], BF16, "pt%d" % i) for i in range(2)]
    R_pt = [Res("pt0"), Res("pt1")]
    rl = kb.al([128, 512], F32, "rl")
    yt = kb.al([128, 512], F32, "yt")
    yb = [kb.al([128, 512], BF16, "yb%d" % i) for i in range(2)]
    R_y = Res("y")
    R_yb = [Res("yb0"), Res("yb1")]
    ps_s = [pb[0], pb[1]]
    R_pss = [R_pb[0], R_pb[1]]
    ps_o, R_pso = pb[2], R_pb[2]
    ps_l, R_psl = pb[3], R_pb[3]
    nq = 0
    npt = 0
    for hh in range(4):
        mla = hh < 2
        if mla:
            bq, bqpe, bk, bkpe = hh, 2, 3 + hh, 5
        else:
            bq, bk = 6 + hh - 2, 8 + hh - 2
            hf = hh - 2
            V(lambda e, hf=hf: e.tensor_tensor(out=bt[:], in0=cw[:, hf, :].unsqueeze(2).to_broadcast([128, NT, NT]),
                                              in1=cend[:, hf, :].unsqueeze(1).to_broadcast([128, NT, NT]), op=ALU.subtract), r=[R_cs], w=[R_bt])
        kb.dma("sp", kn, qkg[bk, :, :], r=[R_qkg], w=[R_ar])
        if mla:
            kb.dma("sp", kpe, qkg[bkpe, (hh * 64):(hh * 64 + 64), :], r=[R_qkg], w=[R_ar])
        vsrc = vd[:, hh * 128:(hh + 1) * 128].rearrange("(j p) d -> p j d", p=128)
        for j0 in range(0, NT, 4):
            j1 = min(NT, j0 + 4)
            kb.dma("sp", vt[:, j0:j1, :], vsrc[:, j0:j1, :], r=[R_vd], w=[R_ar])
        for i in range(NQ):
            qb = nq % 2
            nq += 1
            cols = slice(i * 512, (i + 1) * 512)
            kb.dma("sp", qn_t[qb][:], qkg[bq, :, cols], r=[R_qkg], w=[R_q[qb]])
            if mla:
                kb.dma("sp", qpe_t[qb][:], qkg[bqpe, (hh * 64):(hh * 64 + 64), cols], r=[R_qkg], w=[R_q[qb]])
            kb.dma("sp", g_t[qb][:], qkg[10 + hh, :, cols], r=[R_qkg], w=[R_q[qb]])
            nk = 4 * i + 4
            for j in range(nk):
                d = j - 4 * i
                c0 = max(d, 0) * 128
                sb_ = npt % 2
                npt += 1
                pss, Rs = ps_s[sb_], R_pss[sb_]
                ptile, Rp = pt_t[sb_], R_pt[sb_]
                T(lambda e, pss=pss, j=j, qb=qb, c0=c0: e.matmul(out=pss[:, c0:512], lhsT=kn[:, j * 128:(j + 1) * 128], rhs=qn_t[qb][:, c0:512], start=True, stop=(not mla)),
                  r=[R_ar, R_q[qb]], w=[Rs])
                if mla:
                    T(lambda e, pss=pss, j=j, qb=qb, c0=c0: e.matmul(out=pss[:, c0:512], lhsT=kpe[:, j * 128:(j + 1) * 128], rhs=qpe_t[qb][:, c0:512], start=False, stop=True),
                      r=[R_ar, R_q[qb]], w=[Rs])
                    A(lambda e, pss=pss, ptile=ptile, c0=c0: e.activation(out=ptile[:, c0:512], in_=pss[:, c0:512], func=AF.Exp), r=[Rs], w=[Rp])
                else:
                    for uu in range(max(d, 0), 4):
                        u = 4 * i + uu
                        A(lambda e, pss=pss, ptile=ptile, uu=uu, j=j, u=u: e.activation(out=ptile[:, uu * 128:(uu + 1) * 128], in_=pss[:, uu * 128:(uu + 1) * 128],
                                                                                       func=AF.Exp, bias=bt[:, j, u:u + 1]), r=[Rs, R_bt], w=[Rp])
                if d >= 0:
                    P(lambda e, ptile=ptile, c0=c0: e.tensor_tensor(out=ptile[:, c0:c0 + 128], in0=ptile[:, c0:c0 + 128], in1=mtri[:], op=ALU.mult), r=[Rp, R_c], w=[Rp])
                T(lambda e, ptile=ptile, j=j, c0=c0, nk=nk: e.matmul(out=ps_o[:, c0:512], lhsT=vt[:, j, :], rhs=ptile[:, c0:512], start=(j == 0), stop=(j == nk - 1)),
                  r=[R_ar, Rp], w=[R_pso])
                T(lambda e, ptile=ptile, j=j, c0=c0, nk=nk: e.matmul(out=ps_l[:, c0:512], lhsT=onesb[:], rhs=ptile[:, c0:512], start=(j == 0), stop=(j == nk - 1)),
                  r=[R_c, Rp], w=[R_psl])
            V(lambda e: e.reciprocal(out=rl[:], in_=ps_l[:]), r=[R_psl], w=[R_y])
            V(lambda e: e.tensor_tensor(out=yt[:], in0=ps_o[:], in1=rl[:], op=ALU.mult), r=[R_pso, R_y], w=[R_y])
            V(lambda e, qb=qb: e.tensor_tensor(out=yb[qb][:], in0=yt[:], in1=g_t[qb][:], op=ALU.mult), r=[R_y, R_q[qb]], w=[R_yb[qb]])
            t0 = i * 512
            if TPC >= 512:
                dc, off = t0 // TPC, t0 % TPC
                kb.dma("sp", mt[dc, hh, :, off:off + 512], yb[qb][:], r=[R_yb[qb]], w=[R_mt])
            else:
                n = 512 // TPC
                dc = t0 // TPC
                kb.dma("sp", mt[dc:dc + n, hh, :, :].rearrange("c d t -> d c t"), yb[qb][:].rearrange("d (c t) -> d c t", c=n), r=[R_yb[qb]], w=[R_mt])
    return R_mt


def prep_A(inp, core, S):
    f = np.float32
    x = np.ascontiguousarray(inp["x"][0, :S])
    w_in = inp["w_in"][0]
    sp = np.cumsum([0, 512, 512, 64, 2048, 2048, 2048, 16, 2048, 2048])
    h0 = 2 * core
    cq, ckv, kpe = w_in[:, sp[0]:sp[1]], w_in[:, sp[1]:sp[2]], w_in[:, sp[2]:sp[3]]
    fq = w_in[:, sp[3] + h0 * 128: sp[3] + (h0 + 2) * 128]
    fk = w_in[:, sp[4] + h0 * 128: sp[4] + (h0 + 2) * 128]
    fv = w_in[:, sp[5] + h0 * 128: sp[5] + (h0 + 2) * 128]
    ff = w_in[:, sp[6] + h0: sp[6] + h0 + 2]
    ga = w_in[:, sp[7] + h0 * 128: sp[7] + (h0 + 2) * 128]
    gb = w_in[:, sp[8] + h0 * 128: sp[8] + (h0 + 2) * 128]
    w_own = np.ascontiguousarray(np.concatenate([cq, ckv, kpe, fv, ff, fq, fk, ga, gb], axis=1)).astype(f)
    assert w_own.shape[1] == NCOLS
    w_qup = np.ascontiguousarray(inp["w_q_up"][0][:, h0 * 192:(h0 + 2) * 192])
    w_kvup = np.ascontiguousarray(inp["w_kv_up"][0][:, h0 * 256:(h0 + 2) * 256])
    gvec = np.zeros(NV, f)
    gvec[QL:QL + 512] = inp["mla_q_latent_g"][0]
    gvec[KL:KL + 512] = inp["mla_kv_latent_g"][0]
    gvec[QN:QN + 192] = inp["mla_q_norm_g"][0]
    gvec[KN:KN + 192] = inp["mla_k_norm_g"][0]
    gvec[FQ:FQ + 128] = inp["fox_q_norm_g"][0]
    gvec[FK:FK + 128] = inp["fox_k_norm_g"][0]
    gvec[BF:BF + 2] = inp["b_forget"][0][h0:h0 + 2]
    bg = inp["b_gate"][0]
    gvec[BG:BG + 256] = bg[h0 * 128:(h0 + 2) * 128]
    gvec[BG + 256:BG + 512] = bg[2048 + h0 * 128:2048 + (h0 + 2) * 128]
    gvec[IF:IF + 32] = (10000.0 ** (-np.arange(32, dtype=np.float32) / 32)).astype(f)
    gv = np.ascontiguousarray(np.broadcast_to(gvec, (128, NV)))
    pos = np.ascontiguousarray(inp["positions"][0, :S].reshape(S // 128, 128).T).astype(np.int32)
    return {"x": x, "w_in": w_own, "w_qup": w_qup, "w_kvup": w_kvup, "gmix": np.ascontiguousarray(inp["mix_norm_g"][0]), "gv": gv, "pos": pos}


def build_B(kb, Tn, io, R_mtin=None):
    NT = Tn // 128
    mtin, x_own, w_out, gffn, gffn_b, w_pq, skeys = io["mtin"], io["x_own"], io["w_out"], io["gffn"], io["gffn_b"], io["w_pq"], io["skeys"]
    pu, pv, out, x2d, h2d, pout = io["pu"], io["pv"], io["out"], io["x2d"], io["h2d"], io["pout"]
    if R_mtin is None:
        R_mtin = Res("mtin")
    R_x2d, R_h2d, R_pout = Res("x2d"), Res("h2d"), Res("pout")
    V = lambda fn, r=(), w=(): kb.op("dve", fn, r, w)
    A = lambda fn, r=(), w=(): kb.op("act", fn, r, w)
    P = lambda fn, r=(), w=(): kb.op("pool", fn, r, w)
    T = lambda fn, r=(), w=(): kb.op("pe", fn, r, w, pe_acc=True)
    pbk = kb.pbank
    R_pb = [Res("pbB%d" % i) for i in range(8)]

    identf = kb.al([128, 128], F32)
    ident = kb.al([128, 128], BF16)
    nhalf = kb.al([128, 8], F32)
    iota_i = kb.al([128, 16], I32)
    iota_f = kb.al([128, 16], F32)
    R_c = Res("constsB")
    P(lambda e: e.memset(identf[:], 0.0), w=[R_c])
    P(lambda e: e.affine_select(out=identf[:], in_=identf[:], pattern=[[-1, 128]], compare_op=ALU.not_equal,
                                fill=1.0, base=0, channel_multiplier=1), r=[R_c], w=[R_c])
    P(lambda e: e.memset(nhalf[:], -0.5), w=[R_c])
    P(lambda e: e.iota(iota_i[:], pattern=[[1, 16]], base=0, channel_multiplier=0), w=[R_c])
    V(lambda e: e.tensor_copy(out=ident[:], in_=identf[:]), r=[R_c], w=[R_c])
    V(lambda e: e.tensor_copy(out=iota_f[:], in_=iota_i[:]), r=[R_c], w=[R_c])
    gT2 = kb.al([128, 16], F32)
    R_g = Res("gB")
    kb.dma("sp", gT2[:], gffn.rearrange("(c p) -> p c", p=128), w=[R_g], allow_slow_non_contiguous=True)

    m0 = kb.mark()
    ar2 = kb.al([128, 32 * Tn], BF16)
    ar3 = kb.al([128, 16384], BF16)
    R_ar2, R_ar3 = Res("ar2"), Res("ar3")
    mT = ar2[:, 0:32 * Tn].rearrange("p (c t) -> p c t", c=32)
    wo = ar3[:, :].rearrange("p (c n) -> p c n", c=32)
    for c in range(8):
        kb.dma("sp", mT[:, c * 4:(c + 1) * 4, :], mtin[c].rearrange("h d t -> d h t"), r=[R_mtin], w=[R_ar2])
    w_out_v = w_out.rearrange("(c p) n -> p c n", p=128)
    xs = [kb.al([128, 512], F32) for _ in range(2)]
    R_xs = [Res("xs0"), Res("xs1")]
    n = 0
    for ds in range(4):
        for g in range(8):
            kb.dma("pool", wo[:, 4 * g:4 * g + 4, :], w_out_v[:, 4 * g:4 * g + 4, ds * 512:(ds + 1) * 512], w=[R_ar3])
        for tt in range(NT):
            b = n % 2
            n += 1
            kb.dma("sp", xs[b][:], x_own[tt * 128:(tt + 1) * 128, ds * 512:(ds + 1) * 512], w=[R_xs[b]])
            pk = pbk[b]
            for c in range(32):
                T(lambda e, pk=pk, c=c, tt=tt: e.matmul(out=pk[:], lhsT=mT[:, c, tt * 128:(tt + 1) * 128], rhs=wo[:, c, :], start=(c == 0), stop=(c == 31)),
                  r=[R_ar2, R_ar3], w=[R_pb[b]])
            V(lambda e, b=b, pk=pk: e.tensor_tensor(out=xs[b][:], in0=xs[b][:], in1=pk[:], op=ALU.add), r=[R_xs[b], R_pb[b]], w=[R_xs[b]])
            kb.dma("sp", x2d[tt * 128:(tt + 1) * 128, ds * 512:(ds + 1) * 512], xs[b][:], r=[R_xs[b]], w=[R_x2d])

    kb.release(m0)
    h2T = kb.al([128, 16, Tn], BF16)
    R_h2T = Res("h2T")
    m1 = kb.mark()
    gb = kb.al([128, D], F32)
    kb.dma("sp", gb[:], gffn_b, w=[R_g])
    xt = [kb.al([128, D], F32) for _ in range(2)]
    R_xt = [Res("xtB0"), Res("xtB1")]
    junk = kb.al([128, D], BF16)
    R_junk = Res("junkB")
    xn = kb.al([128, D], BF16)
    R_xn = Res("xnB")
    hrow = kb.al([128, D], BF16)
    R_hrow = Res("hrow")
    st = kb.al([128, 8], F32)
    rs = kb.al([128, 8], F32)
    R_st = Res("stB")
    pT = pbk[7][:, :].bitcast(BF16)
    pTv = pT.rearrange("p (c t) -> p c t", c=8)
    R_pT = R_pb[7]
    for tt in range(NT):
        b = tt % 2
        kb.dma("sp", xt[b][:], x2d[tt * 128:(tt + 1) * 128, :], r=[R_x2d], w=[R_xt[b]])
        A(lambda e, b=b: e.activation(out=junk[:], in_=xt[b][:], func=AF.Square, accum_out=st[:, 0:1]), r=[R_xt[b]], w=[R_junk, R_st])
        V(lambda e: e.tensor_scalar(out=rs[:, 0:1], in0=st[:, 0:1], scalar1=1.0 / D, scalar2=EPS, op0=ALU.mult, op1=ALU.add), r=[R_st], w=[R_st])
        P(lambda e: e.tensor_tensor(out=rs[:, 0:1], in0=rs[:, 0:1], in1=nhalf[:, 0:1], op=ALU.pow), r=[R_st, R_c], w=[R_st])
        V(lambda e, b=b: e.tensor_scalar(out=xn[:], in0=xt[b][:], scalar1=rs[:, 0:1], scalar2=None, op0=ALU.mult), r=[R_xt[b], R_st], w=[R_xn])
        for half in range(2):
            for c in range(8):
                cc = half * 8 + c
                T(lambda e, c=c, cc=cc: e.transpose(out=pT[:, c * 128:(c + 1) * 128], in_=xn[:, cc * 128:(cc + 1) * 128], identity=ident[:]), r=[R_xn, R_c], w=[R_pT])
            V(lambda e, half=half, tt=tt: e.tensor_tensor(out=h2T[:, half * 8:half * 8 + 8, tt * 128:(tt + 1) * 128], in0=pTv,
                                                          in1=gT2[:, half * 8:half * 8 + 8].unsqueeze(2).to_broadcast([128, 8, 128]), op=ALU.mult),
              r=[R_pT, R_g], w=[R_h2T])
        V(lambda e: e.tensor_tensor(out=hrow[:], in0=xn[:], in1=gb[:], op=ALU.mult), r=[R_xn, R_g], w=[R_hrow])
        kb.dma("sp", h2d[tt * 128:(tt + 1) * 128, :], hrow[:], r=[R_hrow], w=[R_h2d])

    kb.release(m1)
    ar3 = kb.al([128, 16 * Tn], BF16)
    skT = kb.al([128, 16, 128], BF16)
    R_ar2, R_ar3 = Res("ar2b"), Res("ar3b")
    m2 = kb.mark()
    ar2 = kb.al([128, 32768], BF16)
    R_junk = Res("junkB2")
    wpq = ar2[:, :].rearrange("p (c n) -> p c n", c=16)
    w_pq_v = w_pq.rearrange("(c p) n -> p c n", p=128)
    for c in range(16):
        kb.dma("pool", wpq[:, c, :], w_pq_v[:, c, :], w=[R_ar2])
    qT = ar3[:, 0:16 * Tn].rearrange("p (c t) -> p c t", c=16)
    skb = kb.al([128, 16, 128], BF16)
    R_sk = Res("sk")
    kb.dma("pool", skb[:], skeys.rearrange("h k d -> k h d"), w=[R_sk])
    for half in range(2):
        for c in range(8):
            T(lambda e, c=c, half=half: e.transpose(out=pT[:, c * 128:(c + 1) * 128], in_=skb[:, half * 8 + c, :], identity=ident[:]), r=[R_sk, R_c], w=[R_pT])
        V(lambda e, half=half: e.tensor_copy(out=skT[:, half * 8:half * 8 + 8, :], in_=pTv), r=[R_pT], w=[R_sk])
    W = min(512, Tn)
    n = 0
    for hc in range(16):
        for c0 in range(0, Tn, W):
            b = n % 2
            n += 1
            pk = pbk[b]
            for c in range(16):
                T(lambda e, pk=pk, c=c, hc=hc, c0=c0: e.matmul(out=pk[:, 0:W], lhsT=wpq[:, c, hc * 128:(hc + 1) * 128], rhs=h2T[:, c, c0:c0 + W], start=(c == 0), stop=(c == 15)),
                  r=[R_ar2, R_h2T], w=[R_pb[b]])
            A(lambda e, pk=pk, hc=hc, c0=c0: e.copy(out=qT[:, hc, c0:c0 + W], in_=pk[:, 0:W]), r=[R_pb[b]], w=[R_ar3])

    kb.release(m2)
    eiT = kb.al([128, Tn], I32)
    gtT = kb.al([128, Tn], F32)
    m3 = kb.mark()
    sc = kb.al([128, 16, 128], F32)
    R_sc = Res("sc")
    wk = kb.al([128, 256], F32)
    R_wk = Res("wk")
    tv = kb.al([128, 16, 16], F32)
    ti = kb.al([128, 16, 16], U32)
    tif = kb.al([128, 16, 16], F32)
    R_tv = Res("tv")
    cand = kb.al([128, 8, 16, 16], F32)
    R_cand = Res("cand")
    cv = kb.al([128, 8, 16], F32)
    cp = kb.al([128, 8, 16], U32)
    ci = kb.al([128, 8, 16], I32)
    ikf = kb.al([128, 8, 16], F32)
    jkf = kb.al([128, 8, 16], F32)
    R_cv = Res("cv")
    eq = kb.al([128, 8, 16, 16], F32)
    R_eq = Res("eq")
    i1s = kb.al([128, 8, 16], F32)
    i2s = kb.al([128, 8, 16], F32)
    ef = kb.al([128, 128], F32)
    gt = kb.al([128, 8, 16], F32)
    zs = kb.al([128, 8], F32)
    R_sel = Res("sel")
    R_eiT = Res("eiT")
    tvv = tv[:, :, :].rearrange("p (h c) k -> p h c k", c=2)
    tifv = tif[:, :, :].rearrange("p (h c) k -> p h c k", c=2)
    pTf = pbk[6]
    for tt in range(NT):
        for hc in range(16):
            T(lambda e, hc=hc, tt=tt: e.matmul(out=pbk[hc // 4][:, (hc % 4) * 128:(hc % 4 + 1) * 128], lhsT=qT[:, hc, tt * 128:(tt + 1) * 128], rhs=skT[:, hc, :], start=True, stop=True),
              r=[R_ar3, R_sk], w=[R_pb[hc // 4]])
        for k in range(4):
            A(lambda e, k=k: e.copy(out=sc[:, 4 * k:4 * k + 4, :], in_=pbk[k][:, :].rearrange("p (a b) -> p a b", a=4)), r=[R_pb[k]], w=[R_sc])
        for hc in range(16):
            V(lambda e, hc=hc: e.max(out=tv[:, hc, 0:8], in_=sc[:, hc, :]), r=[R_sc], w=[R_tv])
            V(lambda e, hc=hc: e.max_index(out=ti[:, hc, 0:8], in_max=tv[:, hc, 0:8], in_values=sc[:, hc, :]), r=[R_sc, R_tv], w=[R_tv])
            V(lambda e, hc=hc: e.match_replace(out=wk[:, 0:128], in_to_replace=tv[:, hc, 0:8], in_values=sc[:, hc, :], imm_value=-1e30), r=[R_sc, R_tv], w=[R_wk])
            V(lambda e, hc=hc: e.max(out=tv[:, hc, 8:16], in_=wk[:, 0:128]), r=[R_wk], w=[R_tv])
            V(lambda e, hc=hc: e.max_index(out=ti[:, hc, 8:16], in_max=tv[:, hc, 8:16], in_values=wk[:, 0:128]), r=[R_wk, R_tv], w=[R_tv])
        V(lambda e: e.tensor_copy(out=tif[:], in_=ti[:]), r=[R_tv], w=[R_tv])
        V(lambda e: e.tensor_tensor(out=cand[:], in0=tvv[:, :, 0, :].unsqueeze(3).to_broadcast([128, 8, 16, 16]),
                                    in1=tvv[:, :, 1, :].unsqueeze(2).to_broadcast([128, 8, 16, 16]), op=ALU.add), r=[R_tv], w=[R_cand])
        for h in range(8):
            cf = cand[:, h, :, :].rearrange("p a b -> p (a b)")
            V(lambda e, h=h, cf=cf: e.max(out=cv[:, h, 0:8], in_=cf), r=[R_cand], w=[R_cv])
            V(lambda e, h=h, cf=cf: e.max_index(out=cp[:, h, 0:8], in_max=cv[:, h, 0:8], in_values=cf), r=[R_cand, R_cv], w=[R_cv])
            V(lambda e, h=h, cf=cf: e.match_replace(out=wk[:], in_to_replace=cv[:, h, 0:8], in_values=cf, imm_value=-1e30), r=[R_cand, R_cv], w=[R_wk])
            V(lambda e, h=h: e.max(out=cv[:, h, 8:16], in_=wk[:]), r=[R_wk], w=[R_cv])
            V(lambda e, h=h: e.max_index(out=cp[:, h, 8:16], in_max=cv[:, h, 8:16], in_values=wk[:]), r=[R_wk, R_cv], w=[R_cv])
        V(lambda e: e.tensor_tensor(out=gt[:], in0=cv[:], in1=cv[:, :, 0:1].to_broadcast([128, 8, 16]), op=ALU.subtract), r=[R_cv], w=[R_sel])
        A(lambda e: e.activation(out=gt[:], in_=gt[:], func=AF.Exp), r=[R_sel], w=[R_sel])
        V(lambda e: e.tensor_reduce(out=zs[:], in_=gt[:], axis=AX.X, op=ALU.add), r=[R_sel], w=[R_sel])
        V(lambda e: e.reciprocal(out=zs[:], in_=zs[:]), r=[R_sel], w=[R_sel])
        V(lambda e: e.tensor_tensor(out=gt[:], in0=gt[:], in1=zs[:, :].unsqueeze(2).to_broadcast([128, 8, 16]), op=ALU.mult), r=[R_sel], w=[R_sel])
        cpi = cp[:, :, :].bitcast(I32)
        V(lambda e, cpi=cpi: e.tensor_single_scalar(out=ci[:], in_=cpi, scalar=4, op=ALU.arith_shift_right), r=[R_cv], w=[R_cv])
        V(lambda e: e.tensor_copy(out=ikf[:], in_=ci[:]), r=[R_cv], w=[R_cv])
        V(lambda e, cpi=cpi: e.tensor_single_scalar(out=ci[:], in_=cpi, scalar=15, op=ALU.bitwise_and), r=[R_cv], w=[R_cv])
        V(lambda e: e.tensor_copy(out=jkf[:], in_=ci[:]), r=[R_cv], w=[R_cv])
        io16 = iota_f[:, :].unsqueeze(1).unsqueeze(1).to_broadcast([128, 8, 16, 16])
        for sel, kf, c in ((i1s, ikf, 0), (i2s, jkf, 1)):
            V(lambda e, kf=kf: e.tensor_tensor(out=eq[:], in0=io16, in1=kf[:, :, :].unsqueeze(3).to_broadcast([128, 8, 16, 16]), op=ALU.is_equal), r=[R_cv, R_c], w=[R_eq])
            V(lambda e, c=c: e.tensor_tensor(out=eq[:], in0=eq[:], in1=tifv[:, :, c, :].unsqueeze(2).to_broadcast([128, 8, 16, 16]), op=ALU.mult), r=[R_eq, R_tv], w=[R_eq])
            V(lambda e, sel=sel: e.tensor_reduce(out=sel[:], in_=eq[:], axis=AX.X, op=ALU.add), r=[R_eq], w=[R_sel])
        V(lambda e: e.scalar_tensor_tensor(out=ef[:], in0=i1s[:].rearrange("p h k -> p (h k)"), scalar=128.0, in1=i2s[:].rearrange("p h k -> p (h k)"), op0=ALU.mult, op1=ALU.add), r=[R_sel], w=[R_sel])
        T(lambda e: e.transpose(out=pTf[:, 0:128], in_=ef[:], identity=identf[:]), r=[R_sel, R_c], w=[R_pb[6]])
        T(lambda e: e.transpose(out=pTf[:, 128:256], in_=gt[:].rearrange("p h k -> p (h k)"), identity=identf[:]), r=[R_sel, R_c], w=[R_pb[6]])
        V(lambda e, tt=tt: e.tensor_copy(out=eiT[:, tt * 128:(tt + 1) * 128], in_=pTf[:, 0:128]), r=[R_pb[6]], w=[R_eiT])
        V(lambda e, tt=tt: e.tensor_copy(out=gtT[:, tt * 128:(tt + 1) * 128], in_=pTf[:, 128:256]), r=[R_pb[6]], w=[R_eiT])

    if "dbg_ei" in io:
        kb.dma("sp", io["dbg_ei"], eiT[:], r=[R_eiT])
        kb.dma("sp", io["dbg_gt"], gtT[:], r=[R_eiT])
    kb.release(m3)
    junk5 = kb.al([128, D], BF16)
    R_junk5 = Res("junk5B5")
    ug = [kb.al([128, D], BF16) for _ in range(2)]
    vg = [kb.al([128, D], BF16) for _ in range(2)]
    hb = [kb.al([128, D], BF16) for _ in range(2)]
    R_ug = [Res("ug0"), Res("ug1")]
    R_vg = [Res("vg0"), Res("vg1")]
    R_hb = [Res("hb0"), Res("hb1")]
    acol = kb.al([128, 4], F32)
    wv = kb.al([128, 2], BF16)
    R_a = Res("acol")
    orow = [kb.al([1, D], F32) for _ in range(2)]
    R_or = [Res("or0"), Res("or1")]
    for t in range(Tn):
        b = t % 2
        kb.dma("pool", None, None, r=[R_eiT], w=[R_ug[b]],
               fn=lambda e, b=b, t=t: e.indirect_dma_start(out=ug[b][:], out_offset=None, in_=pu, in_offset=bass.IndirectOffsetOnAxis(ap=eiT[:, t:t + 1], axis=0)))
        kb.dma("pool", None, None, r=[R_eiT], w=[R_vg[b]],
               fn=lambda e, b=b, t=t: e.indirect_dma_start(out=vg[b][:], out_offset=None, in_=pv, in_offset=bass.IndirectOffsetOnAxis(ap=eiT[:, t:t + 1], axis=0)))
        kb.dma("sp", hb[b][:], h2d[t:t + 1, :].partition_broadcast(128)[:, 0, :], r=[R_h2d], w=[R_hb[b]])
        V(lambda e, b=b: e.scalar_tensor_tensor(out=junk5[:], in0=ug[b][:], scalar=1.0, in1=hb[b][:], op0=ALU.mult, op1=ALU.mult, accum_out=acol[:, 0:1]),
          r=[R_ug[b], R_hb[b]], w=[R_junk5, R_a])
        A(lambda e: e.activation(out=acol[:, 1:2], in_=acol[:, 0:1], func=AF.Gelu), r=[R_a], w=[R_a])
        V(lambda e, t=t: e.tensor_tensor(out=wv[:, 0:1], in0=acol[:, 1:2], in1=gtT[:, t:t + 1], op=ALU.mult), r=[R_a, R_eiT], w=[R_a])
        for k in range(4):
            pk = pbk[4 * b + k]
            T(lambda e, pk=pk, k=k, b=b: e.matmul(out=pk[0:1, :], lhsT=wv[:, 0:1], rhs=vg[b][:, k * 512:(k + 1) * 512], start=True, stop=True),
              r=[R_a, R_vg[b]], w=[R_pb[4 * b + k]])
            A(lambda e, pk=pk, k=k, b=b: e.copy(out=orow[b][0:1, k * 512:(k + 1) * 512], in_=pk[0:1, :]), r=[R_pb[4 * b + k]], w=[R_or[b]])
        kb.dma("sp", pout[t:t + 1, :], orow[b][:], r=[R_or[b]], w=[R_pout])

    kb.release(m3)
    xt6 = [kb.al([128, D], F32) for _ in range(2)]
    R_xt6 = [Res("xtB60"), Res("xtB61")]
    po = [kb.al([128, D], F32) for _ in range(2)]
    R_po = [Res("po0"), Res("po1")]
    for tt in range(NT):
        b = tt % 2
        kb.dma("sp", xt6[b][:], x2d[tt * 128:(tt + 1) * 128, :], r=[R_x2d], w=[R_xt6[b]])
        kb.dma("sp", po[b][:], pout[tt * 128:(tt + 1) * 128, :], r=[R_pout], w=[R_po[b]])
        V(lambda e, b=b: e.tensor_tensor(out=po[b][:], in0=po[b][:], in1=xt6[b][:], op=ALU.add), r=[R_po[b], R_xt6[b]], w=[R_po[b]])
        kb.dma("sp", out[tt * 128:(tt + 1) * 128, :], po[b][:], r=[R_po[b]])


def wout_perm():
    idx = []
    for i in range(8):
        for hh in range(4):
            base = (0 if hh < 2 else 2048) + (2 * i + hh % 2) * 128
            idx.extend(range(base, base + 128))
    return np.array(idx)


def prep_B(inp, core, Tn, S):
    f = np.float32
    t0 = core * Tn
    return {
        "x_own": np.ascontiguousarray(inp["x"][0, t0:t0 + Tn]),
        "w_out": np.ascontiguousarray(inp["w_out"][0][wout_perm()]),
        "gffn": np.ascontiguousarray(inp["ffn_norm_g"][0]),
        "gffn_b": np.ascontiguousarray(np.broadcast_to(inp["ffn_norm_g"][0], (128, D))),
        "w_pq": np.ascontiguousarray(inp["w_peer_q"][0]),
        "skeys": np.ascontiguousarray(inp["peer_sub_keys"][0].reshape(16, 128, 128)),
        "pu": np.ascontiguousarray(inp["peer_u"][0]),
        "pv": np.ascontiguousarray(inp["peer_v"][0]),
    }


S_FULL = 8192
_CACHE = {}


def _build_A_prog(S):
    kb = KB()
    io = {}
    shapes = {"x": ([S, 2048], F32), "w_in": ([2048, NCOLS], F32), "w_qup": ([512, 384], F32), "w_kvup": ([512, 512], F32),
              "gmix": ([2048], F32), "gv": ([128, NV], F32), "pos": ([128, S // 128], I32)}
    for k, (sh, dt) in shapes.items():
        io[k] = kb.dram(k, sh, dt, kind="ExternalInput").ap()
    io["qkg"] = kb.dram("qkg", [14, 128, S], BF16).ap()
    io["vd"] = kb.dram("vd", [S, 512], BF16).ap()
    io["mt"] = kb.dram("mt", [8, 4, 128, S // 8], BF16, kind="ExternalOutput").ap()
    build_A(kb, S, io)
    return kb.build()


def _build_B_prog(Tn):
    kb = KB()
    io = {}
    shapes = {"mtin": ([8, 4, 128, Tn], BF16), "x_own": ([Tn, 2048], F32), "w_out": ([4096, 2048], F32), "gffn": ([2048], F32),
              "gffn_b": ([128, 2048], F32), "w_pq": ([2048, 2048], F32), "skeys": ([16, 128, 128], F32),
              "pu": ([16384, 2048], F32), "pv": ([16384, 2048], F32)}
    for k, (sh, dt) in shapes.items():
        io[k] = kb.dram(k, sh, dt, kind="ExternalInput").ap()
    io["out"] = kb.dram("out", [Tn, 2048], F32, kind="ExternalOutput").ap()
    io["x2d"] = kb.dram("x2d", [Tn, 2048], F32).ap()
    io["h2d"] = kb.dram("h2d", [Tn, 2048], BF16).ap()
    io["pout"] = kb.dram("pout", [Tn, 2048], F32).ap()
    build_B(kb, Tn, io)
    return kb.build()


def kernel(**inputs):
    inp = {k: np.asarray(v) for k, v in inputs.items()}
    S = S_FULL
    Tn = S // 8
    ncA = _build_A_prog(S)
    insA = [prep_A(inp, c, S) for c in range(8)]
    resA = run_bass_kernel_spmd(ncA, insA, core_ids=list(range(8)))
    mts = [np.asarray(resA.results[c]["mt"]) for c in range(8)]
    ncB = _build_B_prog(Tn)
    insB = []
    for j in range(8):
        d = prep_B(inp, j, Tn, S)
        d["mtin"] = np.ascontiguousarray(np.stack([mts[i][j] for i in range(8)], axis=0))
        insB.append(d)
    resB = run_bass_kernel_spmd(ncB, insB, core_ids=list(range(8)))
    out = np.concatenate([np.asarray(resB.results[j]["out"]) for j in range(8)], axis=0)
    return out.reshape(1, S, 2048).astype(np.float32)
```

```python
import math
import numpy as np
from contextlib import ExitStack
import concourse.bass as bass
import concourse.mybir as mybir
from concourse.bass_utils import run_bass_kernel_spmd

F32 = mybir.dt.float32
BF16 = mybir.dt.bfloat16
I32 = mybir.dt.int32
U32 = mybir.dt.uint32
ALU = mybir.AluOpType
AF = mybir.ActivationFunctionType
AX = mybir.AxisListType

NDS = 40
NDS_HW = 26
ARENA_BYTES = 206 * 1024


class Res:
    __slots__ = ("name", "w", "rs")

    def __init__(self, name=""):
        self.name = name
        self.w = None
        self.rs = {}


class KB:
    ENG = ("pe", "act", "dve", "pool", "sp")

    def __init__(self):
        self.nc = bass.Bass("TRN2", target_bir_lowering=False)
        self.es = ExitStack()
        self.q = {e: [] for e in self.ENG}
        self.cnt = {e: 0 for e in self.ENG}
        self.seen = {e: {} for e in self.ENG}
        self.sem = {e: self.es.enter_context(self.nc.semaphore("s_" + e)) for e in self.ENG}
        self.dsems = [self.es.enter_context(self.nc.semaphore("d%d" % i)) for i in range(NDS)]
        self.dcnt = [0] * NDS
        self.dnext = 0
        self.dnext_sw = 0
        self.nuniq = 0
        self.big = self.es.enter_context(self.nc.sbuf_tensor("big", [128, ARENA_BYTES // 2], BF16))
        self.top = 0
        self.pbank = [self.es.enter_context(self.nc.psum_tensor("pbank%d" % i, [128, 512], F32)) for i in range(8)]

    def al(self, shape, dt, name=None, parts=None):
        esz = {F32: 4, I32: 4, U32: 4, BF16: 2}[dt]
        n = 1
        for d in shape[1:]:
            n *= d
        nbytes = (n * esz + 63) // 64 * 64
        assert self.top + nbytes <= ARENA_BYTES, ("arena overflow", self.top, nbytes, name)
        o = self.top // 2
        self.top += nbytes
        v = self.big[0:shape[0], o:o + (n * esz) // 2]
        if dt != BF16:
            v = v.bitcast(dt)
        if len(shape) == 3:
            v = v.rearrange("p (a b) -> p a b", a=shape[1])
        elif len(shape) == 4:
            v = v.rearrange("p (a b c) -> p a b c", a=shape[1], b=shape[2])
        return v

    def barrier(self):
        toks = [(o, self.sem[o], self.cnt[o]) for o in self.ENG if self.cnt[o] > 0]
        toks += [("d%d" % i, self.dsems[i], self.dcnt[i]) for i in range(NDS) if self.dcnt[i] > 0]
        for e in self.ENG:
            self._waits(e, toks)

    def arena_reset(self):
        self.barrier()
        self.top = 0

    def mark(self):
        return self.top

    def release(self, m):
        self.barrier()
        self.top = m

    def sb(self, shape, dt, name=None):
        self.nuniq += 1
        return self.es.enter_context(self.nc.sbuf_tensor(name or ("sb%d" % self.nuniq), list(shape), dt))

    def ps(self, shape, dt=F32, name=None):
        self.nuniq += 1
        return self.es.enter_context(self.nc.psum_tensor(name or ("ps%d" % self.nuniq), list(shape), dt))

    def dram(self, name, shape, dt, kind="Internal"):
        return self.nc.dram_tensor(name, list(shape), dt, kind=kind)

    def _collect(self, r, w):
        toks = []
        for x in r:
            toks.append(x.w)
        for x in w:
            toks.append(x.w)
            toks.extend(x.rs.values())
        return toks

    def _waits(self, eng, toks):
        need = {}
        seen = self.seen[eng]
        for t in toks:
            if t is None:
                continue
            k, h, v = t
            if seen.get(k, 0) >= v:
                continue
            if k not in need or need[k][1] < v:
                need[k] = (h, v)
        for k, (h, v) in need.items():
            seen[k] = v
            self.q[eng].append(lambda e, h=h, v=v: e.wait_ge(h, v))

    def _record(self, tok, r, w):
        k = tok[0]
        for x in r:
            old = x.rs.get(k)
            if old is None or old[2] < tok[2]:
                x.rs[k] = tok
        for x in w:
            x.w = tok
            x.rs = {}

    def op(self, eng, fn, r=(), w=(), pe_acc=False, inc=True):
        toks = self._collect(r, w)
        if pe_acc:
            toks = [t for t in toks if t is None or t[0] != eng]
        self._waits(eng, toks)
        h = self.sem[eng]
        if inc:
            self.cnt[eng] += 1
            n = self.cnt[eng]
            self.q[eng].append(lambda e: fn(e).then_inc(h, 1))
        else:
            n = self.cnt[eng] + 1
            self.q[eng].append(lambda e: fn(e))
        tok = (eng, h, n)
        self._record(tok, r, w)
        return tok

    def dma(self, qe, out, in_, r=(), w=(), fn=None, **kw):
        toks = self._collect(r, w)
        if qe == "pool":
            i = NDS_HW + self.dnext_sw
            self.dnext_sw = (self.dnext_sw + 1) % (NDS - NDS_HW)
        else:
            i = self.dnext
            self.dnext = (i + 1) % NDS_HW
        key = "d%d" % i
        h = self.dsems[i]
        if self.dcnt[i] > 0:
            toks.append((key, h, self.dcnt[i]))
        self._waits(qe, toks)
        self.dcnt[i] += 16
        v = self.dcnt[i]
        if fn is None:
            self.q[qe].append(lambda e: e.dma_start(out=out, in_=in_, **kw).then_inc(h, 16))
        else:
            self.q[qe].append(lambda e: fn(e).then_inc(h, 16))
        tok = (key, h, v)
        self._record(tok, r, w)
        return tok

    def wait_all_dma(self, eng="sp"):
        toks = [("d%d" % i, self.dsems[i], self.dcnt[i]) for i in range(NDS) if self.dcnt[i] > 0]
        self._waits(eng, toks)

    def build(self):
        self.wait_all_dma("sp")
        nc = self.nc
        q = self.q
        with nc.Block() as block:
            @block.tensor
            def _(e):
                for f in q["pe"]:
                    f(e)

            @block.scalar
            def _(e):
                for f in q["act"]:
                    f(e)

            @block.vector
            def _(e):
                for f in q["dve"]:
                    f(e)

            @block.gpsimd
            def _(e):
                for f in q["pool"]:
                    f(e)

            @block.sync
            def _(e):
                for f in q["sp"]:
                    f(e)
        self.es.close()
        return nc


D = 2048
EPS = 1e-6
NC = 8
NH = 16
G_KL, G_QL, G_QN, G_KN, G_FQ, G_FK, G_BF, G_IF = 0, 512, 1024, 1216, 1408, 1536, 1664, 1680
G_NV = 1712


def build_F(kb, S, io):
    NT = S // 128
    NU = NT // NC
    TO = NU * 128
    x, xo = io["x"], io["x_own"]
    V = lambda fn, r=(), w=(): kb.op("dve", fn, r, w)
    A = lambda fn, r=(), w=(): kb.op("act", fn, r, w)
    P = lambda fn, r=(), w=(): kb.op("pool", fn, r, w)
    T = lambda fn, r=(), w=(), inc=True: kb.op("pe", fn, r, w, pe_acc=True, inc=inc)
    pbk = kb.pbank
    R_pb = [Res("pb%d" % i) for i in range(8)]
    pT = pbk[7][:, :].bitcast(BF16)
    pTv = pT.rearrange("p (c t) -> p c t", c=8)
    R_pT = R_pb[7]
    pT2 = [pT, pbk[6][:, :].bitcast(BF16)]
    pT2v = [pTv, pT2[1].rearrange("p (c t) -> p c t", c=8)]
    R_pT2 = [R_pb[7], R_pb[6]]
    tcount = [0]
    ktn_d, ktp_d, ktf_d, vm_d, vf_d, ht_d = io["ktn_d"], io["ktp_d"], io["ktf_d"], io["vm_d"], io["vf_d"], io["ht_d"]
    qtn_d, qtp_d, qtf_d, gt_d = io["qtn_d"], io["qtp_d"], io["qtf_d"], io["gt_d"]
    R_ktn, R_ktp, R_ktf, R_vm, R_vf, R_htd = Res(), Res(), Res(), Res(), Res(), Res()
    R_qtn, R_qtp, R_qtf, R_gtd = Res(), Res(), Res(), Res()

    identf = kb.al([128, 128], F32)
    ident = kb.al([128, 128], BF16)
    trif = kb.al([128, 128], F32)
    onesf = kb.al([128, 128], F32)
    onesb = kb.al([128, 128], BF16)
    nhalf = kb.al([128, 16], F32)
    R_c = Res("consts")
    P(lambda e: e.memset(identf[:], 0.0), w=[R_c])
    P(lambda e: e.affine_select(out=identf[:], in_=identf[:], pattern=[[-1, 128]], compare_op=ALU.not_equal, fill=1.0, base=0, channel_multiplier=1), r=[R_c], w=[R_c])
    P(lambda e: e.memset(onesf[:], 1.0), w=[R_c])
    P(lambda e: e.affine_select(out=trif[:], in_=onesf[:], pattern=[[1, 128]], compare_op=ALU.is_ge, fill=0.0, base=0, channel_multiplier=-1), r=[R_c], w=[R_c])
    P(lambda e: e.memset(nhalf[:], -0.5), w=[R_c])
    epsc = kb.al([128, 1], F32)
    P(lambda e: e.memset(epsc[:], EPS), w=[R_c])
    V(lambda e: e.tensor_copy(out=ident[:], in_=identf[:]), r=[R_c], w=[R_c])
    V(lambda e: e.tensor_copy(out=onesb[:], in_=onesf[:]), r=[R_c], w=[R_c])
    gT = kb.al([128, 16], F32)
    gv = kb.al([128, G_NV], F32)
    R_g = Res("gains")
    kb.dma("sp", gT[:], io["gmix"].rearrange("(c p) -> p c", p=128), w=[R_g], allow_slow_non_contiguous=True)
    kb.dma("sp", gv[:], io["gvf"], w=[R_g])
    V(lambda e: e.tensor_scalar(out=gv[:, G_QN:G_QN + 192], in0=gv[:, G_QN:G_QN + 192], scalar1=192 ** -0.5, scalar2=None, op0=ALU.mult), r=[R_g], w=[R_g])
    V(lambda e: e.tensor_scalar(out=gv[:, G_FQ:G_FQ + 128], in0=gv[:, G_FQ:G_FQ + 128], scalar1=128 ** -0.5, scalar2=None, op0=ALU.mult), r=[R_g], w=[R_g])
    flog = kb.al([128, NH, NT], F32)
    cend = kb.al([128, NH, NT], F32)
    R_fl = Res("flog")
    st = kb.al([128, 64], F32)
    rs = kb.al([128, 64], F32)
    R_st = Res("st")
    iota_i = kb.al([128, 16], I32)
    iota_f = kb.al([128, 16], F32)
    gT2 = kb.al([128, 16], F32)
    R_gB = Res("gB")
    P(lambda e: e.iota(iota_i[:], pattern=[[1, 16]], base=0, channel_multiplier=0), w=[R_c])
    V(lambda e: e.tensor_copy(out=iota_f[:], in_=iota_i[:]), r=[R_c], w=[R_c])
    kb.dma("sp", gT2[:], io["gffn"].rearrange("(c p) -> p c", p=128), w=[R_gB], allow_slow_non_contiguous=True)
    m_base = kb.mark()

    def rstd(a, b, inv_n, S_=None):
        st_, rs_, R_ = S_ if S_ is not None else (st, rs, R_st)
        A(lambda e: e.activation(out=rs_[:, a:b], in_=st_[:, a:b], func=AF.Sqrt, scale=inv_n, bias=epsc[:, 0:1]), r=[R_, R_c], w=[R_])
        V(lambda e: e.reciprocal(out=rs_[:, a:b], in_=rs_[:, a:b]), r=[R_], w=[R_])

    def rope_tables(posd, n, sin_t, cos_t, R_rp):
        posi = kb.al([128, n], I32)
        posf = kb.al([128, n], F32)
        ang = kb.al([128, n, 32], F32)
        tq = kb.al([128, n, 32], F32)
        tki = kb.al([128, n, 32], I32)
        kb.dma("sp", posi[:], posd, w=[R_rp])
        V(lambda e: e.tensor_copy(out=posf[:], in_=posi[:]), r=[R_rp], w=[R_rp])
        V(lambda e: e.tensor_tensor(out=ang[:], in0=posf[:, :].unsqueeze(2).to_broadcast([128, n, 32]),
                                    in1=gv[:, G_IF:G_IF + 32].unsqueeze(1).to_broadcast([128, n, 32]), op=ALU.mult), r=[R_rp, R_g], w=[R_rp])
        for dst, shift in ((sin_t, 0.0), (cos_t, math.pi / 2)):
            V(lambda e, shift=shift: e.tensor_scalar(out=tq[:], in0=ang[:], scalar1=shift, scalar2=1.0 / (2 * math.pi), op0=ALU.add, op1=ALU.mult), r=[R_rp], w=[R_rp])
            V(lambda e: e.tensor_copy(out=tki[:], in_=tq[:]), r=[R_rp], w=[R_rp])
            V(lambda e: e.tensor_copy(out=tq[:], in_=tki[:]), r=[R_rp], w=[R_rp])
            V(lambda e: e.tensor_scalar(out=tq[:], in0=tq[:], scalar1=-2 * math.pi, scalar2=None, op0=ALU.mult), r=[R_rp], w=[R_rp])
            V(lambda e, shift=shift: e.scalar_tensor_tensor(out=tq[:], in0=ang[:], scalar=shift, in1=tq[:], op0=ALU.add, op1=ALU.add), r=[R_rp], w=[R_rp])
            V(lambda e, dst=dst: e.tensor_scalar(out=dst[:], in0=tq[:], scalar1=math.pi, scalar2=-2 * math.pi, op0=ALU.is_gt, op1=ALU.mult), r=[R_rp], w=[R_rp])
            V(lambda e, dst=dst: e.tensor_tensor(out=tq[:], in0=tq[:], in1=dst[:], op=ALU.add), r=[R_rp], w=[R_rp])
            V(lambda e: e.tensor_scalar(out=tq[:], in0=tq[:], scalar1=-3.1415925, scalar2=3.1415925, op0=ALU.max, op1=ALU.min), r=[R_rp], w=[R_rp])
            A(lambda e, dst=dst: e.activation(out=dst[:], in_=tq[:], func=AF.Sin), r=[R_rp], w=[R_rp])

    def load_w(dst, src, c0, c1, R):
        sv = src.rearrange("(c p) n -> p c n", p=128)
        n = c1 - c0
        step = 2048
        for c in range(0, 16, 4):
            for a in range(0, n, step):
                b = min(n, a + step)
                kb.dma("pool", dst[:, c:c + 4, a:b], sv[:, c:c + 4, c0 + a:c0 + b], w=[R])

    def x_to_hT(xt_b, R_xt_b, junk, R_junk, xn, R_xn, hT, R_hT, S_=None):
        st_, rs_, R_ = S_ if S_ is not None else (st, rs, R_st)
        A(lambda e: e.activation(out=junk[:], in_=xt_b[:], func=AF.Square, accum_out=st_[:, 0:1]), r=[R_xt_b], w=[R_junk, R_])
        rstd(0, 1, 1.0 / D, S_)
        V(lambda e: e.tensor_scalar(out=xn[:], in0=xt_b[:], scalar1=rs_[:, 0:1], scalar2=None, op0=ALU.mult), r=[R_xt_b, R_], w=[R_xn])
        for half in range(2):
            ti_ = tcount[0] % 2
            tcount[0] += 1
            for c in range(8):
                cc = half * 8 + c
                T(lambda e, c=c, cc=cc, ti_=ti_: e.transpose(out=pT2[ti_][:, c * 128:(c + 1) * 128], in_=xn[:, cc * 128:(cc + 1) * 128], identity=ident[:]), r=[R_xn, R_c], w=[R_pT2[ti_]], inc=(c == 7))
            V(lambda e, half=half, ti_=ti_: e.tensor_tensor(out=hT[:, half * 8:half * 8 + 8, :], in0=pT2v[ti_], in1=gT[:, half * 8:half * 8 + 8].unsqueeze(2).to_broadcast([128, 8, 128]), op=ALU.mult),
              r=[R_pT2[ti_], R_g], w=[R_hT])

    def head_norm(src, nh, dh, so, R_src, extra=None, R_extra=None):
        sq = kb_sq[:, 0:nh * dh].rearrange("p (h d) -> p h d", h=nh)
        A(lambda e: e.activation(out=sq, in_=src, func=AF.Square), r=[R_src], w=[R_sq])
        V(lambda e: e.tensor_reduce(out=st[:, so:so + nh], in_=sq, axis=AX.X, op=ALU.add), r=[R_sq], w=[R_st])
        if extra is not None:
            V(lambda e: e.tensor_scalar(out=st[:, so:so + nh], in0=st[:, so:so + nh], scalar1=extra, scalar2=None, op0=ALU.add), r=[R_st] + ([R_extra] if R_extra is not None else []), w=[R_st])

    def scale_heads(dst, src, nh, dh, so, goff, R_src, R_dst, tmp):
        V(lambda e: e.tensor_tensor(out=tmp, in0=src, in1=rs[:, so:so + nh].unsqueeze(2).to_broadcast([128, nh, dh]), op=ALU.mult), r=[R_src, R_st], w=[R_sq])
        V(lambda e: e.tensor_tensor(out=dst, in0=tmp, in1=gv[:, goff:goff + dh].unsqueeze(1).to_broadcast([128, nh, dh]), op=ALU.mult), r=[R_sq, R_g], w=[R_dst])

    def rope_heads(pe_in, pe_out, cs_row, sn_row, nh, R_in, R_out, R_rp):
        cs = cs_row.unsqueeze(1).to_broadcast([128, nh, 32])
        sn = sn_row.unsqueeze(1).to_broadcast([128, nh, 32])
        x1, x2 = pe_in[:, :, 0:32], pe_in[:, :, 32:64]
        t = [kb_pet[:, i, 0:nh, :] for i in range(4)]
        V(lambda e: e.tensor_tensor(out=t[0], in0=x1, in1=cs, op=ALU.mult), r=[R_in, R_rp], w=[R_pet])
        V(lambda e: e.tensor_tensor(out=t[1], in0=x2, in1=sn, op=ALU.mult), r=[R_in, R_rp], w=[R_pet])
        V(lambda e: e.tensor_tensor(out=t[2], in0=x1, in1=sn, op=ALU.mult), r=[R_in, R_rp], w=[R_pet])
        V(lambda e: e.tensor_tensor(out=t[3], in0=x2, in1=cs, op=ALU.mult), r=[R_in, R_rp], w=[R_pet])
        V(lambda e: e.tensor_tensor(out=pe_out[:, :, 0:32], in0=t[0], in1=t[1], op=ALU.subtract), r=[R_pet], w=[R_out])
        V(lambda e: e.tensor_tensor(out=pe_out[:, :, 32:64], in0=t[2], in1=t[3], op=ALU.add), r=[R_pet], w=[R_out])

    def transpose_out(src_flat, nblk, dst_fn, R_src):
        for b0 in range(0, nblk, 8):
            b1 = min(nblk, b0 + 8)
            ti_ = tcount[0] % 2
            tcount[0] += 1
            for c in range(b1 - b0):
                T(lambda e, c=c, b0=b0, ti_=ti_: e.transpose(out=pT2[ti_][:, c * 128:(c + 1) * 128], in_=src_flat[:, (b0 + c) * 128:(b0 + c + 1) * 128], identity=ident[:]), r=[R_src, R_c], w=[R_pT2[ti_]], inc=(c == b1 - b0 - 1))
            sb_ = kb_stage_i[0] % 2
            kb_stage_i[0] += 1
            stg, R_stg = kb_stage[sb_], R_stage[sb_]
            A(lambda e, stg=stg, n=b1 - b0, ti_=ti_: e.copy(out=stg[:, 0:n, :], in_=pT2v[ti_][:, 0:n, :]), r=[R_pT2[ti_]], w=[R_stg])
            dst_fn(b0, b1, stg, R_stg)

    w1 = kb.al([128, 16, 592], BF16)
    wkvu = kb.al([128, 4, 4096], BF16)
    R_w1, R_wkvu = Res(), Res()
    load_w(w1, io["w_kv1"], 0, 592, R_w1)
    wkv_v = io["w_kvup"].rearrange("(c p) n -> p c n", p=128)
    for c in range(4):
        for a in range(0, 4096, 2048):
            kb.dma("pool", wkvu[:, c, a:a + 2048], wkv_v[:, c, a:a + 2048], w=[R_wkvu])
    sin_a = kb.al([128, NT, 32], F32)
    cos_a = kb.al([128, NT, 32], F32)
    R_rpa = Res("ropeA")
    m_r = kb.mark()
    rope_tables(io["pos_all"], NT, sin_a, cos_a, R_rpa)
    kb.release(m_r)
    xt = [kb.al([128, D], F32) for _ in range(2)]
    R_xt = [Res(), Res()]
    junk = kb.al([128, D], BF16)
    R_junk = Res()
    xn = kb.al([128, D], BF16)
    R_xn = Res()
    hT = kb.al([128, 16, 128], BF16)
    R_hT = Res()
    kb_sq = kb.al([128, 2048], F32)
    R_sq = Res()
    kb_pet = kb.al([128, 4, 16, 32], F32)
    R_pet = Res()
    kb_stage = [kb.al([128, 8, 128], BF16) for _ in range(2)]
    R_stage = [Res(), Res()]
    kb_stage_i = [0]
    lat = kb.al([128, 512], BF16)
    R_lat = Res()
    latT = kb.al([128, 4, 128], BF16)
    R_latT = Res()
    kpe_f = kb.al([128, 64], F32)
    R_kpe = Res()
    kbuf = kb.al([128, 16, 128], BF16)
    R_kbuf = Res()
    pein = kb.al([128, 16, 64], F32)
    R_pein = Res()
    peout = kb.al([128, 16, 64], BF16)
    R_peout = Res()
    vbuf = kb.al([128, 16, 128], BF16)
    R_vbuf = Res()
    tmp8 = kb_sq

    stA = kb.al([128, 8], F32)
    rsA = kb.al([128, 8], F32)
    R_stA = Res("stA")
    SA = (stA, rsA, R_stA)
    latT2 = [latT, kb.al([128, 4, 128], BF16)]
    R_latT2 = [R_latT, Res()]
    kpe2 = [kpe_f, kb.al([128, 64], F32)]
    R_kpe2 = [R_kpe, Res()]
    kb.dma("sp", xt[0][:], x[0:128, :], w=[R_xt[0]])

    def stageA(t):
        b = t % 2
        if t + 1 < NT:
            kb.dma("sp", xt[1 - b][:], x[(t + 1) * 128:(t + 2) * 128, :], w=[R_xt[1 - b]])
        x_to_hT(xt[b], R_xt[b], junk, R_junk, xn, R_xn, hT, R_hT, SA)
        kb.dma("sp", ht_d[t], hT[:], r=[R_hT], w=[R_htd])
        for c in range(16):
            T(lambda e, c=c: e.matmul(out=pbk[4][:], lhsT=hT[:, c, :], rhs=w1[:, c, 0:512], start=(c == 0), stop=(c == 15)), r=[R_hT, R_w1], w=[R_pb[4]], inc=(c == 15))
        for c in range(16):
            T(lambda e, c=c: e.matmul(out=pbk[5][:, 0:80], lhsT=hT[:, c, :], rhs=w1[:, c, 512:592], start=(c == 0), stop=(c == 15)), r=[R_hT, R_w1], w=[R_pb[5]], inc=(c == 15))
        A(lambda e: e.activation(out=junk[:, 0:512], in_=pbk[4][:], func=AF.Square, accum_out=stA[:, 0:1]), r=[R_pb[4]], w=[R_junk, R_stA])
        A(lambda e, b=b: e.activation(out=junk[:, 0:64], in_=pbk[5][:, 0:64], func=AF.Square, accum_out=stA[:, 2 + b:3 + b]), r=[R_pb[5]], w=[R_junk, R_stA])
        rstd(0, 1, 1.0 / 512, SA)
        V(lambda e: e.scalar_tensor_tensor(out=lat[:], in0=pbk[4][:], scalar=rsA[:, 0:1], in1=gv[:, G_KL:G_KL + 512], op0=ALU.mult, op1=ALU.mult), r=[R_pb[4], R_stA, R_g], w=[R_lat])
        A(lambda e, b=b: e.copy(out=kpe2[b][:], in_=pbk[5][:, 0:64]), r=[R_pb[5]], w=[R_kpe2[b]])
        A(lambda e, t=t: e.copy(out=flog[:, :, t], in_=pbk[5][:, 64:80]), r=[R_pb[5]], w=[R_fl])
        ti_ = tcount[0] % 2
        tcount[0] += 1
        for c in range(4):
            T(lambda e, c=c, ti_=ti_: e.transpose(out=pT2[ti_][:, c * 128:(c + 1) * 128], in_=lat[:, c * 128:(c + 1) * 128], identity=ident[:]), r=[R_lat, R_c], w=[R_pT2[ti_]], inc=(c == 3))
        A(lambda e, b=b, ti_=ti_: e.copy(out=latT2[b][:], in_=pT2v[ti_][:, 0:4, :]), r=[R_pT2[ti_]], w=[R_latT2[b]])

    def stageB(t):
        b = t % 2
        for half in range(2):
            for c in range(4):
                for k in range(4):
                    T(lambda e, c=c, k=k, half=half, b=b: e.matmul(out=pbk[k][:], lhsT=latT2[b][:, c, :], rhs=wkvu[:, c, half * 2048 + k * 512:half * 2048 + (k + 1) * 512], start=(c == 0), stop=(c == 3)),
                      r=[R_latT2[b], R_wkvu], w=[R_pb[k]])
            for k in range(4):
                src = pbk[k][:, :].rearrange("p (h two d) -> p h two d", h=2, two=2)
                h0 = half * 8 + 2 * k
                head_norm(src[:, :, 0, :], 2, 128, 32 + h0, R_pb[k], extra=stA[:, 2 + b:3 + b], R_extra=R_stA)
                A(lambda e, src=src, h0=h0: e.copy(out=vbuf[:, h0:h0 + 2, :], in_=src[:, :, 1, :]), r=[R_pb[k]], w=[R_vbuf])
            rstd(32 + half * 8, 32 + half * 8 + 8, 1.0 / 192)
            for k in range(4):
                src = pbk[k][:, :].rearrange("p (h two d) -> p h two d", h=2, two=2)
                h0 = half * 8 + 2 * k
                scale_heads(kbuf[:, h0:h0 + 2, :], src[:, :, 0, :], 2, 128, 32 + h0, G_KN, R_pb[k], R_kbuf, tmp8[:, 0:256].rearrange("p (h d) -> p h d", h=2))
        transpose_out(kbuf[:, :, :].rearrange("p h d -> p (h d)"), 16,
                      lambda b0, b1, stg, R_stg, t=t: kb.dma("sp", ktn_d[b0:b1, :, t * 128:(t + 1) * 128].rearrange("b p s -> p b s"), stg[:, 0:b1 - b0, :], r=[R_stg], w=[R_ktn]), R_kbuf)
        kb.dma("sp", vm_d[t * 128:(t + 1) * 128, :], vbuf[:].rearrange("p h d -> p (h d)"), r=[R_vbuf], w=[R_vm])
        V(lambda e, b=b: e.tensor_tensor(out=pein[:], in0=kpe2[b][:, :].unsqueeze(1).to_broadcast([128, 16, 64]), in1=rs[:, 32:48].unsqueeze(2).to_broadcast([128, 16, 64]), op=ALU.mult), r=[R_kpe2[b], R_st], w=[R_pein])
        V(lambda e: e.tensor_tensor(out=pein[:], in0=pein[:], in1=gv[:, G_KN + 128:G_KN + 192].unsqueeze(1).to_broadcast([128, 16, 64]), op=ALU.mult), r=[R_pein, R_g], w=[R_pein])
        rope_heads(pein, peout, cos_a[:, t, :], sin_a[:, t, :], 16, R_pein, R_peout, R_rpa)
        transpose_out(peout[:, :, :].rearrange("p h d -> p (h d)"), 8,
                      lambda b0, b1, stg, R_stg, t=t: kb.dma("sp", ktp_d[b0:b1, :, t * 128:(t + 1) * 128].rearrange("b p s -> p b s"), stg[:, 0:b1 - b0, :], r=[R_stg], w=[R_ktp]), R_peout)

    stageA(0)
    for t in range(NT):
        if t + 1 < NT:
            stageA(t + 1)
        stageB(t)

    kb.release(m_base)
    wfk = kb.al([128, 16, 2048], BF16)
    wfv = kb.al([128, 16, 2048], BF16)
    R_wfk, R_wfv = Res(), Res()
    load_w(wfk, io["w_fk"], 0, 2048, R_wfk)
    load_w(wfv, io["w_fv"], 0, 2048, R_wfv)
    hTb = [kb.al([128, 16, 128], BF16) for _ in range(2)]
    R_hTb = [Res(), Res()]
    vbf = [kb.al([128, 2048], BF16) for _ in range(2)]
    R_vbf = [Res(), Res()]
    kb_sq = kb.al([128, 2048], F32)
    R_sq = Res()
    kb_stage = [kb.al([128, 8, 128], BF16) for _ in range(2)]
    R_stage = [Res(), Res()]
    kbuf2 = kb.al([128, 16, 128], BF16)
    R_kbuf2 = Res()
    tmp82 = kb_sq
    kb.dma("sp", hTb[0][:], ht_d[0], r=[R_htd], w=[R_hTb[0]])
    for t in range(NT):
        b = t % 2
        if t + 1 < NT:
            kb.dma("sp", hTb[1 - b][:], ht_d[t + 1], r=[R_htd], w=[R_hTb[1 - b]])
        for c in range(16):
            for k in range(4):
                T(lambda e, c=c, k=k, b=b: e.matmul(out=pbk[k][:], lhsT=hTb[b][:, c, :], rhs=wfk[:, c, k * 512:(k + 1) * 512], start=(c == 0), stop=(c == 15)), r=[R_hTb[b], R_wfk], w=[R_pb[k]])
        for k in range(4):
            src = pbk[k][:, :].rearrange("p (h d) -> p h d", h=4)
            head_norm(src, 4, 128, 16 + 4 * k, R_pb[k])
        rstd(16, 32, 1.0 / 128)
        for k in range(4):
            src = pbk[k][:, :].rearrange("p (h d) -> p h d", h=4)
            scale_heads(kbuf2[:, 4 * k:4 * k + 4, :], src, 4, 128, 16 + 4 * k, G_FK, R_pb[k], R_kbuf2, tmp82[:, 0:512].rearrange("p (h d) -> p h d", h=4))
        vbank = [4, 5, 6, 0]
        for c in range(16):
            for k in range(4):
                kk = vbank[k]
                T(lambda e, c=c, k=k, kk=kk, b=b: e.matmul(out=pbk[kk][:], lhsT=hTb[b][:, c, :], rhs=wfv[:, c, k * 512:(k + 1) * 512], start=(c == 0), stop=(c == 15)), r=[R_hTb[b], R_wfv], w=[R_pb[kk]])
        for k in range(4):
            kk = vbank[k]
            A(lambda e, k=k, kk=kk, b=b: e.copy(out=vbf[b][:, k * 512:(k + 1) * 512], in_=pbk[kk][:]), r=[R_pb[kk]], w=[R_vbf[b]])
        kb.dma("sp", vf_d[t * 128:(t + 1) * 128, :], vbf[b][:], r=[R_vbf[b]], w=[R_vf])
        transpose_out(kbuf2[:, :, :].rearrange("p h d -> p (h d)"), 16,
                      lambda b0, b1, stg, R_stg, t=t: kb.dma("sp", ktf_d[b0:b1, :, t * 128:(t + 1) * 128].rearrange("b p s -> p b s"), stg[:, 0:b1 - b0, :], r=[R_stg], w=[R_ktf]), R_kbuf2)

    kb.release(m_base)
    hTo = kb.al([128, 16, TO], BF16)
    R_hTo = Res()
    sin_o = kb.al([128, NU, 32], F32)
    cos_o = kb.al([128, NU, 32], F32)
    R_rpo = Res("ropeO")
    m_q = kb.mark()
    rope_tables(io["pos_own"], NU, sin_o, cos_o, R_rpo)
    kb.release(m_q)
    xt_q = [kb.al([128, D], F32) for _ in range(2)]
    R_xt_q = [Res(), Res()]
    junk_q = kb.al([128, D], BF16)
    R_junk_q = Res()
    xn_q = kb.al([128, D], BF16)
    R_xn_q = Res()
    hT1 = kb.al([128, 16, 128], BF16)
    R_hT1 = Res()
    for u in range(NU):
        b = u % 2
        kb.dma("sp", xt_q[b][:], xo[u * 128:(u + 1) * 128, :], w=[R_xt_q[b]])
        x_to_hT(xt_q[b], R_xt_q[b], junk_q, R_junk_q, xn_q, R_xn_q, hT1, R_hT1)
        V(lambda e, u=u: e.tensor_copy(out=hTo[:, :, u * 128:(u + 1) * 128], in_=hT1[:]), r=[R_hT1], w=[R_hTo])
    kb.release(m_q)
    wq1 = kb.al([128, 16, 512], BF16)
    wqu = kb.al([128, 4, 3072], BF16)
    wfq = kb.al([128, 16, 2048], BF16)
    R_wq1, R_wqu, R_wfq = Res(), Res(), Res()
    load_w(wq1, io["w_cq"], 0, 512, R_wq1)
    wqu_v = io["w_qup"].rearrange("(c p) n -> p c n", p=128)
    for c in range(4):
        for a in range(0, 3072, 1536):
            kb.dma("pool", wqu[:, c, a:a + 1536], wqu_v[:, c, a:a + 1536], w=[R_wqu])
    load_w(wfq, io["w_fq"], 0, 2048, R_wfq)
    junk_q = kb.al([128, D], BF16)
    R_junk_q = Res()
    kb_sq = kb.al([128, 2048], F32)
    R_sq = Res()
    kb_pet = kb.al([128, 4, 16, 32], F32)
    R_pet = Res()
    kb_stage = [kb.al([128, 8, 128], BF16) for _ in range(2)]
    R_stage = [Res(), Res()]
    lat_q = kb.al([128, 512], BF16)
    R_lat_q = Res()
    latT_q = kb.al([128, 4, 128], BF16)
    R_latT_q = Res()
    kbuf_q = kb.al([128, 16, 128], BF16)
    R_kbuf_q = Res()
    pein_q = kb.al([128, 16, 64], F32)
    R_pein_q = Res()
    peout_q = kb.al([128, 16, 64], BF16)
    R_peout_q = Res()
    tmp8 = kb_sq
    for u in range(NU):
        hTu = hTo[:, :, u * 128:(u + 1) * 128]
        for c in range(16):
            T(lambda e, c=c, hTu=hTu: e.matmul(out=pbk[6][:], lhsT=hTu[:, c, :], rhs=wq1[:, c, :], start=(c == 0), stop=(c == 15)), r=[R_hTo, R_wq1], w=[R_pb[6]])
        A(lambda e: e.activation(out=junk_q[:, 0:512], in_=pbk[6][:], func=AF.Square, accum_out=st[:, 0:1]), r=[R_pb[6]], w=[R_junk_q, R_st])
        rstd(0, 1, 1.0 / 512)
        V(lambda e: e.scalar_tensor_tensor(out=lat_q[:], in0=pbk[6][:], scalar=rs[:, 0:1], in1=gv[:, G_QL:G_QL + 512], op0=ALU.mult, op1=ALU.mult), r=[R_pb[6], R_st, R_g], w=[R_lat_q])
        for c in range(4):
            T(lambda e, c=c: e.transpose(out=pT[:, c * 128:(c + 1) * 128], in_=lat_q[:, c * 128:(c + 1) * 128], identity=ident[:]), r=[R_lat_q, R_c], w=[R_pT])
        A(lambda e: e.copy(out=latT_q[:], in_=pTv[:, 0:4, :]), r=[R_pT], w=[R_latT_q])
        for grp in range(8):
            kk = grp % 6
            for c in range(4):
                T(lambda e, c=c, grp=grp, kk=kk: e.matmul(out=pbk[kk][:, 0:384], lhsT=latT_q[:, c, :], rhs=wqu[:, c, grp * 384:(grp + 1) * 384], start=(c == 0), stop=(c == 3)), r=[R_latT_q, R_wqu], w=[R_pb[kk]])
            src = pbk[kk][:, 0:384].rearrange("p (h d) -> p h d", h=2)
            h0 = 2 * grp
            head_norm(src, 2, 192, 48, R_pb[kk])
            rstd(48, 50, 1.0 / 192)
            scale_heads(kbuf_q[:, h0:h0 + 2, :], src[:, :, 0:128], 2, 128, 48, G_QN, R_pb[kk], R_kbuf_q, tmp8[:, 0:256].rearrange("p (h d) -> p h d", h=2))
            scale_heads(pein_q[:, h0:h0 + 2, :], src[:, :, 128:192], 2, 64, 48, G_QN + 128, R_pb[kk], R_pein_q, tmp8[:, 256:384].rearrange("p (h d) -> p h d", h=2))
        transpose_out(kbuf_q[:, :, :].rearrange("p h d -> p (h d)"), 16,
                      lambda b0, b1, stg, R_stg, u=u: kb.dma("sp", qtn_d[b0:b1, :, u * 128:(u + 1) * 128].rearrange("b p s -> p b s"), stg[:, 0:b1 - b0, :], r=[R_stg], w=[R_qtn]), R_kbuf_q)
        rope_heads(pein_q, peout_q, cos_o[:, u, :], sin_o[:, u, :], 16, R_pein_q, R_peout_q, R_rpo)
        transpose_out(peout_q[:, :, :].rearrange("p h d -> p (h d)"), 8,
                      lambda b0, b1, stg, R_stg, u=u: kb.dma("sp", qtp_d[b0:b1, :, u * 128:(u + 1) * 128].rearrange("b p s -> p b s"), stg[:, 0:b1 - b0, :], r=[R_stg], w=[R_qtp]), R_peout_q)
        for c in range(16):
            for k in range(4):
                T(lambda e, c=c, k=k, hTu=hTu: e.matmul(out=pbk[k][:], lhsT=hTu[:, c, :], rhs=wfq[:, c, k * 512:(k + 1) * 512], start=(c == 0), stop=(c == 15)), r=[R_hTo, R_wfq], w=[R_pb[k]])
        for k in range(4):
            src = pbk[k][:, :].rearrange("p (h d) -> p h d", h=4)
            head_norm(src, 4, 128, 16 + 4 * k, R_pb[k])
        rstd(16, 32, 1.0 / 128)
        for k in range(4):
            src = pbk[k][:, :].rearrange("p (h d) -> p h d", h=4)
            scale_heads(kbuf_q[:, 4 * k:4 * k + 4, :], src, 4, 128, 16 + 4 * k, G_FQ, R_pb[k], R_kbuf_q, tmp8[:, 0:512].rearrange("p (h d) -> p h d", h=4))
        transpose_out(kbuf_q[:, :, :].rearrange("p h d -> p (h d)"), 16,
                      lambda b0, b1, stg, R_stg, u=u: kb.dma("sp", qtf_d[b0:b1, :, u * 128:(u + 1) * 128].rearrange("b p s -> p b s"), stg[:, 0:b1 - b0, :], r=[R_stg], w=[R_qtf]), R_kbuf_q)
    kb.release(m_q)
    wg = kb.al([128, 16, 2048], BF16)
    R_wg = Res()
    bgb = kb.al([128, 2048], F32)
    R_bg = Res()
    gtmp = kb.al([128, 2048], F32)
    R_gtmp = Res()
    gsb = kb.al([128, 2048], BF16)
    R_gsb = Res()
    kb_stage = [kb.al([128, 8, 128], BF16) for _ in range(2)]
    R_stage = [Res(), Res()]
    for gh in range(2):
        load_w(wg, io["w_gate"], gh * 2048, (gh + 1) * 2048, R_wg)
        kb.dma("sp", bgb[:], io["bg_b"][:, gh * 2048:(gh + 1) * 2048], w=[R_bg])
        for u in range(NU):
            hTu = hTo[:, :, u * 128:(u + 1) * 128]
            for c in range(16):
                for k in range(4):
                    T(lambda e, c=c, k=k, hTu=hTu: e.matmul(out=pbk[k][:], lhsT=hTu[:, c, :], rhs=wg[:, c, k * 512:(k + 1) * 512], start=(c == 0), stop=(c == 15)), r=[R_hTo, R_wg], w=[R_pb[k]])
            for k in range(4):
                V(lambda e, k=k: e.tensor_tensor(out=gtmp[:, k * 512:(k + 1) * 512], in0=pbk[k][:], in1=bgb[:, k * 512:(k + 1) * 512], op=ALU.add), r=[R_pb[k], R_bg], w=[R_gtmp])
            A(lambda e: e.activation(out=gsb[:], in_=gtmp[:], func=AF.Sigmoid), r=[R_gtmp], w=[R_gsb])
            transpose_out(gsb, 16,
                          lambda b0, b1, stg, R_stg, u=u, gh=gh: kb.dma("sp", gt_d[gh * 16 + b0:gh * 16 + b1, :, u * 128:(u + 1) * 128].rearrange("b p s -> p b s"), stg[:, 0:b1 - b0, :], r=[R_stg], w=[R_gtd]), R_gsb)

    kb.release(m_base)
    lzf = flog[:, :, :].rearrange("p h j -> p (h j)")
    cendf = cend[:, :, :].rearrange("p h j -> p (h j)")
    onesr = kb.al([128, NT], F32)
    csel = kb.al([128, NU, NT], F32)
    cown = kb.al([128, NH, NU], F32)
    ctmp = kb.al([128, NH, NT], F32)
    R_cs = Res("cums")
    kb.dma("sp", csel[:], io["csel"], w=[R_cs])
    V(lambda e: e.tensor_tensor(out=flog[:], in0=flog[:], in1=gv[:, G_BF:G_BF + 16].unsqueeze(2).to_broadcast([128, NH, NT]), op=ALU.add), r=[R_fl, R_g], w=[R_fl])
    A(lambda e: e.activation(out=flog[:], in_=flog[:], func=AF.Exp, scale=-1.0), r=[R_fl], w=[R_fl])
    A(lambda e: e.activation(out=flog[:], in_=flog[:], func=AF.Ln, bias=1.0), r=[R_fl], w=[R_fl])
    P(lambda e: e.memset(onesr[:], 1.0), w=[R_cs])
    W = NH * NT
    for c0 in range(0, W, 512):
        c1 = min(W, c0 + 512)
        T(lambda e, c0=c0, c1=c1: e.matmul(out=pbk[0][:, 0:c1 - c0], lhsT=trif[:], rhs=lzf[:, c0:c1], start=True, stop=True), r=[R_fl, R_c], w=[R_pb[0]])
        T(lambda e, c0=c0, c1=c1: e.matmul(out=pbk[1][:, 0:c1 - c0], lhsT=onesf[:], rhs=lzf[:, c0:c1], start=True, stop=True), r=[R_fl, R_c], w=[R_pb[1]])
        V(lambda e, c0=c0, c1=c1: e.tensor_copy(out=cendf[:, c0:c1], in_=pbk[1][:, 0:c1 - c0]), r=[R_pb[1]], w=[R_cs])
        V(lambda e, c0=c0, c1=c1: e.tensor_copy(out=ctmp[:, :, :].rearrange("p h j -> p (h j)")[:, c0:c1], in_=pbk[0][:, 0:c1 - c0]), r=[R_pb[0]], w=[R_cs])
    V(lambda e: e.tensor_copy(out=flog[:], in_=cend[:]), r=[R_cs, R_fl], w=[R_fl])
    for h in range(NH):
        V(lambda e, h=h: e.tensor_tensor_scan(out=cend[:, h, :], data0=onesr[:], data1=flog[:, h, :], initial=0.0, op0=ALU.mult, op1=ALU.add), r=[R_fl, R_cs], w=[R_cs])
    V(lambda e: e.tensor_tensor(out=flog[:], in0=cend[:], in1=flog[:], op=ALU.subtract), r=[R_cs, R_fl], w=[R_fl])
    V(lambda e: e.tensor_tensor(out=flog[:], in0=flog[:], in1=ctmp[:], op=ALU.add), r=[R_cs, R_fl], w=[R_fl])
    for u in range(NU):
        V(lambda e, u=u: e.tensor_tensor(out=ctmp[:], in0=cend[:], in1=csel[:, u, :].unsqueeze(1).to_broadcast([128, NH, NT]), op=ALU.mult), r=[R_cs], w=[R_cs])
        V(lambda e, u=u: e.tensor_reduce(out=cown[:, :, u], in_=ctmp[:], axis=AX.X, op=ALU.add), r=[R_cs], w=[R_cs])

    kposf = kb.al([128, NT], F32)
    kposi = kb.al([128, NT], I32)
    qposb = kb.al([128, TO], F32)
    R_pos = Res()
    P(lambda e: e.iota(kposi[:], pattern=[[128, NT]], base=0, channel_multiplier=1), w=[R_pos])
    V(lambda e: e.tensor_copy(out=kposf[:], in_=kposi[:]), r=[R_pos], w=[R_pos])
    kb.dma("sp", qposb[:], io["qpos_b"], w=[R_pos])
    cmask = kb.al([128, NT, 128], BF16)
    R_cm = Res("cmask")
    for b8 in range(NU):
        V(lambda e, b8=b8: e.tensor_tensor(out=cmask[:, b8 * NC:(b8 + 1) * NC, :], in0=qposb[:, b8 * 128:(b8 + 1) * 128].unsqueeze(1).to_broadcast([128, NC, 128]),
                                          in1=kposf[:, b8 * NC:(b8 + 1) * NC].unsqueeze(2).to_broadcast([128, NC, 128]), op=ALU.is_ge), r=[R_pos], w=[R_cm])
    knt = [kb.al([128, S], BF16) for _ in range(2)]
    kpp = [kb.al([128, S], BF16) for _ in range(2)]
    vth = [kb.al([128, NT, 128], BF16) for _ in range(2)]
    R_k, R_kp, R_v = [Res(), Res()], [Res(), Res()], [Res(), Res()]
    qn_t = [kb.al([128, TO], BF16) for _ in range(2)]
    qpp = [kb.al([128, TO], BF16) for _ in range(2)]
    g_t = [kb.al([128, TO], BF16) for _ in range(2)]
    R_q, R_qp, R_gt = [Res(), Res()], [Res(), Res()], [Res(), Res()]
    bt = [kb.al([128, NT, NU], F32) for _ in range(2)]
    R_bt = [Res(), Res()]
    CW = min(512, TO)
    NPASS = TO // CW
    ND = 4
    sadd = [kb.al([128, CW], F32) for _ in range(ND)]
    R_sadd = [Res() for _ in range(ND)]
    pt_t = [kb.al([128, CW], BF16) for _ in range(ND)]
    R_pt = [Res() for _ in range(ND)]
    rl = kb.al([128, CW], F32)
    yt = kb.al([128, CW], F32)
    R_y = Res()
    yb = [kb.al([128, CW], BF16) for _ in range(2)]
    R_yb = [Res(), Res()]
    mt_d = io["mt_d"]
    R_mtd = Res("mt_d")
    NHH = 2 * NH
    OB, LB = 4, 5

    def load_head(hh):
        hb = hh % 2
        mla = hh < NH
        h = hh % NH
        if mla:
            kb.dma("sp", knt[hb][:], ktn_d[h], r=[R_ktn], w=[R_k[hb]])
            if h % 2 == 0:
                pb_ = (h // 2) % 2
                kb.dma("sp", kpp[pb_][:], ktp_d[h // 2], r=[R_ktp], w=[R_kp[pb_]])
                kb.dma("sp", qpp[pb_][:], qtp_d[h // 2], r=[R_qtp], w=[R_qp[pb_]])
            vsrc = vm_d[:, h * 128:(h + 1) * 128].rearrange("(j p) d -> p j d", p=128)
            kb.dma("sp", qn_t[hb][:], qtn_d[h], r=[R_qtn], w=[R_q[hb]])
        else:
            kb.dma("sp", knt[hb][:], ktf_d[h], r=[R_ktf], w=[R_k[hb]])
            vsrc = vf_d[:, h * 128:(h + 1) * 128].rearrange("(j p) d -> p j d", p=128)
            kb.dma("sp", qn_t[hb][:], qtf_d[h], r=[R_qtf], w=[R_q[hb]])
            V(lambda e, h=h, hb=hb: e.tensor_tensor(out=bt[hb][:], in0=flog[:, h, :].unsqueeze(2).to_broadcast([128, NT, NU]),
                                                  in1=cown[:, h, :].unsqueeze(1).to_broadcast([128, NT, NU]), op=ALU.subtract), r=[R_fl, R_cs], w=[R_bt[hb]])
            V(lambda e, hb=hb: e.tensor_scalar(out=bt[hb][:], in0=bt[hb][:], scalar1=0.0, scalar2=None, op0=ALU.min), r=[R_bt[hb]], w=[R_bt[hb]])
        for j0 in range(0, NT, 4):
            j1 = min(NT, j0 + 4)
            kb.dma("sp", vth[hb][:, j0:j1, :], vsrc[:, j0:j1, :], r=[R_vm, R_vf], w=[R_v[hb]])
        kb.dma("sp", g_t[hb][:], gt_d[hh], r=[R_gtd], w=[R_gt[hb]])

    def geom(ps, j):
        a0 = ps * CW
        c0 = max(a0, (j // NC) * 128)
        return a0, c0, a0 + CW

    def emit_S(idx, hh, ps, j):
        hb, sb_ = hh % 2, idx % ND
        mla = hh < NH
        h = hh % NH
        a0, c0, a1 = geom(ps, j)
        pss, Rs = pbk[sb_], R_pb[sb_]
        T(lambda e: e.matmul(out=pss[:, c0 - a0:a1 - a0], lhsT=knt[hb][:, j * 128:(j + 1) * 128], rhs=qn_t[hb][:, c0:a1], start=True, stop=(not mla)),
          r=[R_k[hb], R_q[hb]], w=[Rs], inc=(not mla))
        if mla:
            pb_ = (h // 2) % 2
            r0 = (h % 2) * 64
            T(lambda e: e.matmul(out=pss[:, c0 - a0:a1 - a0], lhsT=kpp[pb_][r0:r0 + 64, j * 128:(j + 1) * 128], rhs=qpp[pb_][r0:r0 + 64, c0:a1], start=False, stop=True),
              r=[R_kp[pb_], R_qp[pb_]], w=[Rs])

    def emit_rest(idx, hh, ps, j, first, last):
        hb, sb_ = hh % 2, idx % ND
        mla = hh < NH
        a0, c0, a1 = geom(ps, j)
        pss, Rs = pbk[sb_], R_pb[sb_]
        ptile, Rp = pt_t[sb_], R_pt[sb_]
        lo, hi = c0 - a0, a1 - a0
        if mla:
            A(lambda e: e.activation(out=ptile[:, lo:hi], in_=pss[:, lo:hi], func=AF.Exp), r=[Rs], w=[Rp])
        else:
            nu = (hi - lo) // 128
            V(lambda e: e.tensor_tensor(out=sadd[sb_][:, lo:hi].rearrange("p (u q) -> p u q", u=nu), in0=pss[:, lo:hi].rearrange("p (u q) -> p u q", u=nu),
                                        in1=bt[hb][:, j, c0 // 128:a1 // 128].unsqueeze(2).to_broadcast([128, nu, 128]), op=ALU.add), r=[Rs, R_bt[hb]], w=[R_sadd[sb_]])
            A(lambda e: e.activation(out=ptile[:, lo:hi], in_=sadd[sb_][:, lo:hi], func=AF.Exp), r=[R_sadd[sb_]], w=[Rp])
        if c0 == (j // NC) * 128:
            P(lambda e: e.tensor_tensor(out=ptile[:, lo:lo + 128], in0=ptile[:, lo:lo + 128], in1=cmask[:, j, :], op=ALU.mult), r=[Rp, R_cm], w=[Rp])
        T(lambda e: e.matmul(out=pbk[OB][:, lo:hi], lhsT=vth[hb][:, j, :], rhs=ptile[:, lo:hi], start=first, stop=last), r=[R_v[hb], Rp], w=[R_pb[OB]], inc=False)
        T(lambda e: e.matmul(out=pbk[LB][:, lo:hi], lhsT=onesb[:], rhs=ptile[:, lo:hi], start=first, stop=last), r=[R_c, Rp], w=[R_pb[LB], R_pb[OB]])
        if last:
            V(lambda e: e.reciprocal(out=rl[:], in_=pbk[LB][:, 0:CW]), r=[R_pb[LB]], w=[R_y])
            V(lambda e: e.tensor_tensor(out=yt[:], in0=pbk[OB][:, 0:CW], in1=rl[:], op=ALU.mult), r=[R_pb[OB], R_y], w=[R_y])
            yi = (hh * NPASS + ps) % 2
            P(lambda e: e.tensor_tensor(out=yb[yi][:], in0=yt[:], in1=g_t[hb][:, a0:a1], op=ALU.mult), r=[R_y, R_gt[hb]], w=[R_yb[yi]])
            kb.dma("sp", mt_d[hh][:, a0:a1], yb[yi][:], r=[R_yb[yi]], w=[R_mtd])

    steps = []
    for hh in range(NHH):
        for ps in range(NPASS):
            nj = min(NT, ((ps + 1) * CW // 128) * NC)
            for j in range(nj):
                steps.append((hh, ps, j, j == 0, j == nj - 1))
    load_head(0)
    LOOK = ND - 1
    for i in range(min(LOOK, len(steps))):
        emit_S(i, *steps[i][:3])
    for idx, (hh, ps, j, first, last) in enumerate(steps):
        if first and ps == 0 and hh + 1 < NHH:
            load_head(hh + 1)
        if idx + LOOK < len(steps):
            emit_S(idx + LOOK, *steps[idx + LOOK][:3])
        emit_rest(idx, hh, ps, j, first, last)
    kb.release(m_base)
    return {"identf": identf, "ident": ident, "nhalf": nhalf, "iota_f": iota_f, "gT2": gT2, "R_c": R_c, "R_g": R_gB, "R_mtd": R_mtd}


F_IN = lambda S: {
    "x": ([S, D], F32), "x_own": ([S // 8, D], F32), "w_kv1": ([D, 592], F32), "w_kvup": ([512, 4096], F32), "w_fk": ([D, 2048], F32), "w_fv": ([D, 2048], F32),
    "w_cq": ([D, 512], F32), "w_qup": ([512, 3072], F32), "w_fq": ([D, 2048], F32), "w_gate": ([D, 4096], F32), "gmix": ([D], F32), "gvf": ([128, G_NV], F32),
    "pos_all": ([128, S // 128], I32), "pos_own": ([128, S // 1024], I32), "csel": ([128, S // 1024, S // 128], F32), "qpos_b": ([128, S // 8], F32), "bg_b": ([128, 4096], F32),
    "w_out": ([4096, D], F32), "gffn": ([D], F32), "gffn_b": ([128, D], F32), "w_pq": ([D, D], F32), "skeys": ([16, 128, 128], F32),
    "pu": ([16384, D], F32), "pv": ([16384, D], F32)}
F_SCR = lambda S: {
    "ktn_d": ([16, 128, S], BF16), "ktp_d": ([8, 128, S], BF16), "ktf_d": ([16, 128, S], BF16), "vm_d": ([S, 2048], BF16), "vf_d": ([S, 2048], BF16),
    "ht_d": ([S // 128, 128, 16, 128], BF16), "qtn_d": ([16, 128, S // 8], BF16), "qtp_d": ([8, 128, S // 8], BF16), "qtf_d": ([16, 128, S // 8], BF16),
    "gt_d": ([32, 128, S // 8], BF16), "mt_d": ([32, 128, S // 8], BF16), "x2d": ([S // 8, D], F32), "h2d": ([S // 8, D], BF16), "pout": ([S // 8, D], F32)}


def own_tiles(core, S):
    return [core + 8 * u for u in range(S // 1024)]


def prep_F(inp, core, S):
    f = np.float32
    NT, NU = S // 128, S // 1024
    w_in = inp["w_in"][0]
    sp = np.cumsum([0, 512, 512, 64, 2048, 2048, 2048, 16, 2048, 2048])
    tiles = own_tiles(core, S)
    rows = np.concatenate([np.arange(t * 128, (t + 1) * 128) for t in tiles])
    x = np.ascontiguousarray(inp["x"][0, :S])
    gvec = np.zeros(G_NV, f)
    gvec[G_KL:G_KL + 512] = inp["mla_kv_latent_g"][0]
    gvec[G_QL:G_QL + 512] = inp["mla_q_latent_g"][0]
    gvec[G_QN:G_QN + 192] = inp["mla_q_norm_g"][0]
    gvec[G_KN:G_KN + 192] = inp["mla_k_norm_g"][0]
    gvec[G_FQ:G_FQ + 128] = inp["fox_q_norm_g"][0]
    gvec[G_FK:G_FK + 128] = inp["fox_k_norm_g"][0]
    gvec[G_BF:G_BF + 16] = inp["b_forget"][0]
    gvec[G_IF:G_IF + 32] = (10000.0 ** (-np.arange(32, dtype=np.float32) / 32)).astype(f)
    pos = inp["positions"][0, :S].astype(np.int32)
    csel = np.zeros((NU, NT), f)
    for u, t in enumerate(tiles):
        csel[u, t] = 1.0
    bc = lambda a: np.ascontiguousarray(np.broadcast_to(a, (128,) + a.shape))
    return {
        "x": x, "x_own": np.ascontiguousarray(x[rows]),
        "w_kv1": np.ascontiguousarray(np.concatenate([w_in[:, sp[1]:sp[2]], w_in[:, sp[2]:sp[3]], w_in[:, sp[6]:sp[7]]], axis=1)),
        "w_kvup": np.ascontiguousarray(inp["w_kv_up"][0]), "w_fk": np.ascontiguousarray(w_in[:, sp[4]:sp[5]]), "w_fv": np.ascontiguousarray(w_in[:, sp[5]:sp[6]]),
        "w_cq": np.ascontiguousarray(w_in[:, sp[0]:sp[1]]), "w_qup": np.ascontiguousarray(inp["w_q_up"][0]), "w_fq": np.ascontiguousarray(w_in[:, sp[3]:sp[4]]),
        "w_gate": np.ascontiguousarray(w_in[:, sp[7]:sp[9]]), "gmix": np.ascontiguousarray(inp["mix_norm_g"][0]), "gvf": bc(gvec),
        "pos_all": np.ascontiguousarray(pos.reshape(NT, 128).T), "pos_own": np.ascontiguousarray(pos[rows].reshape(NU, 128).T),
        "csel": bc(csel), "qpos_b": bc(rows.astype(f)), "bg_b": bc(inp["b_gate"][0].astype(f)),
        "w_out": np.ascontiguousarray(inp["w_out"][0]), "gffn": np.ascontiguousarray(inp["ffn_norm_g"][0]), "gffn_b": bc(inp["ffn_norm_g"][0].astype(f)),
        "w_pq": np.ascontiguousarray(inp["w_peer_q"][0]), "skeys": np.ascontiguousarray(inp["peer_sub_keys"][0].reshape(16, 128, 128)),
        "pu": np.ascontiguousarray(inp["peer_u"][0]), "pv": np.ascontiguousarray(inp["peer_v"][0]),
    }


def build_B(kb, Tn, io, R_mtin=None, shared=None):
    NT = Tn // 128
    mtin, x_own, w_out, gffn, gffn_b, w_pq, skeys = io.get("mtin"), io["x_own"], io["w_out"], io["gffn"], io["gffn_b"], io["w_pq"], io["skeys"]
    pu, pv, out, x2d, h2d, pout = io["pu"], io["pv"], io["out"], io["x2d"], io["h2d"], io["pout"]
    if R_mtin is None:
        R_mtin = Res("mtin")
    R_x2d, R_h2d, R_pout = Res("x2d"), Res("h2d"), Res("pout")
    V = lambda fn, r=(), w=(): kb.op("dve", fn, r, w)
    A = lambda fn, r=(), w=(): kb.op("act", fn, r, w)
    P = lambda fn, r=(), w=(): kb.op("pool", fn, r, w)
    T = lambda fn, r=(), w=(), inc=True: kb.op("pe", fn, r, w, pe_acc=True, inc=inc)
    pbk = kb.pbank
    R_pb = [Res("pbB%d" % i) for i in range(8)]

    if shared is None:
        identf = kb.al([128, 128], F32)
        ident = kb.al([128, 128], BF16)
        nhalf = kb.al([128, 8], F32)
        iota_i = kb.al([128, 16], I32)
        iota_f = kb.al([128, 16], F32)
        R_c = Res("constsB")
        P(lambda e: e.memset(identf[:], 0.0), w=[R_c])
        P(lambda e: e.affine_select(out=identf[:], in_=identf[:], pattern=[[-1, 128]], compare_op=ALU.not_equal,
                                    fill=1.0, base=0, channel_multiplier=1), r=[R_c], w=[R_c])
        P(lambda e: e.memset(nhalf[:], -0.5), w=[R_c])
        P(lambda e: e.iota(iota_i[:], pattern=[[1, 16]], base=0, channel_multiplier=0), w=[R_c])
        V(lambda e: e.tensor_copy(out=ident[:], in_=identf[:]), r=[R_c], w=[R_c])
        V(lambda e: e.tensor_copy(out=iota_f[:], in_=iota_i[:]), r=[R_c], w=[R_c])
        gT2 = kb.al([128, 16], F32)
        R_g = Res("gB")
        kb.dma("sp", gT2[:], gffn.rearrange("(c p) -> p c", p=128), w=[R_g], allow_slow_non_contiguous=True)
    else:
        identf, ident, nhalf, iota_f, gT2, R_c, R_g = (shared[k] for k in ("identf", "ident", "nhalf", "iota_f", "gT2", "R_c", "R_g"))

    m0 = kb.mark()
    ar2 = kb.al([128, 32 * Tn], BF16)
    R_ar2 = Res("ar2")
    mT = ar2[:, 0:32 * Tn].rearrange("p (c t) -> p c t", c=32)
    for c in range(8):
        kb.dma("sp", mT[:, c * 4:(c + 1) * 4, :], mtin[c].rearrange("h d t -> d h t"), r=[R_mtin], w=[R_ar2])
    ar3 = kb.al([128, 16384], BF16)
    R_ar3 = Res("ar3")
    wo = ar3[:, :].rearrange("p (c n) -> p c n", c=32)
    w_out_v = w_out.rearrange("(c p) n -> p c n", p=128)
    xs = [kb.al([128, 512], F32) for _ in range(2)]
    R_xs = [Res("xs0"), Res("xs1")]
    n = 0
    for ds in range(4):
        for g in range(8):
            kb.dma("pool", wo[:, 4 * g:4 * g + 4, :], w_out_v[:, 4 * g:4 * g + 4, ds * 512:(ds + 1) * 512], w=[R_ar3])
        for tt in range(NT):
            b = n % 2
            n += 1
            kb.dma("sp", xs[b][:], x_own[tt * 128:(tt + 1) * 128, ds * 512:(ds + 1) * 512], w=[R_xs[b]])
            pk = pbk[b]
            for c in range(32):
                T(lambda e, pk=pk, c=c, tt=tt: e.matmul(out=pk[:], lhsT=mT[:, c, tt * 128:(tt + 1) * 128], rhs=wo[:, c, :], start=(c == 0), stop=(c == 31)),
                  r=[R_ar2, R_ar3], w=[R_pb[b]], inc=(c == 31))
            V(lambda e, b=b, pk=pk: e.tensor_tensor(out=xs[b][:], in0=xs[b][:], in1=pk[:], op=ALU.add), r=[R_xs[b], R_pb[b]], w=[R_xs[b]])
            kb.dma("sp", x2d[tt * 128:(tt + 1) * 128, ds * 512:(ds + 1) * 512], xs[b][:], r=[R_xs[b]], w=[R_x2d])

    kb.release(m0)
    h2T = kb.al([128, 16, Tn], BF16)
    R_h2T = Res("h2T")
    i1T = kb.al([128, Tn], BF16)
    i2T = kb.al([128, Tn], F32)
    gtT = kb.al([128, Tn], F32)
    R_eiT = Res("eiT")
    m1 = kb.mark()
    gb = kb.al([128, D], F32)
    kb.dma("sp", gb[:], gffn_b, w=[R_g])
    xt = [kb.al([128, D], F32) for _ in range(2)]
    R_xt = [Res("xtB0"), Res("xtB1")]
    junk = kb.al([128, D], BF16)
    R_junk = Res("junkB")
    xn = kb.al([128, D], BF16)
    R_xn = Res("xnB")
    hrow = kb.al([128, D], BF16)
    R_hrow = Res("hrow")
    st = kb.al([128, 8], F32)
    rs = kb.al([128, 8], F32)
    R_st = Res("stB")
    pT = pbk[7][:, :].bitcast(BF16)
    pTv = pT.rearrange("p (c t) -> p c t", c=8)
    R_pT = R_pb[7]
    for tt in range(NT):
        b = tt % 2
        kb.dma("sp", xt[b][:], x2d[tt * 128:(tt + 1) * 128, :], r=[R_x2d], w=[R_xt[b]])
        A(lambda e, b=b: e.activation(out=junk[:], in_=xt[b][:], func=AF.Square, accum_out=st[:, 0:1]), r=[R_xt[b]], w=[R_junk, R_st])
        V(lambda e: e.tensor_scalar(out=rs[:, 0:1], in0=st[:, 0:1], scalar1=1.0 / D, scalar2=EPS, op0=ALU.mult, op1=ALU.add), r=[R_st], w=[R_st])
        P(lambda e: e.tensor_tensor(out=rs[:, 0:1], in0=rs[:, 0:1], in1=nhalf[:, 0:1], op=ALU.pow), r=[R_st, R_c], w=[R_st])
        V(lambda e, b=b: e.tensor_scalar(out=xn[:], in0=xt[b][:], scalar1=rs[:, 0:1], scalar2=None, op0=ALU.mult), r=[R_xt[b], R_st], w=[R_xn])
        for half in range(2):
            for c in range(8):
                cc = half * 8 + c
                T(lambda e, c=c, cc=cc: e.transpose(out=pT[:, c * 128:(c + 1) * 128], in_=xn[:, cc * 128:(cc + 1) * 128], identity=ident[:]), r=[R_xn, R_c], w=[R_pT])
            V(lambda e, half=half, tt=tt: e.tensor_tensor(out=h2T[:, half * 8:half * 8 + 8, tt * 128:(tt + 1) * 128], in0=pTv,
                                                          in1=gT2[:, half * 8:half * 8 + 8].unsqueeze(2).to_broadcast([128, 8, 128]), op=ALU.mult),
              r=[R_pT, R_g], w=[R_h2T])
        V(lambda e: e.tensor_tensor(out=hrow[:], in0=xn[:], in1=gb[:], op=ALU.mult), r=[R_xn, R_g], w=[R_hrow])
        kb.dma("sp", h2d[tt * 128:(tt + 1) * 128, :], hrow[:], r=[R_hrow], w=[R_h2d])

    kb.release(m1)
    ar3 = kb.al([128, 16 * Tn], BF16)
    skT = kb.al([128, 16, 128], BF16)
    R_ar2, R_ar3 = Res("ar2b"), Res("ar3b")
    m2 = kb.mark()
    ar2 = kb.al([128, 32768], BF16)
    R_junk = Res("junkB2")
    wpq = ar2[:, :].rearrange("p (c n) -> p c n", c=16)
    w_pq_v = w_pq.rearrange("(c p) n -> p c n", p=128)
    for c in range(16):
        kb.dma("pool", wpq[:, c, :], w_pq_v[:, c, :], w=[R_ar2])
    qT = ar3[:, 0:16 * Tn].rearrange("p (c t) -> p c t", c=16)
    skb = kb.al([128, 16, 128], BF16)
    R_sk = Res("sk")
    kb.dma("pool", skb[:], skeys.rearrange("h k d -> k h d"), w=[R_sk])
    for half in range(2):
        for c in range(8):
            T(lambda e, c=c, half=half: e.transpose(out=pT[:, c * 128:(c + 1) * 128], in_=skb[:, half * 8 + c, :], identity=ident[:]), r=[R_sk, R_c], w=[R_pT])
        V(lambda e, half=half: e.tensor_copy(out=skT[:, half * 8:half * 8 + 8, :], in_=pTv), r=[R_pT], w=[R_sk])
    W = min(512, Tn)
    n = 0
    for hc in range(16):
        for c0 in range(0, Tn, W):
            b = n % 2
            n += 1
            pk = pbk[b]
            for c in range(16):
                T(lambda e, pk=pk, c=c, hc=hc, c0=c0: e.matmul(out=pk[:, 0:W], lhsT=wpq[:, c, hc * 128:(hc + 1) * 128], rhs=h2T[:, c, c0:c0 + W], start=(c == 0), stop=(c == 15)),
                  r=[R_ar2, R_h2T], w=[R_pb[b]], inc=(c == 15))
            A(lambda e, pk=pk, hc=hc, c0=c0: e.copy(out=qT[:, hc, c0:c0 + W], in_=pk[:, 0:W]), r=[R_pb[b]], w=[R_ar3])

    kb.release(m2)
    m3 = kb.mark()
    sc = kb.al([128, 16, 128], F32)
    R_sc = Res("sc")
    wk = kb.al([128, 256], F32)
    R_wk = Res("wk")
    tv = kb.al([128, 16, 16], F32)
    ti = kb.al([128, 16, 16], U32)
    tif = kb.al([128, 16, 16], F32)
    R_tv = Res("tv")
    cand = kb.al([128, 8, 16, 16], F32)
    R_cand = Res("cand")
    cv = kb.al([128, 8, 16], F32)
    cp = kb.al([128, 8, 16], U32)
    ci = kb.al([128, 8, 16], I32)
    ikf = kb.al([128, 8, 16], F32)
    jkf = kb.al([128, 8, 16], F32)
    R_cv = Res("cv")
    eq = kb.al([128, 8, 16, 16], F32)
    R_eq = Res("eq")
    i1s = kb.al([128, 8, 16], F32)
    i2s = kb.al([128, 8, 16], F32)
    ef = kb.al([128, 128], F32)
    gt = kb.al([128, 8, 16], F32)
    zs = kb.al([128, 8], F32)
    R_sel = Res("sel")
    tvv = tv[:, :, :].rearrange("p (h c) k -> p h c k", c=2)
    tifv = tif[:, :, :].rearrange("p (h c) k -> p h c k", c=2)
    pTf = pbk[6]
    for tt in range(NT):
        for hc in range(16):
            T(lambda e, hc=hc, tt=tt: e.matmul(out=pbk[hc // 4][:, (hc % 4) * 128:(hc % 4 + 1) * 128], lhsT=qT[:, hc, tt * 128:(tt + 1) * 128], rhs=skT[:, hc, :], start=True, stop=True),
              r=[R_ar3, R_sk], w=[R_pb[hc // 4]])
        for k in range(4):
            A(lambda e, k=k: e.copy(out=sc[:, 4 * k:4 * k + 4, :], in_=pbk[k][:, :].rearrange("p (a b) -> p a b", a=4)), r=[R_pb[k]], w=[R_sc])
        for hc in range(16):
            V(lambda e, hc=hc: e.max(out=tv[:, hc, 0:8], in_=sc[:, hc, :]), r=[R_sc], w=[R_tv])
            V(lambda e, hc=hc: e.max_index(out=ti[:, hc, 0:8], in_max=tv[:, hc, 0:8], in_values=sc[:, hc, :]), r=[R_sc, R_tv], w=[R_tv])
            V(lambda e, hc=hc: e.match_replace(out=wk[:, 0:128], in_to_replace=tv[:, hc, 0:8], in_values=sc[:, hc, :], imm_value=-1e30), r=[R_sc, R_tv], w=[R_wk])
            V(lambda e, hc=hc: e.max(out=tv[:, hc, 8:16], in_=wk[:, 0:128]), r=[R_wk], w=[R_tv])
            V(lambda e, hc=hc: e.max_index(out=ti[:, hc, 8:16], in_max=tv[:, hc, 8:16], in_values=wk[:, 0:128]), r=[R_wk, R_tv], w=[R_tv])
        V(lambda e: e.tensor_copy(out=tif[:], in_=ti[:]), r=[R_tv], w=[R_tv])
        V(lambda e: e.tensor_tensor(out=cand[:], in0=tvv[:, :, 0, :].unsqueeze(3).to_broadcast([128, 8, 16, 16]),
                                    in1=tvv[:, :, 1, :].unsqueeze(2).to_broadcast([128, 8, 16, 16]), op=ALU.add), r=[R_tv], w=[R_cand])
        for h in range(8):
            cf = cand[:, h, :, :].rearrange("p a b -> p (a b)")
            V(lambda e, h=h, cf=cf: e.max(out=cv[:, h, 0:8], in_=cf), r=[R_cand], w=[R_cv])
            V(lambda e, h=h, cf=cf: e.max_index(out=cp[:, h, 0:8], in_max=cv[:, h, 0:8], in_values=cf), r=[R_cand, R_cv], w=[R_cv])
            V(lambda e, h=h, cf=cf: e.match_replace(out=wk[:], in_to_replace=cv[:, h, 0:8], in_values=cf, imm_value=-1e30), r=[R_cand, R_cv], w=[R_wk])
            V(lambda e, h=h: e.max(out=cv[:, h, 8:16], in_=wk[:]), r=[R_wk], w=[R_cv])
            V(lambda e, h=h: e.max_index(out=cp[:, h, 8:16], in_max=cv[:, h, 8:16], in_values=wk[:]), r=[R_wk, R_cv], w=[R_cv])
        V(lambda e: e.tensor_tensor(out=gt[:], in0=cv[:], in1=cv[:, :, 0:1].to_broadcast([128, 8, 16]), op=ALU.subtract), r=[R_cv], w=[R_sel])
        A(lambda e: e.activation(out=gt[:], in_=gt[:], func=AF.Exp), r=[R_sel], w=[R_sel])
        V(lambda e: e.tensor_reduce(out=zs[:], in_=gt[:], axis=AX.X, op=ALU.add), r=[R_sel], w=[R_sel])
        V(lambda e: e.reciprocal(out=zs[:], in_=zs[:]), r=[R_sel], w=[R_sel])
        V(lambda e: e.tensor_tensor(out=gt[:], in0=gt[:], in1=zs[:, :].unsqueeze(2).to_broadcast([128, 8, 16]), op=ALU.mult), r=[R_sel], w=[R_sel])
        cpi = cp[:, :, :].bitcast(I32)
        V(lambda e, cpi=cpi: e.tensor_single_scalar(out=ci[:], in_=cpi, scalar=4, op=ALU.arith_shift_right), r=[R_cv], w=[R_cv])
        V(lambda e: e.tensor_copy(out=ikf[:], in_=ci[:]), r=[R_cv], w=[R_cv])
        V(lambda e, cpi=cpi: e.tensor_single_scalar(out=ci[:], in_=cpi, scalar=15, op=ALU.bitwise_and), r=[R_cv], w=[R_cv])
        V(lambda e: e.tensor_copy(out=jkf[:], in_=ci[:]), r=[R_cv], w=[R_cv])
        io16 = iota_f[:, :].unsqueeze(1).unsqueeze(1).to_broadcast([128, 8, 16, 16])
        for sel, kf, c in ((i1s, ikf, 0), (i2s, jkf, 1)):
            V(lambda e, kf=kf: e.tensor_tensor(out=eq[:], in0=io16, in1=kf[:, :, :].unsqueeze(3).to_broadcast([128, 8, 16, 16]), op=ALU.is_equal), r=[R_cv, R_c], w=[R_eq])
            V(lambda e, c=c: e.tensor_tensor(out=eq[:], in0=eq[:], in1=tifv[:, :, c, :].unsqueeze(2).to_broadcast([128, 8, 16, 16]), op=ALU.mult), r=[R_eq, R_tv], w=[R_eq])
            V(lambda e, sel=sel: e.tensor_reduce(out=sel[:], in_=eq[:], axis=AX.X, op=ALU.add), r=[R_eq], w=[R_sel])
        T(lambda e: e.transpose(out=pTf[:, 0:128], in_=i1s[:].rearrange("p h k -> p (h k)"), identity=identf[:]), r=[R_sel, R_c], w=[R_pb[6]])
        T(lambda e: e.transpose(out=pTf[:, 128:256], in_=i2s[:].rearrange("p h k -> p (h k)"), identity=identf[:]), r=[R_sel, R_c], w=[R_pb[6]])
        T(lambda e: e.transpose(out=pTf[:, 256:384], in_=gt[:].rearrange("p h k -> p (h k)"), identity=identf[:]), r=[R_sel, R_c], w=[R_pb[6]])
        V(lambda e, tt=tt: e.tensor_copy(out=i1T[:, tt * 128:(tt + 1) * 128], in_=pTf[:, 0:128]), r=[R_pb[6]], w=[R_eiT])
        V(lambda e, tt=tt: e.tensor_copy(out=i2T[:, tt * 128:(tt + 1) * 128], in_=pTf[:, 128:256]), r=[R_pb[6]], w=[R_eiT])
        V(lambda e, tt=tt: e.tensor_copy(out=gtT[:, tt * 128:(tt + 1) * 128], in_=pTf[:, 256:384]), r=[R_pb[6]], w=[R_eiT])

    import os
    _stop = int(os.environ.get("DENSE_STOP", "9"))
    if _stop == 0:
        return
    kb.release(m1)
    TH = min(512, Tn)
    NTH = TH // 128
    iob = kb.al([128, 128], BF16)
    ioi = kb.al([128, 128], I32)
    ioc = kb.al([128, 16], F32)
    R_io = Res("iota")
    P(lambda e: e.iota(ioi[:], pattern=[[1, 128]], base=0, channel_multiplier=0), w=[R_io])
    V(lambda e: e.tensor_copy(out=iob[:], in_=ioi[:]), r=[R_io], w=[R_io])
    V(lambda e: e.tensor_copy(out=ioc[:], in_=ioi[:, 0:16]), r=[R_io], w=[R_io])
    oacc = kb.al([128, NTH, D], F32)
    R_oacc = Res("oacc")
    Gc = [kb.al([128, 16, TH], BF16) for _ in range(2)]
    R_Gc = [Res("Gc0"), Res("Gc1")]
    i2c = kb.al([128, TH], F32)
    R_i2c = Res("i2c")
    ablk = [kb.al([128, 32, 128], BF16) for _ in range(2)]
    R_ablk = [Res(), Res()]
    btmp = kb.al([128, 64, 16], F32)
    bblk = kb.al([128, 64, 16], BF16)
    R_bblk = Res("bblk")
    ub = [kb.al([128, D], BF16) for _ in range(2)]
    R_ub = [Res(), Res()]
    uT = [kb.al([128, 16, 128], BF16) for _ in range(2)]
    R_uT = [Res(), Res()]
    vb = [[kb.al([128, D], BF16) for _ in range(4)] for _ in range(2)]
    R_vb = [[Res() for _ in range(4)] for _ in range(2)]
    wg = [[kb.al([128, TH], BF16) for _ in range(4)] for _ in range(2)]
    R_wg = [[Res() for _ in range(4)] for _ in range(2)]
    ga = [kb.al([128, TH], BF16) for _ in range(2)]
    R_ga = [Res(), Res()]
    pu_v = pu.rearrange("(i c) d -> c i d", c=128)
    pv_v = pv.rearrange("(i c) d -> c i d", c=128)
    pT5s = [pbk[7][:, :].bitcast(BF16), pbk[6][:, :].bitcast(BF16)]
    pT5vs = [x_.rearrange("p (c t) -> p c t", c=8) for x_ in pT5s]
    cnt5 = {"pg": 0, "ab": 0, "po": 0, "ev": 0}
    evb = [kb.al([128, 512], F32) for _ in range(2)]
    R_evb = [Res(), Res()]
    R_oacc_t = [Res() for _ in range(16)]

    def build_G(hf, cc):
        tk0, c0, gbuf = hf * TH, cc * 16, (hf * 8 + cc) % 2
        V(lambda e: e.tensor_scalar(out=i2c[:], in0=i2T[:, tk0:tk0 + TH], scalar1=float(-c0), scalar2=None, op0=ALU.add), r=[R_eiT], w=[R_i2c])
        for sb64 in range(TH // 64):
            t64 = sb64 * 64
            V(lambda e, t64=t64: e.tensor_tensor(out=btmp[:], in0=ioc[:, :].unsqueeze(1).to_broadcast([128, 64, 16]),
                                                in1=i2c[:, t64:t64 + 64].unsqueeze(2).to_broadcast([128, 64, 16]), op=ALU.is_equal), r=[R_i2c, R_io], w=[R_bblk])
            V(lambda e, t64=t64: e.tensor_tensor(out=bblk[:], in0=btmp[:], in1=gtT[:, tk0 + t64:tk0 + t64 + 64].unsqueeze(2).to_broadcast([128, 64, 16]), op=ALU.mult),
              r=[R_bblk, R_eiT], w=[R_bblk])
            for q32 in range(2):
                t0 = t64 + q32 * 32
                ab = cnt5["ab"] % 2
                cnt5["ab"] += 1
                V(lambda e, ab=ab, t0=t0: e.tensor_tensor(out=ablk[ab][:], in0=iob[:, :].unsqueeze(1).to_broadcast([128, 32, 128]),
                                                         in1=i1T[:, tk0 + t0:tk0 + t0 + 32].unsqueeze(2).to_broadcast([128, 32, 128]), op=ALU.is_equal), r=[R_eiT, R_io], w=[R_ablk[ab]])
                pg = pbk[4 + cnt5["pg"] % 2]
                R_pg = R_pb[4 + cnt5["pg"] % 2]
                cnt5["pg"] += 1
                for t in range(32):
                    T(lambda e, pg=pg, t=t, ab=ab, q32=q32: e.matmul(out=pg[:, t * 16:(t + 1) * 16], lhsT=ablk[ab][:, t, :], rhs=bblk[:, q32 * 32 + t, :], start=True, stop=True),
                      r=[R_ablk[ab], R_bblk], w=[R_pg], inc=(t == 31))
                A(lambda e, pg=pg, t0=t0, gbuf=gbuf: e.copy(out=Gc[gbuf][:, :, t0:t0 + 32].rearrange("p c t -> p t c"), in_=pg[:, :].rearrange("p (t c) -> p t c", c=16)), r=[R_pg], w=[R_Gc[gbuf]])

    def stage1(i, hf, c):
        ubf, gp, cj = i % 2, (i // 4) % 2, i % 4
        kb.dma("pool", ub[ubf][:], pu_v[c], w=[R_ub[ubf]])
        kb.dma("pool", vb[gp][cj][:], pv_v[c], w=[R_vb[gp][cj]])
        for half in range(2):
            for k in range(8):
                kk = half * 8 + k
                T(lambda e, k=k, kk=kk, ubf=ubf, half=half: e.transpose(out=pT5s[half][:, k * 128:(k + 1) * 128], in_=ub[ubf][:, kk * 128:(kk + 1) * 128], identity=ident[:]), r=[R_ub[ubf], R_c], w=[R_pb[7 - half]], inc=(k == 7))
            A(lambda e, half=half, ubf=ubf: e.copy(out=uT[ubf][:, half * 8:half * 8 + 8, :], in_=pT5vs[half]), r=[R_pb[7 - half]], w=[R_uT[ubf]])

    def stage2(i, hf, c):
        ubf, gp, cj = i % 2, (i // 4) % 2, i % 4
        tk0, gbuf, cl = hf * TH, (hf * 8 + c // 16) % 2, c % 16
        pa, R_pa = pbk[i % 2], R_pb[i % 2]
        for k in range(16):
            T(lambda e, pa=pa, k=k, ubf=ubf: e.matmul(out=pa[:, 0:TH], lhsT=uT[ubf][:, k, :], rhs=h2T[:, k, tk0:tk0 + TH], start=(k == 0), stop=(k == 15)),
              r=[R_uT[ubf], R_h2T], w=[R_pa], inc=(k == 15))
        A(lambda e, pa=pa, ubf=ubf: e.activation(out=ga[ubf][:], in_=pa[:, 0:TH], func=AF.Gelu), r=[R_pa], w=[R_ga[ubf]])
        V(lambda e, ubf=ubf, gp=gp, cj=cj, cl=cl, gbuf=gbuf: e.tensor_tensor(out=wg[gp][cj][:], in0=ga[ubf][:], in1=Gc[gbuf][:, cl, :], op=ALU.mult), r=[R_ga[ubf], R_Gc[gbuf]], w=[R_wg[gp][cj]])

    def stage3(i, hf):
        gp = (i // 4) % 2
        for tt in range(NTH):
            for ds in range(4):
                po = pbk[2 + cnt5["po"] % 4]
                R_po = R_pb[2 + cnt5["po"] % 4]
                cnt5["po"] += 1
                for cj in range(4):
                    T(lambda e, po=po, gp=gp, cj=cj, tt=tt, ds=ds: e.matmul(out=po[:], lhsT=wg[gp][cj][:, tt * 128:(tt + 1) * 128], rhs=vb[gp][cj][:, ds * 512:(ds + 1) * 512], start=(cj == 0), stop=(cj == 3)),
                      r=[R_wg[gp][cj], R_vb[gp][cj]], w=[R_po], inc=(cj == 3))
                R_oa = R_oacc_t[tt * 4 + ds]
                if (tt * 4 + ds) % 2 == 0:
                    V(lambda e, po=po, tt=tt, ds=ds: e.tensor_tensor(out=oacc[:, tt, ds * 512:(ds + 1) * 512], in0=oacc[:, tt, ds * 512:(ds + 1) * 512], in1=po[:], op=ALU.add), r=[R_po, R_oa], w=[R_oa])
                else:
                    ei = cnt5["ev"] % 2
                    cnt5["ev"] += 1
                    A(lambda e, po=po, ei=ei: e.copy(out=evb[ei][:], in_=po[:]), r=[R_po], w=[R_evb[ei]])
                    P(lambda e, tt=tt, ds=ds, ei=ei: e.tensor_tensor(out=oacc[:, tt, ds * 512:(ds + 1) * 512], in0=oacc[:, tt, ds * 512:(ds + 1) * 512], in1=evb[ei][:], op=ALU.add), r=[R_evb[ei], R_oa], w=[R_oa])

    items = [(hf, c) for hf in range(Tn // TH) for c in range(128)]
    for tt in range(NTH):
        kb.dma("sp", oacc[:, tt, :], x2d[tt * 128:(tt + 1) * 128, :], r=[R_x2d], w=R_oacc_t[tt * 4:tt * 4 + 4])
    build_G(0, 0)
    stage1(0, *items[0])
    for i, (hf, c) in enumerate(items):
        if i + 1 < len(items):
            stage1(i + 1, *items[i + 1])
        stage2(i, hf, c)
        if c % 16 == 7 and not (hf == Tn // TH - 1 and c // 16 == 7):
            nhf, ncc = (hf, c // 16 + 1) if c // 16 < 7 else (hf + 1, 0)
            build_G(nhf, ncc)
        if i % 4 == 3:
            stage3(i, hf)
        if c == 127:
            tk0 = hf * TH
            for tt in range(NTH):
                kb.dma("sp", out[tk0 + tt * 128:tk0 + (tt + 1) * 128, :], oacc[:, tt, :], r=R_oacc_t[tt * 4:tt * 4 + 4])
            if hf + 1 < Tn // TH:
                for tt in range(NTH):
                    kb.dma("sp", oacc[:, tt, :], x2d[tk0 + TH + tt * 128:tk0 + TH + (tt + 1) * 128, :], r=[R_x2d], w=R_oacc_t[tt * 4:tt * 4 + 4])


def wout_perm():
    idx = []
    for i in range(8):
        for hh in range(4):
            base = (0 if hh < 2 else 2048) + (2 * i + hh % 2) * 128
            idx.extend(range(base, base + 128))
    return np.array(idx)


def prep_B(inp, core, Tn, S):
    f = np.float32
    t0 = core * Tn
    return {
        "x_own": np.ascontiguousarray(inp["x"][0, t0:t0 + Tn]),
        "w_out": np.ascontiguousarray(inp["w_out"][0][wout_perm()]),
        "gffn": np.ascontiguousarray(inp["ffn_norm_g"][0]),
        "gffn_b": np.ascontiguousarray(np.broadcast_to(inp["ffn_norm_g"][0], (128, D))),
        "w_pq": np.ascontiguousarray(inp["w_peer_q"][0]),
        "skeys": np.ascontiguousarray(inp["peer_sub_keys"][0].reshape(16, 128, 128)),
        "pu": np.ascontiguousarray(inp["peer_u"][0]),
        "pv": np.ascontiguousarray(inp["peer_v"][0]),
    }


S_FULL = 8192


def _build_fused(S):
    kb = KB()
    io = {}
    for k, (sh, dt) in F_IN(S).items():
        io[k] = kb.dram(k, sh, dt, kind="ExternalInput").ap()
    for k, (sh, dt) in F_SCR(S).items():
        io[k] = kb.dram(k, sh, dt).ap()
    io["out"] = kb.dram("out", [S // 8, 2048], F32, kind="ExternalOutput").ap()
    shared = build_F(kb, S, io)
    io["mtin"] = io["mt_d"].rearrange("(c h) d t -> c h d t", c=8)
    kb.arena_reset()
    build_B(kb, S // 8, io, R_mtin=shared["R_mtd"], shared=None)
    return kb.build()


def kernel(**inputs):
    inp = {k: np.asarray(v) for k, v in inputs.items()}
    S = S_FULL
    nc = _build_fused(S)
    ins = [prep_F(inp, c, S) for c in range(8)]
    res = run_bass_kernel_spmd(nc, ins, core_ids=list(range(8)))
    out = np.zeros((S, 2048), np.float32)
    for c in range(8):
        rows = np.concatenate([np.arange(t * 128, (t + 1) * 128) for t in own_tiles(c, S)])
        out[rows] = np.asarray(res.results[c]["out"])
    return out.reshape(1, S, 2048)
```

```python
import math
import numpy as np
from contextlib import ExitStack
import concourse.bass as bass
import concourse.mybir as mybir
from concourse.bass_utils import run_bass_kernel_spmd

F32 = mybir.dt.float32
BF16 = mybir.dt.bfloat16
I32 = mybir.dt.int32
U32 = mybir.dt.uint32
ALU = mybir.AluOpType
AF = mybir.ActivationFunctionType
AX = mybir.AxisListType

NDS = 40
NDS_HW = 26
ARENA_BYTES = 206 * 1024


class Res:
    __slots__ = ("name", "w", "rs")

    def __init__(self, name=""):
        self.name = name
        self.w = None
        self.rs = {}


class KB:
    ENG = ("pe", "act", "dve", "pool", "sp")

    def __init__(self):
        self.nc = bass.Bass("TRN2", target_bir_lowering=False)
        self.es = ExitStack()
        self.q = {e: [] for e in self.ENG}
        self.cnt = {e: 0 for e in self.ENG}
        self.seen = {e: {} for e in self.ENG}
        self.sem = {e: self.es.enter_context(self.nc.semaphore("s_" + e)) for e in self.ENG}
        self.dsems = [self.es.enter_context(self.nc.semaphore("d%d" % i)) for i in range(NDS)]
        self.dcnt = [0] * NDS
        self.dnext = 0
        self.dnext_sw = 0
        self.nuniq = 0
        self.big = self.es.enter_context(self.nc.sbuf_tensor("big", [128, ARENA_BYTES // 2], BF16))
        self.top = 0
        self.pbank = [self.es.enter_context(self.nc.psum_tensor("pbank%d" % i, [128, 512], F32)) for i in range(8)]

    def al(self, shape, dt, name=None, parts=None):
        esz = {F32: 4, I32: 4, U32: 4, BF16: 2}[dt]
        n = 1
        for d in shape[1:]:
            n *= d
        nbytes = (n * esz + 63) // 64 * 64
        assert self.top + nbytes <= ARENA_BYTES, ("arena overflow", self.top, nbytes, name)
        o = self.top // 2
        self.top += nbytes
        v = self.big[0:shape[0], o:o + (n * esz) // 2]
        if dt != BF16:
            v = v.bitcast(dt)
        if len(shape) == 3:
            v = v.rearrange("p (a b) -> p a b", a=shape[1])
        elif len(shape) == 4:
            v = v.rearrange("p (a b c) -> p a b c", a=shape[1], b=shape[2])
        return v

    def barrier(self):
        toks = [(o, self.sem[o], self.cnt[o]) for o in self.ENG if self.cnt[o] > 0]
        toks += [("d%d" % i, self.dsems[i], self.dcnt[i]) for i in range(NDS) if self.dcnt[i] > 0]
        for e in self.ENG:
            self._waits(e, toks)

    def arena_reset(self):
        self.barrier()
        self.top = 0

    def mark(self):
        return self.top

    def release(self, m):
        self.barrier()
        self.top = m

    def sb(self, shape, dt, name=None):
        self.nuniq += 1
        return self.es.enter_context(self.nc.sbuf_tensor(name or ("sb%d" % self.nuniq), list(shape), dt))

    def ps(self, shape, dt=F32, name=None):
        self.nuniq += 1
        return self.es.enter_context(self.nc.psum_tensor(name or ("ps%d" % self.nuniq), list(shape), dt))

    def dram(self, name, shape, dt, kind="Internal"):
        return self.nc.dram_tensor(name, list(shape), dt, kind=kind)

    def _collect(self, r, w):
        toks = []
        for x in r:
            toks.append(x.w)
        for x in w:
            toks.append(x.w)
            toks.extend(x.rs.values())
        return toks

    def _waits(self, eng, toks):
        need = {}
        seen = self.seen[eng]
        for t in toks:
            if t is None:
                continue
            k, h, v = t
            if seen.get(k, 0) >= v:
                continue
            if k not in need or need[k][1] < v:
                need[k] = (h, v)
        for k, (h, v) in need.items():
            seen[k] = v
            self.q[eng].append(lambda e, h=h, v=v: e.wait_ge(h, v))

    def _record(self, tok, r, w):
        k = tok[0]
        for x in r:
            old = x.rs.get(k)
            if old is None or old[2] < tok[2]:
                x.rs[k] = tok
        for x in w:
            x.w = tok
            x.rs = {}

    def op(self, eng, fn, r=(), w=(), pe_acc=False, inc=True):
        toks = self._collect(r, w)
        if pe_acc:
            toks = [t for t in toks if t is None or t[0] != eng]
        self._waits(eng, toks)
        h = self.sem[eng]
        if inc:
            self.cnt[eng] += 1
            n = self.cnt[eng]
            self.q[eng].append(lambda e: fn(e).then_inc(h, 1))
        else:
            n = self.cnt[eng] + 1
            self.q[eng].append(lambda e: fn(e))
        tok = (eng, h, n)
        self._record(tok, r, w)
        return tok

    def dma(self, qe, out, in_, r=(), w=(), fn=None, **kw):
        toks = self._collect(r, w)
        if qe == "pool":
            i = NDS_HW + self.dnext_sw
            self.dnext_sw = (self.dnext_sw + 1) % (NDS - NDS_HW)
        else:
            i = self.dnext
            self.dnext = (i + 1) % NDS_HW
        key = "d%d" % i
        h = self.dsems[i]
        if self.dcnt[i] > 0:
            toks.append((key, h, self.dcnt[i]))
        self._waits(qe, toks)
        self.dcnt[i] += 16
        v = self.dcnt[i]
        if fn is None:
            self.q[qe].append(lambda e: e.dma_start(out=out, in_=in_, **kw).then_inc(h, 16))
        else:
            self.q[qe].append(lambda e: fn(e).then_inc(h, 16))
        tok = (key, h, v)
        self._record(tok, r, w)
        return tok

    def wait_all_dma(self, eng="sp"):
        toks = [("d%d" % i, self.dsems[i], self.dcnt[i]) for i in range(NDS) if self.dcnt[i] > 0]
        self._waits(eng, toks)

    def build(self):
        self.wait_all_dma("sp")
        nc = self.nc
        q = self.q
        with nc.Block() as block:
            @block.tensor
            def _(e):
                for f in q["pe"]:
                    f(e)

            @block.scalar
            def _(e):
                for f in q["act"]:
                    f(e)

            @block.vector
            def _(e):
                for f in q["dve"]:
                    f(e)

            @block.gpsimd
            def _(e):
                for f in q["pool"]:
                    f(e)

            @block.sync
            def _(e):
                for f in q["sp"]:
                    f(e)
        self.es.close()
        return nc


D = 2048
EPS = 1e-6
NC = 8
NH = 16
G_KL, G_QL, G_QN, G_KN, G_FQ, G_FK, G_BF, G_IF = 0, 512, 1024, 1216, 1408, 1536, 1664, 1680
G_NV = 1712


def build_F(kb, S, io):
    NT = S // 128
    NU = NT // NC
    TO = NU * 128
    x, xo = io["x"], io["x_own"]
    V = lambda fn, r=(), w=(): kb.op("dve", fn, r, w)
    A = lambda fn, r=(), w=(): kb.op("act", fn, r, w)
    P = lambda fn, r=(), w=(): kb.op("pool", fn, r, w)
    T = lambda fn, r=(), w=(), inc=True: kb.op("pe", fn, r, w, pe_acc=True, inc=inc)
    pbk = kb.pbank
    R_pb = [Res("pb%d" % i) for i in range(8)]
    pT = pbk[7][:, :].bitcast(BF16)
    pTv = pT.rearrange("p (c t) -> p c t", c=8)
    R_pT = R_pb[7]
    pT2 = [pT, pbk[6][:, :].bitcast(BF16)]
    pT2v = [pTv, pT2[1].rearrange("p (c t) -> p c t", c=8)]
    R_pT2 = [R_pb[7], R_pb[6]]
    tcount = [0]
    ktn_d, ktp_d, ktf_d, vm_d, vf_d, ht_d = io["ktn_d"], io["ktp_d"], io["ktf_d"], io["vm_d"], io["vf_d"], io["ht_d"]
    qtn_d, qtp_d, qtf_d, gt_d = io["qtn_d"], io["qtp_d"], io["qtf_d"], io["gt_d"]
    R_ktn, R_ktp, R_ktf, R_vm, R_vf, R_htd = Res(), Res(), Res(), Res(), Res(), Res()
    R_qtn, R_qtp, R_qtf, R_gtd = Res(), Res(), Res(), Res()

    identf = kb.al([128, 128], F32)
    ident = kb.al([128, 128], BF16)
    trif = kb.al([128, 128], F32)
    onesf = kb.al([128, 128], F32)
    onesb = kb.al([128, 128], BF16)
    nhalf = kb.al([128, 16], F32)
    R_c = Res("consts")
    P(lambda e: e.memset(identf[:], 0.0), w=[R_c])
    P(lambda e: e.affine_select(out=identf[:], in_=identf[:], pattern=[[-1, 128]], compare_op=ALU.not_equal, fill=1.0, base=0, channel_multiplier=1), r=[R_c], w=[R_c])
    P(lambda e: e.memset(onesf[:], 1.0), w=[R_c])
    P(lambda e: e.affine_select(out=trif[:], in_=onesf[:], pattern=[[1, 128]], compare_op=ALU.is_ge, fill=0.0, base=0, channel_multiplier=-1), r=[R_c], w=[R_c])
    P(lambda e: e.memset(nhalf[:], -0.5), w=[R_c])
    epsc = kb.al([128, 1], F32)
    P(lambda e: e.memset(epsc[:], EPS), w=[R_c])
    V(lambda e: e.tensor_copy(out=ident[:], in_=identf[:]), r=[R_c], w=[R_c])
    V(lambda e: e.tensor_copy(out=onesb[:], in_=onesf[:]), r=[R_c], w=[R_c])
    gT = kb.al([128, 16], F32)
    gv = kb.al([128, G_NV], F32)
    R_g = Res("gains")
    kb.dma("sp", gT[:], io["gmix"].rearrange("(c p) -> p c", p=128), w=[R_g], allow_slow_non_contiguous=True)
    kb.dma("sp", gv[:], io["gvf"], w=[R_g])
    V(lambda e: e.tensor_scalar(out=gv[:, G_QN:G_QN + 192], in0=gv[:, G_QN:G_QN + 192], scalar1=192 ** -0.5, scalar2=None, op0=ALU.mult), r=[R_g], w=[R_g])
    V(lambda e: e.tensor_scalar(out=gv[:, G_FQ:G_FQ + 128], in0=gv[:, G_FQ:G_FQ + 128], scalar1=128 ** -0.5, scalar2=None, op0=ALU.mult), r=[R_g], w=[R_g])
    flog = kb.al([128, NH, NT], F32)
    cend = kb.al([128, NH, NT], F32)
    R_fl = Res("flog")
    st = kb.al([128, 64], F32)
    rs = kb.al([128, 64], F32)
    R_st = Res("st")
    iota_i = kb.al([128, 16], I32)
    iota_f = kb.al([128, 16], F32)
    gT2 = kb.al([128, 16], F32)
    R_gB = Res("gB")
    P(lambda e: e.iota(iota_i[:], pattern=[[1, 16]], base=0, channel_multiplier=0), w=[R_c])
    V(lambda e: e.tensor_copy(out=iota_f[:], in_=iota_i[:]), r=[R_c], w=[R_c])
    kb.dma("sp", gT2[:], io["gffn"].rearrange("(c p) -> p c", p=128), w=[R_gB], allow_slow_non_contiguous=True)
    m_base = kb.mark()

    def rstd(a, b, inv_n, S_=None, bias_ap=None, R_bias=None):
        st_, rs_, R_ = S_ if S_ is not None else (st, rs, R_st)
        bap = bias_ap if bias_ap is not None else epsc[:, 0:1]
        A(lambda e: e.activation(out=rs_[:, a:b], in_=st_[:, a:b], func=AF.Sqrt, scale=inv_n, bias=bap), r=[R_, R_c] + ([R_bias] if R_bias is not None else []), w=[R_])
        V(lambda e: e.reciprocal(out=rs_[:, a:b], in_=rs_[:, a:b]), r=[R_], w=[R_])

    def rope_tables(posd, n, sin_t, cos_t, R_rp):
        posi = kb.al([128, n], I32)
        posf = kb.al([128, n], F32)
        ang = kb.al([128, n, 32], F32)
        tq = kb.al([128, n, 32], F32)
        tki = kb.al([128, n, 32], I32)
        kb.dma("sp", posi[:], posd, w=[R_rp])
        V(lambda e: e.tensor_copy(out=posf[:], in_=posi[:]), r=[R_rp], w=[R_rp])
        V(lambda e: e.tensor_tensor(out=ang[:], in0=posf[:, :].unsqueeze(2).to_broadcast([128, n, 32]),
                                    in1=gv[:, G_IF:G_IF + 32].unsqueeze(1).to_broadcast([128, n, 32]), op=ALU.mult), r=[R_rp, R_g], w=[R_rp])
        for dst, shift in ((sin_t, 0.0), (cos_t, math.pi / 2)):
            V(lambda e, shift=shift: e.tensor_scalar(out=tq[:], in0=ang[:], scalar1=shift, scalar2=1.0 / (2 * math.pi), op0=ALU.add, op1=ALU.mult), r=[R_rp], w=[R_rp])
            V(lambda e: e.tensor_copy(out=tki[:], in_=tq[:]), r=[R_rp], w=[R_rp])
            V(lambda e: e.tensor_copy(out=tq[:], in_=tki[:]), r=[R_rp], w=[R_rp])
            V(lambda e: e.tensor_scalar(out=tq[:], in0=tq[:], scalar1=-2 * math.pi, scalar2=None, op0=ALU.mult), r=[R_rp], w=[R_rp])
            V(lambda e, shift=shift: e.scalar_tensor_tensor(out=tq[:], in0=ang[:], scalar=shift, in1=tq[:], op0=ALU.add, op1=ALU.add), r=[R_rp], w=[R_rp])
            V(lambda e, dst=dst: e.tensor_scalar(out=dst[:], in0=tq[:], scalar1=math.pi, scalar2=-2 * math.pi, op0=ALU.is_gt, op1=ALU.mult), r=[R_rp], w=[R_rp])
            V(lambda e, dst=dst: e.tensor_tensor(out=tq[:], in0=tq[:], in1=dst[:], op=ALU.add), r=[R_rp], w=[R_rp])
            V(lambda e: e.tensor_scalar(out=tq[:], in0=tq[:], scalar1=-3.1415925, scalar2=3.1415925, op0=ALU.max, op1=ALU.min), r=[R_rp], w=[R_rp])
            A(lambda e, dst=dst: e.activation(out=dst[:], in_=tq[:], func=AF.Sin), r=[R_rp], w=[R_rp])

    def load_w(dst, src, c0, c1, R):
        sv = src.rearrange("(c p) n -> p c n", p=128)
        n = c1 - c0
        step = 2048
        for c in range(0, 16, 4):
            for a in range(0, n, step):
                b = min(n, a + step)
                kb.dma("pool", dst[:, c:c + 4, a:b], sv[:, c:c + 4, c0 + a:c0 + b], w=[R])

    def x_to_hT(xt_b, R_xt_b, junk, R_junk, xn, R_xn, hT, R_hT, S_=None):
        st_, rs_, R_ = S_ if S_ is not None else (st, rs, R_st)
        A(lambda e: e.activation(out=junk[:], in_=xt_b[:], func=AF.Square, accum_out=st_[:, 0:1]), r=[R_xt_b], w=[R_junk, R_])
        rstd(0, 1, 1.0 / D, S_)
        V(lambda e: e.tensor_scalar(out=xn[:], in0=xt_b[:], scalar1=rs_[:, 0:1], scalar2=None, op0=ALU.mult), r=[R_xt_b, R_], w=[R_xn])
        for half in range(2):
            ti_ = tcount[0] % 2
            tcount[0] += 1
            for c in range(8):
                cc = half * 8 + c
                T(lambda e, c=c, cc=cc, ti_=ti_: e.transpose(out=pT2[ti_][:, c * 128:(c + 1) * 128], in_=xn[:, cc * 128:(cc + 1) * 128], identity=ident[:]), r=[R_xn, R_c], w=[R_pT2[ti_]], inc=(c == 7))
            V(lambda e, half=half, ti_=ti_: e.tensor_tensor(out=hT[:, half * 8:half * 8 + 8, :], in0=pT2v[ti_], in1=gT[:, half * 8:half * 8 + 8].unsqueeze(2).to_broadcast([128, 8, 128]), op=ALU.mult),
              r=[R_pT2[ti_], R_g], w=[R_hT])

    def head_norm(src, nh, dh, so, R_src, extra=None, R_extra=None):
        sq = kb_sq[:, 0:nh * dh].rearrange("p (h d) -> p h d", h=nh)
        A(lambda e: e.activation(out=sq, in_=src, func=AF.Square), r=[R_src], w=[R_sq])
        V(lambda e: e.tensor_reduce(out=st[:, so:so + nh], in_=sq, axis=AX.X, op=ALU.add), r=[R_sq], w=[R_st])
        if extra is not None:
            V(lambda e: e.tensor_scalar(out=st[:, so:so + nh], in0=st[:, so:so + nh], scalar1=extra, scalar2=None, op0=ALU.add), r=[R_st] + ([R_extra] if R_extra is not None else []), w=[R_st])

    def scale_heads(dst, src, nh, dh, so, goff, R_src, R_dst, tmp):
        V(lambda e: e.tensor_tensor(out=tmp, in0=src, in1=rs[:, so:so + nh].unsqueeze(2).to_broadcast([128, nh, dh]), op=ALU.mult), r=[R_src, R_st], w=[R_sq])
        V(lambda e: e.tensor_tensor(out=dst, in0=tmp, in1=gv[:, goff:goff + dh].unsqueeze(1).to_broadcast([128, nh, dh]), op=ALU.mult), r=[R_sq, R_g], w=[R_dst])

    def rope_heads(pe_in, pe_out, cs_row, sn_row, nh, R_in, R_out, R_rp):
        cs = cs_row.unsqueeze(1).to_broadcast([128, nh, 32])
        sn = sn_row.unsqueeze(1).to_broadcast([128, nh, 32])
        x1, x2 = pe_in[:, :, 0:32], pe_in[:, :, 32:64]
        t = [kb_pet[:, i, 0:nh, :] for i in range(4)]
        V(lambda e: e.tensor_tensor(out=t[0], in0=x1, in1=cs, op=ALU.mult), r=[R_in, R_rp], w=[R_pet])
        V(lambda e: e.tensor_tensor(out=t[1], in0=x2, in1=sn, op=ALU.mult), r=[R_in, R_rp], w=[R_pet])
        V(lambda e: e.tensor_tensor(out=t[2], in0=x1, in1=sn, op=ALU.mult), r=[R_in, R_rp], w=[R_pet])
        V(lambda e: e.tensor_tensor(out=t[3], in0=x2, in1=cs, op=ALU.mult), r=[R_in, R_rp], w=[R_pet])
        V(lambda e: e.tensor_tensor(out=pe_out[:, :, 0:32], in0=t[0], in1=t[1], op=ALU.subtract), r=[R_pet], w=[R_out])
        V(lambda e: e.tensor_tensor(out=pe_out[:, :, 32:64], in0=t[2], in1=t[3], op=ALU.add), r=[R_pet], w=[R_out])

    def transpose_out(src_flat, nblk, dst_fn, R_src, scale_col=None):
        for b0 in range(0, nblk, 8):
            b1 = min(nblk, b0 + 8)
            ti_ = tcount[0] % 2
            tcount[0] += 1
            for c in range(b1 - b0):
                T(lambda e, c=c, b0=b0, ti_=ti_: e.transpose(out=pT2[ti_][:, c * 128:(c + 1) * 128], in_=src_flat[:, (b0 + c) * 128:(b0 + c + 1) * 128], identity=ident[:]), r=[R_src, R_c], w=[R_pT2[ti_]], inc=(c == b1 - b0 - 1))
            sb_ = kb_stage_i[0] % 2
            kb_stage_i[0] += 1
            stg, R_stg = kb_stage[sb_], R_stage[sb_]
            if scale_col is None:
                A(lambda e, stg=stg, n=b1 - b0, ti_=ti_: e.copy(out=stg[:, 0:n, :], in_=pT2v[ti_][:, 0:n, :]), r=[R_pT2[ti_]], w=[R_stg])
            else:
                A(lambda e, stg=stg, n=b1 - b0, ti_=ti_: e.mul(out=stg[:, 0:n, :], in_=pT2v[ti_][:, 0:n, :], mul=scale_col[:, 0:1]), r=[R_pT2[ti_], R_g], w=[R_stg])
            dst_fn(b0, b1, stg, R_stg)

    w1 = kb.al([128, 16, 592], BF16)
    wkvu = kb.al([128, 4, 4096], BF16)
    R_w1, R_wkvu = Res(), Res()
    load_w(w1, io["w_kv1"], 0, 592, R_w1)
    wkv_v = io["w_kvup"].rearrange("(c p) n -> p c n", p=128)
    for c in range(4):
        for a in range(0, 4096, 2048):
            kb.dma("pool", wkvu[:, c, a:a + 2048], wkv_v[:, c, a:a + 2048], w=[R_wkvu])
    sin_a = kb.al([128, NT, 32], F32)
    cos_a = kb.al([128, NT, 32], F32)
    R_rpa = Res("ropeA")
    m_r = kb.mark()
    rope_tables(io["pos_all"], NT, sin_a, cos_a, R_rpa)
    kb.release(m_r)
    xt = [kb.al([128, D], F32) for _ in range(2)]
    R_xt = [Res(), Res()]
    junk = kb.al([128, D], BF16)
    R_junk = Res()
    xn = kb.al([128, D], BF16)
    R_xn = Res()
    hT = kb.al([128, 16, 128], BF16)
    R_hT = Res()
    kb_sq = kb.al([128, 2048], F32)
    R_sq = Res()
    kb_pet = kb.al([128, 4, 16, 32], F32)
    R_pet = Res()
    kb_stage = [kb.al([128, 8, 128], BF16) for _ in range(2)]
    R_stage = [Res(), Res()]
    kb_stage_i = [0]
    lat = kb.al([128, 512], BF16)
    R_lat = Res()
    latT = kb.al([128, 4, 128], BF16)
    R_latT = Res()
    kpe_f = kb.al([128, 64], F32)
    R_kpe = Res()
    kbuf = kb.al([128, 16, 128], BF16)
    R_kbuf = Res()
    pein = kb.al([128, 16, 64], F32)
    R_pein = Res()
    peout = kb.al([128, 16, 64], BF16)
    R_peout = Res()
    vbuf = kb.al([128, 16, 128], BF16)
    R_vbuf = Res()
    tmp8 = kb_sq

    gcol_kn = kb.al([128, 1], F32)
    kb.dma("sp", gcol_kn[:], io["gvf"][0:1, G_KN:G_KN + 128].rearrange("o d -> d o"), w=[R_g], allow_slow_non_contiguous=True)
    stA = kb.al([128, 8], F32)
    rsA = kb.al([128, 8], F32)
    R_stA = Res("stA")
    SA = (stA, rsA, R_stA)
    latT2 = [latT, kb.al([128, 4, 128], BF16)]
    R_latT2 = [R_latT, Res()]
    kpe2 = [kpe_f, kb.al([128, 64], F32)]
    R_kpe2 = [R_kpe, Res()]
    kb.dma("sp", xt[0][:], x[0:128, :], w=[R_xt[0]])

    def stageA(t):
        b = t % 2
        if t + 1 < NT:
            kb.dma("sp", xt[1 - b][:], x[(t + 1) * 128:(t + 2) * 128, :], w=[R_xt[1 - b]])
        x_to_hT(xt[b], R_xt[b], junk, R_junk, xn, R_xn, hT, R_hT, SA)
        kb.dma("sp", ht_d[t], hT[:], r=[R_hT], w=[R_htd])
        for c in range(16):
            T(lambda e, c=c: e.matmul(out=pbk[4][:], lhsT=hT[:, c, :], rhs=w1[:, c, 0:512], start=(c == 0), stop=(c == 15)), r=[R_hT, R_w1], w=[R_pb[4]], inc=(c == 15))
        for c in range(16):
            T(lambda e, c=c: e.matmul(out=pbk[5][:, 0:80], lhsT=hT[:, c, :], rhs=w1[:, c, 512:592], start=(c == 0), stop=(c == 15)), r=[R_hT, R_w1], w=[R_pb[5]], inc=(c == 15))
        A(lambda e: e.activation(out=junk[:, 0:512], in_=pbk[4][:], func=AF.Square, accum_out=stA[:, 0:1]), r=[R_pb[4]], w=[R_junk, R_stA])
        A(lambda e, b=b: e.activation(out=junk[:, 0:64], in_=pbk[5][:, 0:64], func=AF.Square, accum_out=stA[:, 2 + b:3 + b]), r=[R_pb[5]], w=[R_junk, R_stA])
        rstd(0, 1, 1.0 / 512, SA)
        V(lambda e, b=b: e.tensor_scalar(out=stA[:, 4 + b:5 + b], in0=stA[:, 2 + b:3 + b], scalar1=1.0 / 192, scalar2=EPS, op0=ALU.mult, op1=ALU.add), r=[R_stA], w=[R_stA])
        V(lambda e: e.scalar_tensor_tensor(out=lat[:], in0=pbk[4][:], scalar=rsA[:, 0:1], in1=gv[:, G_KL:G_KL + 512], op0=ALU.mult, op1=ALU.mult), r=[R_pb[4], R_stA, R_g], w=[R_lat])
        A(lambda e, b=b: e.copy(out=kpe2[b][:], in_=pbk[5][:, 0:64]), r=[R_pb[5]], w=[R_kpe2[b]])
        A(lambda e, t=t: e.copy(out=flog[:, :, t], in_=pbk[5][:, 64:80]), r=[R_pb[5]], w=[R_fl])
        ti_ = tcount[0] % 2
        tcount[0] += 1
        for c in range(4):
            T(lambda e, c=c, ti_=ti_: e.transpose(out=pT2[ti_][:, c * 128:(c + 1) * 128], in_=lat[:, c * 128:(c + 1) * 128], identity=ident[:]), r=[R_lat, R_c], w=[R_pT2[ti_]], inc=(c == 3))
        A(lambda e, b=b, ti_=ti_: e.copy(out=latT2[b][:], in_=pT2v[ti_][:, 0:4, :]), r=[R_pT2[ti_]], w=[R_latT2[b]])

    def stageB(t):
        b = t % 2
        for half in range(2):
            for c in range(4):
                for k in range(4):
                    T(lambda e, c=c, k=k, half=half, b=b: e.matmul(out=pbk[k][:], lhsT=latT2[b][:, c, :], rhs=wkvu[:, c, half * 2048 + k * 512:half * 2048 + (k + 1) * 512], start=(c == 0), stop=(c == 3)),
                      r=[R_latT2[b], R_wkvu], w=[R_pb[k]])
            for k in range(4):
                src = pbk[k][:, :].rearrange("p (h two d) -> p h two d", h=2, two=2)
                h0 = half * 8 + 2 * k
                head_norm(src[:, :, 0, :], 2, 128, 32 + h0, R_pb[k])
                A(lambda e, src=src, h0=h0: e.copy(out=vbuf[:, h0:h0 + 2, :], in_=src[:, :, 1, :]), r=[R_pb[k]], w=[R_vbuf])
            rstd(32 + half * 8, 32 + half * 8 + 8, 1.0 / 192, bias_ap=stA[:, 4 + b:5 + b], R_bias=R_stA)
            for k in range(4):
                src = pbk[k][:, :].rearrange("p (h two d) -> p h two d", h=2, two=2)
                h0 = half * 8 + 2 * k
                V(lambda e, src=src, h0=h0: e.tensor_tensor(out=kbuf[:, h0:h0 + 2, :], in0=src[:, :, 0, :], in1=rs[:, 32 + h0:32 + h0 + 2].unsqueeze(2).to_broadcast([128, 2, 128]), op=ALU.mult), r=[R_pb[k], R_st], w=[R_kbuf])
        transpose_out(kbuf[:, :, :].rearrange("p h d -> p (h d)"), 16,
                      lambda b0, b1, stg, R_stg, t=t: kb.dma("sp", ktn_d[b0:b1, :, t * 128:(t + 1) * 128].rearrange("b p s -> p b s"), stg[:, 0:b1 - b0, :], r=[R_stg], w=[R_ktn]), R_kbuf, scale_col=gcol_kn)
        kb.dma("sp", vm_d[t * 128:(t + 1) * 128, :], vbuf[:].rearrange("p h d -> p (h d)"), r=[R_vbuf], w=[R_vm])
        V(lambda e, b=b: e.tensor_tensor(out=pein[:], in0=kpe2[b][:, :].unsqueeze(1).to_broadcast([128, 16, 64]), in1=rs[:, 32:48].unsqueeze(2).to_broadcast([128, 16, 64]), op=ALU.mult), r=[R_kpe2[b], R_st], w=[R_pein])
        V(lambda e: e.tensor_tensor(out=pein[:], in0=pein[:], in1=gv[:, G_KN + 128:G_KN + 192].unsqueeze(1).to_broadcast([128, 16, 64]), op=ALU.mult), r=[R_pein, R_g], w=[R_pein])
        rope_heads(pein, peout, cos_a[:, t, :], sin_a[:, t, :], 16, R_pein, R_peout, R_rpa)
        transpose_out(peout[:, :, :].rearrange("p h d -> p (h d)"), 8,
                      lambda b0, b1, stg, R_stg, t=t: kb.dma("sp", ktp_d[b0:b1, :, t * 128:(t + 1) * 128].rearrange("b p s -> p b s"), stg[:, 0:b1 - b0, :], r=[R_stg], w=[R_ktp]), R_peout)

    stageA(0)
    for t in range(NT):
        if t + 1 < NT:
            stageA(t + 1)
        stageB(t)

    kb.release(m_base)
    wfk = kb.al([128, 16, 2048], BF16)
    wfv = kb.al([128, 16, 2048], BF16)
    R_wfk, R_wfv = Res(), Res()
    load_w(wfk, io["w_fk"], 0, 2048, R_wfk)
    load_w(wfv, io["w_fv"], 0, 2048, R_wfv)
    hTb = [kb.al([128, 16, 128], BF16) for _ in range(2)]
    R_hTb = [Res(), Res()]
    vbf = [kb.al([128, 2048], BF16) for _ in range(2)]
    R_vbf = [Res(), Res()]
    kb_sq = kb.al([128, 2048], F32)
    R_sq = Res()
    kb_stage = [kb.al([128, 8, 128], BF16) for _ in range(2)]
    R_stage = [Res(), Res()]
    kbuf2 = kb.al([128, 16, 128], BF16)
    R_kbuf2 = Res()
    tmp82 = kb_sq
    kb.dma("sp", hTb[0][:], ht_d[0], r=[R_htd], w=[R_hTb[0]])
    for t in range(NT):
        b = t % 2
        if t + 1 < NT:
            kb.dma("sp", hTb[1 - b][:], ht_d[t + 1], r=[R_htd], w=[R_hTb[1 - b]])
        for c in range(16):
            for k in range(4):
                T(lambda e, c=c, k=k, b=b: e.matmul(out=pbk[k][:], lhsT=hTb[b][:, c, :], rhs=wfk[:, c, k * 512:(k + 1) * 512], start=(c == 0), stop=(c == 15)), r=[R_hTb[b], R_wfk], w=[R_pb[k]])
        for k in range(4):
            src = pbk[k][:, :].rearrange("p (h d) -> p h d", h=4)
            head_norm(src, 4, 128, 16 + 4 * k, R_pb[k])
        rstd(16, 32, 1.0 / 128)
        for k in range(4):
            src = pbk[k][:, :].rearrange("p (h d) -> p h d", h=4)
            scale_heads(kbuf2[:, 4 * k:4 * k + 4, :], src, 4, 128, 16 + 4 * k, G_FK, R_pb[k], R_kbuf2, tmp82[:, 0:512].rearrange("p (h d) -> p h d", h=4))
        vbank = [4, 5, 6, 0]
        for c in range(16):
            for k in range(4):
                kk = vbank[k]
                T(lambda e, c=c, k=k, kk=kk, b=b: e.matmul(out=pbk[kk][:], lhsT=hTb[b][:, c, :], rhs=wfv[:, c, k * 512:(k + 1) * 512], start=(c == 0), stop=(c == 15)), r=[R_hTb[b], R_wfv], w=[R_pb[kk]])
        for k in range(4):
            kk = vbank[k]
            A(lambda e, k=k, kk=kk, b=b: e.copy(out=vbf[b][:, k * 512:(k + 1) * 512], in_=pbk[kk][:]), r=[R_pb[kk]], w=[R_vbf[b]])
        kb.dma("sp", vf_d[t * 128:(t + 1) * 128, :], vbf[b][:], r=[R_vbf[b]], w=[R_vf])
        transpose_out(kbuf2[:, :, :].rearrange("p h d -> p (h d)"), 16,
                      lambda b0, b1, stg, R_stg, t=t: kb.dma("sp", ktf_d[b0:b1, :, t * 128:(t + 1) * 128].rearrange("b p s -> p b s"), stg[:, 0:b1 - b0, :], r=[R_stg], w=[R_ktf]), R_kbuf2)

    kb.release(m_base)
    hTo = kb.al([128, 16, TO], BF16)
    R_hTo = Res()
    sin_o = kb.al([128, NU, 32], F32)
    cos_o = kb.al([128, NU, 32], F32)
    R_rpo = Res("ropeO")
    m_q = kb.mark()
    rope_tables(io["pos_own"], NU, sin_o, cos_o, R_rpo)
    kb.release(m_q)
    xt_q = [kb.al([128, D], F32) for _ in range(2)]
    R_xt_q = [Res(), Res()]
    junk_q = kb.al([128, D], BF16)
    R_junk_q = Res()
    xn_q = kb.al([128, D], BF16)
    R_xn_q = Res()
    hT1 = kb.al([128, 16, 128], BF16)
    R_hT1 = Res()
    for u in range(NU):
        b = u % 2
        kb.dma("sp", xt_q[b][:], xo[u * 128:(u + 1) * 128, :], w=[R_xt_q[b]])
        x_to_hT(xt_q[b], R_xt_q[b], junk_q, R_junk_q, xn_q, R_xn_q, hT1, R_hT1)
        V(lambda e, u=u: e.tensor_copy(out=hTo[:, :, u * 128:(u + 1) * 128], in_=hT1[:]), r=[R_hT1], w=[R_hTo])
    kb.release(m_q)
    wq1 = kb.al([128, 16, 512], BF16)
    wqu = kb.al([128, 4, 3072], BF16)
    wfq = kb.al([128, 16, 2048], BF16)
    R_wq1, R_wqu, R_wfq = Res(), Res(), Res()
    load_w(wq1, io["w_cq"], 0, 512, R_wq1)
    wqu_v = io["w_qup"].rearrange("(c p) n -> p c n", p=128)
    for c in range(4):
        for a in range(0, 3072, 1536):
            kb.dma("pool", wqu[:, c, a:a + 1536], wqu_v[:, c, a:a + 1536], w=[R_wqu])
    load_w(wfq, io["w_fq"], 0, 2048, R_wfq)
    junk_q = kb.al([128, D], BF16)
    R_junk_q = Res()
    kb_sq = kb.al([128, 2048], F32)
    R_sq = Res()
    kb_pet = kb.al([128, 4, 16, 32], F32)
    R_pet = Res()
    kb_stage = [kb.al([128, 8, 128], BF16) for _ in range(2)]
    R_stage = [Res(), Res()]
    lat_q = kb.al([128, 512], BF16)
    R_lat_q = Res()
    latT_q = kb.al([128, 4, 128], BF16)
    R_latT_q = Res()
    kbuf_q = kb.al([128, 16, 128], BF16)
    R_kbuf_q = Res()
    pein_q = kb.al([128, 16, 64], F32)
    R_pein_q = Res()
    peout_q = kb.al([128, 16, 64], BF16)
    R_peout_q = Res()
    tmp8 = kb_sq
    for u in range(NU):
        hTu = hTo[:, :, u * 128:(u + 1) * 128]
        for c in range(16):
            T(lambda e, c=c, hTu=hTu: e.matmul(out=pbk[6][:], lhsT=hTu[:, c, :], rhs=wq1[:, c, :], start=(c == 0), stop=(c == 15)), r=[R_hTo, R_wq1], w=[R_pb[6]])
        A(lambda e: e.activation(out=junk_q[:, 0:512], in_=pbk[6][:], func=AF.Square, accum_out=st[:, 0:1]), r=[R_pb[6]], w=[R_junk_q, R_st])
        rstd(0, 1, 1.0 / 512)
        V(lambda e: e.scalar_tensor_tensor(out=lat_q[:], in0=pbk[6][:], scalar=rs[:, 0:1], in1=gv[:, G_QL:G_QL + 512], op0=ALU.mult, op1=ALU.mult), r=[R_pb[6], R_st, R_g], w=[R_lat_q])
        for c in range(4):
            T(lambda e, c=c: e.transpose(out=pT[:, c * 128:(c + 1) * 128], in_=lat_q[:, c * 128:(c + 1) * 128], identity=ident[:]), r=[R_lat_q, R_c], w=[R_pT])
        A(lambda e: e.copy(out=latT_q[:], in_=pTv[:, 0:4, :]), r=[R_pT], w=[R_latT_q])
        for grp in range(8):
            kk = grp % 6
            for c in range(4):
                T(lambda e, c=c, grp=grp, kk=kk: e.matmul(out=pbk[kk][:, 0:384], lhsT=latT_q[:, c, :], rhs=wqu[:, c, grp * 384:(grp + 1) * 384], start=(c == 0), stop=(c == 3)), r=[R_latT_q, R_wqu], w=[R_pb[kk]])
            src = pbk[kk][:, 0:384].rearrange("p (h d) -> p h d", h=2)
            h0 = 2 * grp
            head_norm(src, 2, 192, 48, R_pb[kk])
            rstd(48, 50, 1.0 / 192)
            scale_heads(kbuf_q[:, h0:h0 + 2, :], src[:, :, 0:128], 2, 128, 48, G_QN, R_pb[kk], R_kbuf_q, tmp8[:, 0:256].rearrange("p (h d) -> p h d", h=2))
            scale_heads(pein_q[:, h0:h0 + 2, :], src[:, :, 128:192], 2, 64, 48, G_QN + 128, R_pb[kk], R_pein_q, tmp8[:, 256:384].rearrange("p (h d) -> p h d", h=2))
        transpose_out(kbuf_q[:, :, :].rearrange("p h d -> p (h d)"), 16,
                      lambda b0, b1, stg, R_stg, u=u: kb.dma("sp", qtn_d[b0:b1, :, u * 128:(u + 1) * 128].rearrange("b p s -> p b s"), stg[:, 0:b1 - b0, :], r=[R_stg], w=[R_qtn]), R_kbuf_q)
        rope_heads(pein_q, peout_q, cos_o[:, u, :], sin_o[:, u, :], 16, R_pein_q, R_peout_q, R_rpo)
        transpose_out(peout_q[:, :, :].rearrange("p h d -> p (h d)"), 8,
                      lambda b0, b1, stg, R_stg, u=u: kb.dma("sp", qtp_d[b0:b1, :, u * 128:(u + 1) * 128].rearrange("b p s -> p b s"), stg[:, 0:b1 - b0, :], r=[R_stg], w=[R_qtp]), R_peout_q)
        for c in range(16):
            for k in range(4):
                T(lambda e, c=c, k=k, hTu=hTu: e.matmul(out=pbk[k][:], lhsT=hTu[:, c, :], rhs=wfq[:, c, k * 512:(k + 1) * 512], start=(c == 0), stop=(c == 15)), r=[R_hTo, R_wfq], w=[R_pb[k]])
        for k in range(4):
            src = pbk[k][:, :].rearrange("p (h d) -> p h d", h=4)
            head_norm(src, 4, 128, 16 + 4 * k, R_pb[k])
        rstd(16, 32, 1.0 / 128)
        for k in range(4):
            src = pbk[k][:, :].rearrange("p (h d) -> p h d", h=4)
            scale_heads(kbuf_q[:, 4 * k:4 * k + 4, :], src, 4, 128, 16 + 4 * k, G_FQ, R_pb[k], R_kbuf_q, tmp8[:, 0:512].rearrange("p (h d) -> p h d", h=4))
        transpose_out(kbuf_q[:, :, :].rearrange("p h d -> p (h d)"), 16,
                      lambda b0, b1, stg, R_stg, u=u: kb.dma("sp", qtf_d[b0:b1, :, u * 128:(u + 1) * 128].rearrange("b p s -> p b s"), stg[:, 0:b1 - b0, :], r=[R_stg], w=[R_qtf]), R_kbuf_q)
    kb.release(m_q)
    wg = kb.al([128, 16, 2048], BF16)
    R_wg = Res()
    bgb = kb.al([128, 2048], F32)
    R_bg = Res()
    gtmp = kb.al([128, 2048], F32)
    R_gtmp = Res()
    gsb = kb.al([128, 2048], BF16)
    R_gsb = Res()
    kb_stage = [kb.al([128, 8, 128], BF16) for _ in range(2)]
    R_stage = [Res(), Res()]
    for gh in range(2):
        load_w(wg, io["w_gate"], gh * 2048, (gh + 1) * 2048, R_wg)
        kb.dma("sp", bgb[:], io["bg_b"][:, gh * 2048:(gh + 1) * 2048], w=[R_bg])
        for u in range(NU):
            hTu = hTo[:, :, u * 128:(u + 1) * 128]
            for c in range(16):
                for k in range(4):
                    T(lambda e, c=c, k=k, hTu=hTu: e.matmul(out=pbk[k][:], lhsT=hTu[:, c, :], rhs=wg[:, c, k * 512:(k + 1) * 512], start=(c == 0), stop=(c == 15)), r=[R_hTo, R_wg], w=[R_pb[k]])
            for k in range(4):
                V(lambda e, k=k: e.tensor_tensor(out=gtmp[:, k * 512:(k + 1) * 512], in0=pbk[k][:], in1=bgb[:, k * 512:(k + 1) * 512], op=ALU.add), r=[R_pb[k], R_bg], w=[R_gtmp])
            A(lambda e: e.activation(out=gsb[:], in_=gtmp[:], func=AF.Sigmoid), r=[R_gtmp], w=[R_gsb])
            transpose_out(gsb, 16,
                          lambda b0, b1, stg, R_stg, u=u, gh=gh: kb.dma("sp", gt_d[gh * 16 + b0:gh * 16 + b1, :, u * 128:(u + 1) * 128].rearrange("b p s -> p b s"), stg[:, 0:b1 - b0, :], r=[R_stg], w=[R_gtd]), R_gsb)

    kb.release(m_base)
    lzf = flog[:, :, :].rearrange("p h j -> p (h j)")
    cendf = cend[:, :, :].rearrange("p h j -> p (h j)")
    onesr = kb.al([128, NT], F32)
    csel = kb.al([128, NU, NT], F32)
    cown = kb.al([128, NH, NU], F32)
    ctmp = kb.al([128, NH, NT], F32)
    R_cs = Res("cums")
    kb.dma("sp", csel[:], io["csel"], w=[R_cs])
    V(lambda e: e.tensor_tensor(out=flog[:], in0=flog[:], in1=gv[:, G_BF:G_BF + 16].unsqueeze(2).to_broadcast([128, NH, NT]), op=ALU.add), r=[R_fl, R_g], w=[R_fl])
    A(lambda e: e.activation(out=flog[:], in_=flog[:], func=AF.Exp, scale=-1.0), r=[R_fl], w=[R_fl])
    A(lambda e: e.activation(out=flog[:], in_=flog[:], func=AF.Ln, bias=1.0), r=[R_fl], w=[R_fl])
    P(lambda e: e.memset(onesr[:], 1.0), w=[R_cs])
    W = NH * NT
    for c0 in range(0, W, 512):
        c1 = min(W, c0 + 512)
        T(lambda e, c0=c0, c1=c1: e.matmul(out=pbk[0][:, 0:c1 - c0], lhsT=trif[:], rhs=lzf[:, c0:c1], start=True, stop=True), r=[R_fl, R_c], w=[R_pb[0]])
        T(lambda e, c0=c0, c1=c1: e.matmul(out=pbk[1][:, 0:c1 - c0], lhsT=onesf[:], rhs=lzf[:, c0:c1], start=True, stop=True), r=[R_fl, R_c], w=[R_pb[1]])
        V(lambda e, c0=c0, c1=c1: e.tensor_copy(out=cendf[:, c0:c1], in_=pbk[1][:, 0:c1 - c0]), r=[R_pb[1]], w=[R_cs])
        V(lambda e, c0=c0, c1=c1: e.tensor_copy(out=ctmp[:, :, :].rearrange("p h j -> p (h j)")[:, c0:c1], in_=pbk[0][:, 0:c1 - c0]), r=[R_pb[0]], w=[R_cs])
    V(lambda e: e.tensor_copy(out=flog[:], in_=cend[:]), r=[R_cs, R_fl], w=[R_fl])
    for h in range(NH):
        V(lambda e, h=h: e.tensor_tensor_scan(out=cend[:, h, :], data0=onesr[:], data1=flog[:, h, :], initial=0.0, op0=ALU.mult, op1=ALU.add), r=[R_fl, R_cs], w=[R_cs])
    V(lambda e: e.tensor_tensor(out=flog[:], in0=cend[:], in1=flog[:], op=ALU.subtract), r=[R_cs, R_fl], w=[R_fl])
    V(lambda e: e.tensor_tensor(out=flog[:], in0=flog[:], in1=ctmp[:], op=ALU.add), r=[R_cs, R_fl], w=[R_fl])
    for u in range(NU):
        V(lambda e, u=u: e.tensor_tensor(out=ctmp[:], in0=cend[:], in1=csel[:, u, :].unsqueeze(1).to_broadcast([128, NH, NT]), op=ALU.mult), r=[R_cs], w=[R_cs])
        V(lambda e, u=u: e.tensor_reduce(out=cown[:, :, u], in_=ctmp[:], axis=AX.X, op=ALU.add), r=[R_cs], w=[R_cs])

    kposf = kb.al([128, NT], F32)
    kposi = kb.al([128, NT], I32)
    qposb = kb.al([128, TO], F32)
    R_pos = Res()
    P(lambda e: e.iota(kposi[:], pattern=[[128, NT]], base=0, channel_multiplier=1), w=[R_pos])
    V(lambda e: e.tensor_copy(out=kposf[:], in_=kposi[:]), r=[R_pos], w=[R_pos])
    kb.dma("sp", qposb[:], io["qpos_b"], w=[R_pos])
    cmask = kb.al([128, NT, 128], BF16)
    R_cm = Res("cmask")
    for b8 in range(NU):
        V(lambda e, b8=b8: e.tensor_tensor(out=cmask[:, b8 * NC:(b8 + 1) * NC, :], in0=qposb[:, b8 * 128:(b8 + 1) * 128].unsqueeze(1).to_broadcast([128, NC, 128]),
                                          in1=kposf[:, b8 * NC:(b8 + 1) * NC].unsqueeze(2).to_broadcast([128, NC, 128]), op=ALU.is_ge), r=[R_pos], w=[R_cm])
    knt = [kb.al([128, S], BF16) for _ in range(2)]
    kpp = [kb.al([128, S], BF16) for _ in range(2)]
    vth = [kb.al([128, NT, 128], BF16) for _ in range(2)]
    R_k, R_kp, R_v = [Res(), Res()], [Res(), Res()], [Res(), Res()]
    qn_t = [kb.al([128, TO], BF16) for _ in range(2)]
    qpp = [kb.al([128, TO], BF16) for _ in range(2)]
    g_t = [kb.al([128, TO], BF16) for _ in range(2)]
    R_q, R_qp, R_gt = [Res(), Res()], [Res(), Res()], [Res(), Res()]
    bt = [kb.al([128, NT, NU], F32) for _ in range(2)]
    R_bt = [Res(), Res()]
    CW = min(512, TO)
    NPASS = TO // CW
    ND = 4
    sadd = [kb.al([128, CW], F32) for _ in range(ND)]
    R_sadd = [Res() for _ in range(ND)]
    pt_t = [kb.al([128, CW], BF16) for _ in range(ND)]
    R_pt = [Res() for _ in range(ND)]
    rl = kb.al([128, CW], F32)
    yt = kb.al([128, CW], F32)
    R_y = Res()
    yb = [kb.al([128, CW], BF16) for _ in range(2)]
    R_yb = [Res(), Res()]
    mt_d = io["mt_d"]
    R_mtd = Res("mt_d")
    NHH = 2 * NH
    OB, LB = 4, 5

    def load_head(hh):
        hb = hh % 2
        mla = hh < NH
        h = hh % NH
        if mla:
            kb.dma("sp", knt[hb][:], ktn_d[h], r=[R_ktn], w=[R_k[hb]])
            if h % 2 == 0:
                pb_ = (h // 2) % 2
                kb.dma("sp", kpp[pb_][:], ktp_d[h // 2], r=[R_ktp], w=[R_kp[pb_]])
                kb.dma("sp", qpp[pb_][:], qtp_d[h // 2], r=[R_qtp], w=[R_qp[pb_]])
            vsrc = vm_d[:, h * 128:(h + 1) * 128].rearrange("(j p) d -> p j d", p=128)
            kb.dma("sp", qn_t[hb][:], qtn_d[h], r=[R_qtn], w=[R_q[hb]])
        else:
            kb.dma("sp", knt[hb][:], ktf_d[h], r=[R_ktf], w=[R_k[hb]])
            vsrc = vf_d[:, h * 128:(h + 1) * 128].rearrange("(j p) d -> p j d", p=128)
            kb.dma("sp", qn_t[hb][:], qtf_d[h], r=[R_qtf], w=[R_q[hb]])
            V(lambda e, h=h, hb=hb: e.tensor_tensor(out=bt[hb][:], in0=flog[:, h, :].unsqueeze(2).to_broadcast([128, NT, NU]),
                                                  in1=cown[:, h, :].unsqueeze(1).to_broadcast([128, NT, NU]), op=ALU.subtract), r=[R_fl, R_cs], w=[R_bt[hb]])
            V(lambda e, hb=hb: e.tensor_scalar(out=bt[hb][:], in0=bt[hb][:], scalar1=0.0, scalar2=None, op0=ALU.min), r=[R_bt[hb]], w=[R_bt[hb]])
        for j0 in range(0, NT, 4):
            j1 = min(NT, j0 + 4)
            kb.dma("sp", vth[hb][:, j0:j1, :], vsrc[:, j0:j1, :], r=[R_vm, R_vf], w=[R_v[hb]])
        kb.dma("sp", g_t[hb][:], gt_d[hh], r=[R_gtd], w=[R_gt[hb]])

    def geom(ps, j):
        a0 = ps * CW
        c0 = max(a0, (j // NC) * 128)
        return a0, c0, a0 + CW

    def emit_S(idx, hh, ps, j):
        hb, sb_ = hh % 2, idx % ND
        mla = hh < NH
        h = hh % NH
        a0, c0, a1 = geom(ps, j)
        pss, Rs = pbk[sb_], R_pb[sb_]
        T(lambda e: e.matmul(out=pss[:, c0 - a0:a1 - a0], lhsT=knt[hb][:, j * 128:(j + 1) * 128], rhs=qn_t[hb][:, c0:a1], start=True, stop=(not mla)),
          r=[R_k[hb], R_q[hb]], w=[Rs], inc=(not mla))
        if mla:
            pb_ = (h // 2) % 2
            r0 = (h % 2) * 64
            T(lambda e: e.matmul(out=pss[:, c0 - a0:a1 - a0], lhsT=kpp[pb_][r0:r0 + 64, j * 128:(j + 1) * 128], rhs=qpp[pb_][r0:r0 + 64, c0:a1], start=False, stop=True),
              r=[R_kp[pb_], R_qp[pb_]], w=[Rs])

    def emit_rest(idx, hh, ps, j, first, last):
        hb, sb_ = hh % 2, idx % ND
        mla = hh < NH
        a0, c0, a1 = geom(ps, j)
        pss, Rs = pbk[sb_], R_pb[sb_]
        ptile, Rp = pt_t[sb_], R_pt[sb_]
        lo, hi = c0 - a0, a1 - a0
        if mla:
            A(lambda e: e.activation(out=ptile[:, lo:hi], in_=pss[:, lo:hi], func=AF.Exp), r=[Rs], w=[Rp])
        else:
            nu = (hi - lo) // 128
            V(lambda e: e.tensor_tensor(out=sadd[sb_][:, lo:hi].rearrange("p (u q) -> p u q", u=nu), in0=pss[:, lo:hi].rearrange("p (u q) -> p u q", u=nu),
                                        in1=bt[hb][:, j, c0 // 128:a1 // 128].unsqueeze(2).to_broadcast([128, nu, 128]), op=ALU.add), r=[Rs, R_bt[hb]], w=[R_sadd[sb_]])
            A(lambda e: e.activation(out=ptile[:, lo:hi], in_=sadd[sb_][:, lo:hi], func=AF.Exp), r=[R_sadd[sb_]], w=[Rp])
        if c0 == (j // NC) * 128:
            P(lambda e: e.tensor_tensor(out=ptile[:, lo:lo + 128], in0=ptile[:, lo:lo + 128], in1=cmask[:, j, :], op=ALU.mult), r=[Rp, R_cm], w=[Rp])
        T(lambda e: e.matmul(out=pbk[OB][:, lo:hi], lhsT=vth[hb][:, j, :], rhs=ptile[:, lo:hi], start=first, stop=last), r=[R_v[hb], Rp], w=[R_pb[OB]], inc=False)
        T(lambda e: e.matmul(out=pbk[LB][:, lo:hi], lhsT=onesb[:], rhs=ptile[:, lo:hi], start=first, stop=last), r=[R_c, Rp], w=[R_pb[LB], R_pb[OB]])
        if last:
            V(lambda e: e.reciprocal(out=rl[:], in_=pbk[LB][:, 0:CW]), r=[R_pb[LB]], w=[R_y])
            V(lambda e: e.tensor_tensor(out=yt[:], in0=pbk[OB][:, 0:CW], in1=rl[:], op=ALU.mult), r=[R_pb[OB], R_y], w=[R_y])
            yi = (hh * NPASS + ps) % 2
            P(lambda e: e.tensor_tensor(out=yb[yi][:], in0=yt[:], in1=g_t[hb][:, a0:a1], op=ALU.mult), r=[R_y, R_gt[hb]], w=[R_yb[yi]])
            kb.dma("sp", mt_d[hh][:, a0:a1], yb[yi][:], r=[R_yb[yi]], w=[R_mtd])

    steps = []
    for hh in range(NHH):
        for ps in range(NPASS):
            nj = min(NT, ((ps + 1) * CW // 128) * NC)
            for j in range(nj):
                steps.append((hh, ps, j, j == 0, j == nj - 1))
    load_head(0)
    LOOK = ND - 1
    for i in range(min(LOOK, len(steps))):
        emit_S(i, *steps[i][:3])
    for idx, (hh, ps, j, first, last) in enumerate(steps):
        if first and ps == 0 and hh + 1 < NHH:
            load_head(hh + 1)
        if idx + LOOK < len(steps):
            emit_S(idx + LOOK, *steps[idx + LOOK][:3])
        emit_rest(idx, hh, ps, j, first, last)
    kb.release(m_base)
    return {"identf": identf, "ident": ident, "nhalf": nhalf, "iota_f": iota_f, "gT2": gT2, "R_c": R_c, "R_g": R_gB, "R_mtd": R_mtd}


F_IN = lambda S: {
    "x": ([S, D], F32), "x_own": ([S // 8, D], F32), "w_kv1": ([D, 592], F32), "w_kvup": ([512, 4096], F32), "w_fk": ([D, 2048], F32), "w_fv": ([D, 2048], F32),
    "w_cq": ([D, 512], F32), "w_qup": ([512, 3072], F32), "w_fq": ([D, 2048], F32), "w_gate": ([D, 4096], F32), "gmix": ([D], F32), "gvf": ([128, G_NV], F32),
    "pos_all": ([128, S // 128], I32), "pos_own": ([128, S // 1024], I32), "csel": ([128, S // 1024, S // 128], F32), "qpos_b": ([128, S // 8], F32), "bg_b": ([128, 4096], F32),
    "w_out": ([4096, D], F32), "gffn": ([D], F32), "gffn_b": ([128, D], F32), "w_pq": ([D, D], F32), "skeys": ([16, 128, 128], F32),
    "pu": ([16384, D], F32), "pv": ([16384, D], F32)}
F_SCR = lambda S: {
    "ktn_d": ([16, 128, S], BF16), "ktp_d": ([8, 128, S], BF16), "ktf_d": ([16, 128, S], BF16), "vm_d": ([S, 2048], BF16), "vf_d": ([S, 2048], BF16),
    "ht_d": ([S // 128, 128, 16, 128], BF16), "qtn_d": ([16, 128, S // 8], BF16), "qtp_d": ([8, 128, S // 8], BF16), "qtf_d": ([16, 128, S // 8], BF16),
    "gt_d": ([32, 128, S // 8], BF16), "mt_d": ([32, 128, S // 8], BF16), "x2d": ([S // 8, D], F32), "h2d": ([S // 8, D], BF16), "pout": ([S // 8, D], F32)}


def own_tiles(core, S):
    return [core + 8 * u for u in range(S // 1024)]


def prep_F(inp, core, S):
    f = np.float32
    NT, NU = S // 128, S // 1024
    w_in = inp["w_in"][0]
    sp = np.cumsum([0, 512, 512, 64, 2048, 2048, 2048, 16, 2048, 2048])
    tiles = own_tiles(core, S)
    rows = np.concatenate([np.arange(t * 128, (t + 1) * 128) for t in tiles])
    x = np.ascontiguousarray(inp["x"][0, :S])
    gvec = np.zeros(G_NV, f)
    gvec[G_KL:G_KL + 512] = inp["mla_kv_latent_g"][0]
    gvec[G_QL:G_QL + 512] = inp["mla_q_latent_g"][0]
    gvec[G_QN:G_QN + 192] = inp["mla_q_norm_g"][0]
    gvec[G_KN:G_KN + 192] = inp["mla_k_norm_g"][0]
    gvec[G_FQ:G_FQ + 128] = inp["fox_q_norm_g"][0]
    gvec[G_FK:G_FK + 128] = inp["fox_k_norm_g"][0]
    gvec[G_BF:G_BF + 16] = inp["b_forget"][0]
    gvec[G_IF:G_IF + 32] = (10000.0 ** (-np.arange(32, dtype=np.float32) / 32)).astype(f)
    pos = inp["positions"][0, :S].astype(np.int32)
    csel = np.zeros((NU, NT), f)
    for u, t in enumerate(tiles):
        csel[u, t] = 1.0
    bc = lambda a: np.ascontiguousarray(np.broadcast_to(a, (128,) + a.shape))
    return {
        "x": x, "x_own": np.ascontiguousarray(x[rows]),
        "w_kv1": np.ascontiguousarray(np.concatenate([w_in[:, sp[1]:sp[2]], w_in[:, sp[2]:sp[3]], w_in[:, sp[6]:sp[7]]], axis=1)),
        "w_kvup": np.ascontiguousarray(inp["w_kv_up"][0]), "w_fk": np.ascontiguousarray(w_in[:, sp[4]:sp[5]]), "w_fv": np.ascontiguousarray(w_in[:, sp[5]:sp[6]]),
        "w_cq": np.ascontiguousarray(w_in[:, sp[0]:sp[1]]), "w_qup": np.ascontiguousarray(inp["w_q_up"][0]), "w_fq": np.ascontiguousarray(w_in[:, sp[3]:sp[4]]),
        "w_gate": np.ascontiguousarray(w_in[:, sp[7]:sp[9]]), "gmix": np.ascontiguousarray(inp["mix_norm_g"][0]), "gvf": bc(gvec),
        "pos_all": np.ascontiguousarray(pos.reshape(NT, 128).T), "pos_own": np.ascontiguousarray(pos[rows].reshape(NU, 128).T),
        "csel": bc(csel), "qpos_b": bc(rows.astype(f)), "bg_b": bc(inp["b_gate"][0].astype(f)),
        "w_out": np.ascontiguousarray(inp["w_out"][0]), "gffn": np.ascontiguousarray(inp["ffn_norm_g"][0]), "gffn_b": bc(inp["ffn_norm_g"][0].astype(f)),
        "w_pq": np.ascontiguousarray(inp["w_peer_q"][0]), "skeys": np.ascontiguousarray(inp["peer_sub_keys"][0].reshape(16, 128, 128)),
        "pu": np.ascontiguousarray(inp["peer_u"][0]), "pv": np.ascontiguousarray(inp["peer_v"][0]),
    }


def build_B(kb, Tn, io, R_mtin=None, shared=None):
    NT = Tn // 128
    mtin, x_own, w_out, gffn, gffn_b, w_pq, skeys = io.get("mtin"), io["x_own"], io["w_out"], io["gffn"], io["gffn_b"], io["w_pq"], io["skeys"]
    pu, pv, out, x2d, h2d, pout = io["pu"], io["pv"], io["out"], io["x2d"], io["h2d"], io["pout"]
    if R_mtin is None:
        R_mtin = Res("mtin")
    R_x2d, R_h2d, R_pout = Res("x2d"), Res("h2d"), Res("pout")
    V = lambda fn, r=(), w=(): kb.op("dve", fn, r, w)
    A = lambda fn, r=(), w=(): kb.op("act", fn, r, w)
    P = lambda fn, r=(), w=(): kb.op("pool", fn, r, w)
    T = lambda fn, r=(), w=(), inc=True: kb.op("pe", fn, r, w, pe_acc=True, inc=inc)
    pbk = kb.pbank
    R_pb = [Res("pbB%d" % i) for i in range(8)]

    if shared is None:
        identf = kb.al([128, 128], F32)
        ident = kb.al([128, 128], BF16)
        nhalf = kb.al([128, 8], F32)
        iota_i = kb.al([128, 16], I32)
        iota_f = kb.al([128, 16], F32)
        R_c = Res("constsB")
        P(lambda e: e.memset(identf[:], 0.0), w=[R_c])
        P(lambda e: e.affine_select(out=identf[:], in_=identf[:], pattern=[[-1, 128]], compare_op=ALU.not_equal,
                                    fill=1.0, base=0, channel_multiplier=1), r=[R_c], w=[R_c])
        P(lambda e: e.memset(nhalf[:], -0.5), w=[R_c])
        P(lambda e: e.iota(iota_i[:], pattern=[[1, 16]], base=0, channel_multiplier=0), w=[R_c])
        V(lambda e: e.tensor_copy(out=ident[:], in_=identf[:]), r=[R_c], w=[R_c])
        V(lambda e: e.tensor_copy(out=iota_f[:], in_=iota_i[:]), r=[R_c], w=[R_c])
        gT2 = kb.al([128, 16], F32)
        R_g = Res("gB")
        kb.dma("sp", gT2[:], gffn.rearrange("(c p) -> p c", p=128), w=[R_g], allow_slow_non_contiguous=True)
    else:
        identf, ident, nhalf, iota_f, gT2, R_c, R_g = (shared[k] for k in ("identf", "ident", "nhalf", "iota_f", "gT2", "R_c", "R_g"))

    m0 = kb.mark()
    ar2 = kb.al([128, 32 * Tn], BF16)
    R_ar2 = Res("ar2")
    mT = ar2[:, 0:32 * Tn].rearrange("p (c t) -> p c t", c=32)
    for c in range(8):
        kb.dma("sp", mT[:, c * 4:(c + 1) * 4, :], mtin[c].rearrange("h d t -> d h t"), r=[R_mtin], w=[R_ar2])
    ar3 = kb.al([128, 16384], BF16)
    R_ar3 = Res("ar3")
    wo = ar3[:, :].rearrange("p (c n) -> p c n", c=32)
    w_out_v = w_out.rearrange("(c p) n -> p c n", p=128)
    xs = [kb.al([128, 512], F32) for _ in range(2)]
    R_xs = [Res("xs0"), Res("xs1")]
    n = 0
    for ds in range(4):
        for g in range(8):
            kb.dma("pool", wo[:, 4 * g:4 * g + 4, :], w_out_v[:, 4 * g:4 * g + 4, ds * 512:(ds + 1) * 512], w=[R_ar3])
        for tt in range(NT):
            b = n % 2
            n += 1
            kb.dma("sp", xs[b][:], x_own[tt * 128:(tt + 1) * 128, ds * 512:(ds + 1) * 512], w=[R_xs[b]])
            pk = pbk[b]
            for c in range(32):
                T(lambda e, pk=pk, c=c, tt=tt: e.matmul(out=pk[:], lhsT=mT[:, c, tt * 128:(tt + 1) * 128], rhs=wo[:, c, :], start=(c == 0), stop=(c == 31)),
                  r=[R_ar2, R_ar3], w=[R_pb[b]], inc=(c == 31))
            V(lambda e, b=b, pk=pk: e.tensor_tensor(out=xs[b][:], in0=xs[b][:], in1=pk[:], op=ALU.add), r=[R_xs[b], R_pb[b]], w=[R_xs[b]])
            kb.dma("sp", x2d[tt * 128:(tt + 1) * 128, ds * 512:(ds + 1) * 512], xs[b][:], r=[R_xs[b]], w=[R_x2d])

    kb.release(m0)
    h2T = kb.al([128, 16, Tn], BF16)
    R_h2T = Res("h2T")
    i1T = kb.al([128, Tn], BF16)
    i2T = kb.al([128, Tn], F32)
    gtT = kb.al([128, Tn], F32)
    R_eiT = Res("eiT")
    m1 = kb.mark()
    gb = kb.al([128, D], F32)
    kb.dma("sp", gb[:], gffn_b, w=[R_g])
    xt = [kb.al([128, D], F32) for _ in range(2)]
    R_xt = [Res("xtB0"), Res("xtB1")]
    junk = kb.al([128, D], BF16)
    R_junk = Res("junkB")
    xn = kb.al([128, D], BF16)
    R_xn = Res("xnB")
    hrow = kb.al([128, D], BF16)
    R_hrow = Res("hrow")
    st = kb.al([128, 8], F32)
    rs = kb.al([128, 8], F32)
    R_st = Res("stB")
    pT = pbk[7][:, :].bitcast(BF16)
    pTv = pT.rearrange("p (c t) -> p c t", c=8)
    R_pT = R_pb[7]
    for tt in range(NT):
        b = tt % 2
        kb.dma("sp", xt[b][:], x2d[tt * 128:(tt + 1) * 128, :], r=[R_x2d], w=[R_xt[b]])
        A(lambda e, b=b: e.activation(out=junk[:], in_=xt[b][:], func=AF.Square, accum_out=st[:, 0:1]), r=[R_xt[b]], w=[R_junk, R_st])
        V(lambda e: e.tensor_scalar(out=rs[:, 0:1], in0=st[:, 0:1], scalar1=1.0 / D, scalar2=EPS, op0=ALU.mult, op1=ALU.add), r=[R_st], w=[R_st])
        P(lambda e: e.tensor_tensor(out=rs[:, 0:1], in0=rs[:, 0:1], in1=nhalf[:, 0:1], op=ALU.pow), r=[R_st, R_c], w=[R_st])
        V(lambda e, b=b: e.tensor_scalar(out=xn[:], in0=xt[b][:], scalar1=rs[:, 0:1], scalar2=None, op0=ALU.mult), r=[R_xt[b], R_st], w=[R_xn])
        for half in range(2):
            for c in range(8):
                cc = half * 8 + c
                T(lambda e, c=c, cc=cc: e.transpose(out=pT[:, c * 128:(c + 1) * 128], in_=xn[:, cc * 128:(cc + 1) * 128], identity=ident[:]), r=[R_xn, R_c], w=[R_pT])
            V(lambda e, half=half, tt=tt: e.tensor_tensor(out=h2T[:, half * 8:half * 8 + 8, tt * 128:(tt + 1) * 128], in0=pTv,
                                                          in1=gT2[:, half * 8:half * 8 + 8].unsqueeze(2).to_broadcast([128, 8, 128]), op=ALU.mult),
              r=[R_pT, R_g], w=[R_h2T])
        V(lambda e: e.tensor_tensor(out=hrow[:], in0=xn[:], in1=gb[:], op=ALU.mult), r=[R_xn, R_g], w=[R_hrow])
        kb.dma("sp", h2d[tt * 128:(tt + 1) * 128, :], hrow[:], r=[R_hrow], w=[R_h2d])

    kb.release(m1)
    ar3 = kb.al([128, 16 * Tn], BF16)
    skT = kb.al([128, 16, 128], BF16)
    R_ar2, R_ar3 = Res("ar2b"), Res("ar3b")
    m2 = kb.mark()
    ar2 = kb.al([128, 32768], BF16)
    R_junk = Res("junkB2")
    wpq = ar2[:, :].rearrange("p (c n) -> p c n", c=16)
    w_pq_v = w_pq.rearrange("(c p) n -> p c n", p=128)
    for c in range(16):
        kb.dma("pool", wpq[:, c, :], w_pq_v[:, c, :], w=[R_ar2])
    qT = ar3[:, 0:16 * Tn].rearrange("p (c t) -> p c t", c=16)
    skb = kb.al([128, 16, 128], BF16)
    R_sk = Res("sk")
    kb.dma("pool", skb[:], skeys.rearrange("h k d -> k h d"), w=[R_sk])
    for half in range(2):
        for c in range(8):
            T(lambda e, c=c, half=half: e.transpose(out=pT[:, c * 128:(c + 1) * 128], in_=skb[:, half * 8 + c, :], identity=ident[:]), r=[R_sk, R_c], w=[R_pT])
        V(lambda e, half=half: e.tensor_copy(out=skT[:, half * 8:half * 8 + 8, :], in_=pTv), r=[R_pT], w=[R_sk])
    W = min(512, Tn)
    n = 0
    for hc in range(16):
        for c0 in range(0, Tn, W):
            b = n % 2
            n += 1
            pk = pbk[b]
            for c in range(16):
                T(lambda e, pk=pk, c=c, hc=hc, c0=c0: e.matmul(out=pk[:, 0:W], lhsT=wpq[:, c, hc * 128:(hc + 1) * 128], rhs=h2T[:, c, c0:c0 + W], start=(c == 0), stop=(c == 15)),
                  r=[R_ar2, R_h2T], w=[R_pb[b]], inc=(c == 15))
            A(lambda e, pk=pk, hc=hc, c0=c0: e.copy(out=qT[:, hc, c0:c0 + W], in_=pk[:, 0:W]), r=[R_pb[b]], w=[R_ar3])

    kb.release(m2)
    m3 = kb.mark()
    sc = kb.al([128, 16, 128], F32)
    R_sc = Res("sc")
    wk = kb.al([128, 256], F32)
    R_wk = Res("wk")
    tv = kb.al([128, 16, 16], F32)
    ti = kb.al([128, 16, 16], U32)
    tif = kb.al([128, 16, 16], F32)
    R_tv = Res("tv")
    cand = kb.al([128, 8, 16, 16], F32)
    R_cand = Res("cand")
    cv = kb.al([128, 8, 16], F32)
    cp = kb.al([128, 8, 16], U32)
    ci = kb.al([128, 8, 16], I32)
    ikf = kb.al([128, 8, 16], F32)
    jkf = kb.al([128, 8, 16], F32)
    R_cv = Res("cv")
    eq = kb.al([128, 8, 16, 16], F32)
    R_eq = Res("eq")
    i1s = kb.al([128, 8, 16], F32)
    i2s = kb.al([128, 8, 16], F32)
    ef = kb.al([128, 128], F32)
    gt = kb.al([128, 8, 16], F32)
    zs = kb.al([128, 8], F32)
    R_sel = Res("sel")
    tvv = tv[:, :, :].rearrange("p (h c) k -> p h c k", c=2)
    tifv = tif[:, :, :].rearrange("p (h c) k -> p h c k", c=2)
    pTf = pbk[6]
    for tt in range(NT):
        for hc in range(16):
            T(lambda e, hc=hc, tt=tt: e.matmul(out=pbk[hc // 4][:, (hc % 4) * 128:(hc % 4 + 1) * 128], lhsT=qT[:, hc, tt * 128:(tt + 1) * 128], rhs=skT[:, hc, :], start=True, stop=True),
              r=[R_ar3, R_sk], w=[R_pb[hc // 4]])
        for k in range(4):
            A(lambda e, k=k: e.copy(out=sc[:, 4 * k:4 * k + 4, :], in_=pbk[k][:, :].rearrange("p (a b) -> p a b", a=4)), r=[R_pb[k]], w=[R_sc])
        for hc in range(16):
            V(lambda e, hc=hc: e.max(out=tv[:, hc, 0:8], in_=sc[:, hc, :]), r=[R_sc], w=[R_tv])
            V(lambda e, hc=hc: e.max_index(out=ti[:, hc, 0:8], in_max=tv[:, hc, 0:8], in_values=sc[:, hc, :]), r=[R_sc, R_tv], w=[R_tv])
            V(lambda e, hc=hc: e.match_replace(out=wk[:, 0:128], in_to_replace=tv[:, hc, 0:8], in_values=sc[:, hc, :], imm_value=-1e30), r=[R_sc, R_tv], w=[R_wk])
            V(lambda e, hc=hc: e.max(out=tv[:, hc, 8:16], in_=wk[:, 0:128]), r=[R_wk], w=[R_tv])
            V(lambda e, hc=hc: e.max_index(out=ti[:, hc, 8:16], in_max=tv[:, hc, 8:16], in_values=wk[:, 0:128]), r=[R_wk, R_tv], w=[R_tv])
        V(lambda e: e.tensor_copy(out=tif[:], in_=ti[:]), r=[R_tv], w=[R_tv])
        V(lambda e: e.tensor_tensor(out=cand[:], in0=tvv[:, :, 0, :].unsqueeze(3).to_broadcast([128, 8, 16, 16]),
                                    in1=tvv[:, :, 1, :].unsqueeze(2).to_broadcast([128, 8, 16, 16]), op=ALU.add), r=[R_tv], w=[R_cand])
        for h in range(8):
            cf = cand[:, h, :, :].rearrange("p a b -> p (a b)")
            V(lambda e, h=h, cf=cf: e.max(out=cv[:, h, 0:8], in_=cf), r=[R_cand], w=[R_cv])
            V(lambda e, h=h, cf=cf: e.max_index(out=cp[:, h, 0:8], in_max=cv[:, h, 0:8], in_values=cf), r=[R_cand, R_cv], w=[R_cv])
            V(lambda e, h=h, cf=cf: e.match_replace(out=wk[:], in_to_replace=cv[:, h, 0:8], in_values=cf, imm_value=-1e30), r=[R_cand, R_cv], w=[R_wk])
            V(lambda e, h=h: e.max(out=cv[:, h, 8:16], in_=wk[:]), r=[R_wk], w=[R_cv])
            V(lambda e, h=h: e.max_index(out=cp[:, h, 8:16], in_max=cv[:, h, 8:16], in_values=wk[:]), r=[R_wk, R_cv], w=[R_cv])
        V(lambda e: e.tensor_tensor(out=gt[:], in0=cv[:], in1=cv[:, :, 0:1].to_broadcast([128, 8, 16]), op=ALU.subtract), r=[R_cv], w=[R_sel])
        A(lambda e: e.activation(out=gt[:], in_=gt[:], func=AF.Exp), r=[R_sel], w=[R_sel])
        V(lambda e: e.tensor_reduce(out=zs[:], in_=gt[:], axis=AX.X, op=ALU.add), r=[R_sel], w=[R_sel])
        V(lambda e: e.reciprocal(out=zs[:], in_=zs[:]), r=[R_sel], w=[R_sel])
        V(lambda e: e.tensor_tensor(out=gt[:], in0=gt[:], in1=zs[:, :].unsqueeze(2).to_broadcast([128, 8, 16]), op=ALU.mult), r=[R_sel], w=[R_sel])
        cpi = cp[:, :, :].bitcast(I32)
        V(lambda e, cpi=cpi: e.tensor_single_scalar(out=ci[:], in_=cpi, scalar=4, op=ALU.arith_shift_right), r=[R_cv], w=[R_cv])
        V(lambda e: e.tensor_copy(out=ikf[:], in_=ci[:]), r=[R_cv], w=[R_cv])
        V(lambda e, cpi=cpi: e.tensor_single_scalar(out=ci[:], in_=cpi, scalar=15, op=ALU.bitwise_and), r=[R_cv], w=[R_cv])
        V(lambda e: e.tensor_copy(out=jkf[:], in_=ci[:]), r=[R_cv], w=[R_cv])
        io16 = iota_f[:, :].unsqueeze(1).unsqueeze(1).to_broadcast([128, 8, 16, 16])
        for sel, kf, c in ((i1s, ikf, 0), (i2s, jkf, 1)):
            V(lambda e, kf=kf: e.tensor_tensor(out=eq[:], in0=io16, in1=kf[:, :, :].unsqueeze(3).to_broadcast([128, 8, 16, 16]), op=ALU.is_equal), r=[R_cv, R_c], w=[R_eq])
            V(lambda e, c=c: e.tensor_tensor(out=eq[:], in0=eq[:], in1=tifv[:, :, c, :].unsqueeze(2).to_broadcast([128, 8, 16, 16]), op=ALU.mult), r=[R_eq, R_tv], w=[R_eq])
            V(lambda e, sel=sel: e.tensor_reduce(out=sel[:], in_=eq[:], axis=AX.X, op=ALU.add), r=[R_eq], w=[R_sel])
        T(lambda e: e.transpose(out=pTf[:, 0:128], in_=i1s[:].rearrange("p h k -> p (h k)"), identity=identf[:]), r=[R_sel, R_c], w=[R_pb[6]])
        T(lambda e: e.transpose(out=pTf[:, 128:256], in_=i2s[:].rearrange("p h k -> p (h k)"), identity=identf[:]), r=[R_sel, R_c], w=[R_pb[6]])
        T(lambda e: e.transpose(out=pTf[:, 256:384], in_=gt[:].rearrange("p h k -> p (h k)"), identity=identf[:]), r=[R_sel, R_c], w=[R_pb[6]])
        V(lambda e, tt=tt: e.tensor_copy(out=i1T[:, tt * 128:(tt + 1) * 128], in_=pTf[:, 0:128]), r=[R_pb[6]], w=[R_eiT])
        V(lambda e, tt=tt: e.tensor_copy(out=i2T[:, tt * 128:(tt + 1) * 128], in_=pTf[:, 128:256]), r=[R_pb[6]], w=[R_eiT])
        V(lambda e, tt=tt: e.tensor_copy(out=gtT[:, tt * 128:(tt + 1) * 128], in_=pTf[:, 256:384]), r=[R_pb[6]], w=[R_eiT])

    import os
    _stop = int(os.environ.get("DENSE_STOP", "9"))
    if _stop == 0:
        return
    kb.release(m1)
    TH = min(512, Tn)
    NTH = TH // 128
    iob = kb.al([128, 128], BF16)
    ioi = kb.al([128, 128], I32)
    ioc = kb.al([128, 16], F32)
    R_io = Res("iota")
    P(lambda e: e.iota(ioi[:], pattern=[[1, 128]], base=0, channel_multiplier=0), w=[R_io])
    V(lambda e: e.tensor_copy(out=iob[:], in_=ioi[:]), r=[R_io], w=[R_io])
    V(lambda e: e.tensor_copy(out=ioc[:], in_=ioi[:, 0:16]), r=[R_io], w=[R_io])
    oacc = kb.al([128, NTH, D], F32)
    R_oacc = Res("oacc")
    Gc = [kb.al([128, 16, TH], BF16) for _ in range(2)]
    R_Gc = [Res("Gc0"), Res("Gc1")]
    i2c = kb.al([128, TH], F32)
    R_i2c = Res("i2c")
    ablk = [kb.al([128, 32, 128], BF16) for _ in range(2)]
    R_ablk = [Res(), Res()]
    btmp = kb.al([128, 64, 16], F32)
    bblk = kb.al([128, 64, 16], BF16)
    R_bblk = Res("bblk")
    ub = [kb.al([128, D], BF16) for _ in range(2)]
    R_ub = [Res(), Res()]
    uT = [kb.al([128, 16, 128], BF16) for _ in range(2)]
    R_uT = [Res(), Res()]
    vb = [[kb.al([128, D], BF16) for _ in range(4)] for _ in range(2)]
    R_vb = [[Res() for _ in range(4)] for _ in range(2)]
    wg = [[kb.al([128, TH], BF16) for _ in range(4)] for _ in range(2)]
    R_wg = [[Res() for _ in range(4)] for _ in range(2)]
    ga = [kb.al([128, TH], BF16) for _ in range(2)]
    R_ga = [Res(), Res()]
    pu_v = pu.rearrange("(i c) d -> c i d", c=128)
    pv_v = pv.rearrange("(i c) d -> c i d", c=128)
    pT5s = [pbk[7][:, :].bitcast(BF16), pbk[6][:, :].bitcast(BF16)]
    pT5vs = [x_.rearrange("p (c t) -> p c t", c=8) for x_ in pT5s]
    cnt5 = {"pg": 0, "ab": 0, "po": 0, "ev": 0}
    evb = [kb.al([128, 512], F32) for _ in range(2)]
    R_evb = [Res(), Res()]
    R_oacc_t = [Res() for _ in range(16)]

    def build_G(hf, cc):
        tk0, c0, gbuf = hf * TH, cc * 16, (hf * 8 + cc) % 2
        V(lambda e: e.tensor_scalar(out=i2c[:], in0=i2T[:, tk0:tk0 + TH], scalar1=float(-c0), scalar2=None, op0=ALU.add), r=[R_eiT], w=[R_i2c])
        for sb64 in range(TH // 64):
            t64 = sb64 * 64
            V(lambda e, t64=t64: e.tensor_tensor(out=btmp[:], in0=ioc[:, :].unsqueeze(1).to_broadcast([128, 64, 16]),
                                                in1=i2c[:, t64:t64 + 64].unsqueeze(2).to_broadcast([128, 64, 16]), op=ALU.is_equal), r=[R_i2c, R_io], w=[R_bblk])
            V(lambda e, t64=t64: e.tensor_tensor(out=bblk[:], in0=btmp[:], in1=gtT[:, tk0 + t64:tk0 + t64 + 64].unsqueeze(2).to_broadcast([128, 64, 16]), op=ALU.mult),
              r=[R_bblk, R_eiT], w=[R_bblk])
            for q32 in range(2):
                t0 = t64 + q32 * 32
                ab = cnt5["ab"] % 2
                cnt5["ab"] += 1
                V(lambda e, ab=ab, t0=t0: e.tensor_tensor(out=ablk[ab][:], in0=iob[:, :].unsqueeze(1).to_broadcast([128, 32, 128]),
                                                         in1=i1T[:, tk0 + t0:tk0 + t0 + 32].unsqueeze(2).to_broadcast([128, 32, 128]), op=ALU.is_equal), r=[R_eiT, R_io], w=[R_ablk[ab]])
                pg = pbk[4 + cnt5["pg"] % 2]
                R_pg = R_pb[4 + cnt5["pg"] % 2]
                cnt5["pg"] += 1
                for t in range(32):
                    T(lambda e, pg=pg, t=t, ab=ab, q32=q32: e.matmul(out=pg[:, t * 16:(t + 1) * 16], lhsT=ablk[ab][:, t, :], rhs=bblk[:, q32 * 32 + t, :], start=True, stop=True),
                      r=[R_ablk[ab], R_bblk], w=[R_pg], inc=(t == 31))
                A(lambda e, pg=pg, t0=t0, gbuf=gbuf: e.copy(out=Gc[gbuf][:, :, t0:t0 + 32].rearrange("p c t -> p t c"), in_=pg[:, :].rearrange("p (t c) -> p t c", c=16)), r=[R_pg], w=[R_Gc[gbuf]])

    def stage1(i, hf, c):
        ubf, gp, cj = i % 2, (i // 4) % 2, i % 4
        kb.dma("pool", ub[ubf][:], pu_v[c], w=[R_ub[ubf]])
        kb.dma("pool", vb[gp][cj][:], pv_v[c], w=[R_vb[gp][cj]])
        for half in range(2):
            for k in range(8):
                kk = half * 8 + k
                T(lambda e, k=k, kk=kk, ubf=ubf, half=half: e.transpose(out=pT5s[half][:, k * 128:(k + 1) * 128], in_=ub[ubf][:, kk * 128:(kk + 1) * 128], identity=ident[:]), r=[R_ub[ubf], R_c], w=[R_pb[7 - half]], inc=(k == 7))
            A(lambda e, half=half, ubf=ubf: e.copy(out=uT[ubf][:, half * 8:half * 8 + 8, :], in_=pT5vs[half]), r=[R_pb[7 - half]], w=[R_uT[ubf]])

    def stage2(i, hf, c):
        ubf, gp, cj = i % 2, (i // 4) % 2, i % 4
        tk0, gbuf, cl = hf * TH, (hf * 8 + c // 16) % 2, c % 16
        pa, R_pa = pbk[i % 2], R_pb[i % 2]
        for k in range(16):
            T(lambda e, pa=pa, k=k, ubf=ubf: e.matmul(out=pa[:, 0:TH], lhsT=uT[ubf][:, k, :], rhs=h2T[:, k, tk0:tk0 + TH], start=(k == 0), stop=(k == 15)),
              r=[R_uT[ubf], R_h2T], w=[R_pa], inc=(k == 15))
        A(lambda e, pa=pa, ubf=ubf: e.activation(out=ga[ubf][:], in_=pa[:, 0:TH], func=AF.Gelu), r=[R_pa], w=[R_ga[ubf]])
        V(lambda e, ubf=ubf, gp=gp, cj=cj, cl=cl, gbuf=gbuf: e.tensor_tensor(out=wg[gp][cj][:], in0=ga[ubf][:], in1=Gc[gbuf][:, cl, :], op=ALU.mult), r=[R_ga[ubf], R_Gc[gbuf]], w=[R_wg[gp][cj]])

    def stage3(i, hf):
        gp = (i // 4) % 2
        for tt in range(NTH):
            for ds in range(4):
                po = pbk[2 + cnt5["po"] % 4]
                R_po = R_pb[2 + cnt5["po"] % 4]
                cnt5["po"] += 1
                for cj in range(4):
                    T(lambda e, po=po, gp=gp, cj=cj, tt=tt, ds=ds: e.matmul(out=po[:], lhsT=wg[gp][cj][:, tt * 128:(tt + 1) * 128], rhs=vb[gp][cj][:, ds * 512:(ds + 1) * 512], start=(cj == 0), stop=(cj == 3)),
                      r=[R_wg[gp][cj], R_vb[gp][cj]], w=[R_po], inc=(cj == 3))
                R_oa = R_oacc_t[tt * 4 + ds]
                if (tt * 4 + ds) % 2 == 0:
                    V(lambda e, po=po, tt=tt, ds=ds: e.tensor_tensor(out=oacc[:, tt, ds * 512:(ds + 1) * 512], in0=oacc[:, tt, ds * 512:(ds + 1) * 512], in1=po[:], op=ALU.add), r=[R_po, R_oa], w=[R_oa])
                else:
                    ei = cnt5["ev"] % 2
                    cnt5["ev"] += 1
                    A(lambda e, po=po, ei=ei: e.copy(out=evb[ei][:], in_=po[:]), r=[R_po], w=[R_evb[ei]])
                    P(lambda e, tt=tt, ds=ds, ei=ei: e.tensor_tensor(out=oacc[:, tt, ds * 512:(ds + 1) * 512], in0=oacc[:, tt, ds * 512:(ds + 1) * 512], in1=evb[ei][:], op=ALU.add), r=[R_evb[ei], R_oa], w=[R_oa])

    items = [(hf, c) for hf in range(Tn // TH) for c in range(128)]
    for tt in range(NTH):
        kb.dma("sp", oacc[:, tt, :], x2d[tt * 128:(tt + 1) * 128, :], r=[R_x2d], w=R_oacc_t[tt * 4:tt * 4 + 4])
    build_G(0, 0)
    stage1(0, *items[0])
    for i, (hf, c) in enumerate(items):
        if i + 1 < len(items):
            stage1(i + 1, *items[i + 1])
        stage2(i, hf, c)
        if c % 16 == 7 and not (hf == Tn // TH - 1 and c // 16 == 7):
            nhf, ncc = (hf, c // 16 + 1) if c // 16 < 7 else (hf + 1, 0)
            build_G(nhf, ncc)
        if i % 4 == 3:
            stage3(i, hf)
        if c == 127:
            tk0 = hf * TH
            for tt in range(NTH):
                kb.dma("sp", out[tk0 + tt * 128:tk0 + (tt + 1) * 128, :], oacc[:, tt, :], r=R_oacc_t[tt * 4:tt * 4 + 4])
            if hf + 1 < Tn // TH:
                for tt in range(NTH):
                    kb.dma("sp", oacc[:, tt, :], x2d[tk0 + TH + tt * 128:tk0 + TH + (tt + 1) * 128, :], r=[R_x2d], w=R_oacc_t[tt * 4:tt * 4 + 4])


def wout_perm():
    idx = []
    for i in range(8):
        for hh in range(4):
            base = (0 if hh < 2 else 2048) + (2 * i + hh % 2) * 128
            idx.extend(range(base, base + 128))
    return np.array(idx)


def prep_B(inp, core, Tn, S):
    f = np.float32
    t0 = core * Tn
    return {
        "x_own": np.ascontiguousarray(inp["x"][0, t0:t0 + Tn]),
        "w_out": np.ascontiguousarray(inp["w_out"][0][wout_perm()]),
        "gffn": np.ascontiguousarray(inp["ffn_norm_g"][0]),
        "gffn_b": np.ascontiguousarray(np.broadcast_to(inp["ffn_norm_g"][0], (128, D))),
        "w_pq": np.ascontiguousarray(inp["w_peer_q"][0]),
        "skeys": np.ascontiguousarray(inp["peer_sub_keys"][0].reshape(16, 128, 128)),
        "pu": np.ascontiguousarray(inp["peer_u"][0]),
        "pv": np.ascontiguousarray(inp["peer_v"][0]),
    }


S_FULL = 8192


def _build_fused(S):
    kb = KB()
    io = {}
    for k, (sh, dt) in F_IN(S).items():
        io[k] = kb.dram(k, sh, dt, kind="ExternalInput").ap()
    for k, (sh, dt) in F_SCR(S).items():
        io[k] = kb.dram(k, sh, dt).ap()
    io["out"] = kb.dram("out", [S // 8, 2048], F32, kind="ExternalOutput").ap()
    shared = build_F(kb, S, io)
    io["mtin"] = io["mt_d"].rearrange("(c h) d t -> c h d t", c=8)
    kb.arena_reset()
    build_B(kb, S // 8, io, R_mtin=shared["R_mtd"], shared=None)
    return kb.build()


def kernel(**inputs):
    inp = {k: np.asarray(v) for k, v in inputs.items()}
    S = S_FULL
    nc = _build_fused(S)
    ins = [prep_F(inp, c, S) for c in range(8)]
    res = run_bass_kernel_spmd(nc, ins, core_ids=list(range(8)))
    out = np.zeros((S, 2048), np.float32)
    for c in range(8):
        rows = np.concatenate([np.arange(t * 128, (t + 1) * 128) for t in own_tiles(c, S)])
        out[rows] = np.asarray(res.results[c]["out"])
    return out.reshape(1, S, 2048)
```

```python
import math
import numpy as np
from contextlib import ExitStack
import concourse.bass as bass
import concourse.mybir as mybir
from concourse.bass_utils import run_bass_kernel_spmd

F32 = mybir.dt.float32
BF16 = mybir.dt.bfloat16
I32 = mybir.dt.int32
U32 = mybir.dt.uint32
ALU = mybir.AluOpType
AF = mybir.ActivationFunctionType
AX = mybir.AxisListType

NDS = 40
NDS_HW = 26
ARENA_BYTES = 206 * 1024


class Res:
    __slots__ = ("name", "w", "rs")

    def __init__(self, name=""):
        self.name = name
        self.w = None
        self.rs = {}


class KB:
    ENG = ("pe", "act", "dve", "pool", "sp")

    def __init__(self):
        self.nc = bass.Bass("TRN2", target_bir_lowering=False)
        self.es = ExitStack()
        self.q = {e: [] for e in self.ENG}
        self.cnt = {e: 0 for e in self.ENG}
        self.seen = {e: {} for e in self.ENG}
        self.sem = {e: self.es.enter_context(self.nc.semaphore("s_" + e)) for e in self.ENG}
        self.dsems = [self.es.enter_context(self.nc.semaphore("d%d" % i)) for i in range(NDS)]
        self.dcnt = [0] * NDS
        self.dnext = 0
        self.dnext_sw = 0
        self.nuniq = 0
        self.big = self.es.enter_context(self.nc.sbuf_tensor("big", [128, ARENA_BYTES // 2], BF16))
        self.top = 0
        self.pbank = [self.es.enter_context(self.nc.psum_tensor("pbank%d" % i, [128, 512], F32)) for i in range(8)]

    def al(self, shape, dt, name=None, parts=None):
        esz = {F32: 4, I32: 4, U32: 4, BF16: 2}[dt]
        n = 1
        for d in shape[1:]:
            n *= d
        nbytes = (n * esz + 63) // 64 * 64
        assert self.top + nbytes <= ARENA_BYTES, ("arena overflow", self.top, nbytes, name)
        o = self.top // 2
        self.top += nbytes
        v = self.big[0:shape[0], o:o + (n * esz) // 2]
        if dt != BF16:
            v = v.bitcast(dt)
        if len(shape) == 3:
            v = v.rearrange("p (a b) -> p a b", a=shape[1])
        elif len(shape) == 4:
            v = v.rearrange("p (a b c) -> p a b c", a=shape[1], b=shape[2])
        return v

    def barrier(self):
        toks = [(o, self.sem[o], self.cnt[o]) for o in self.ENG if self.cnt[o] > 0]
        toks += [("d%d" % i, self.dsems[i], self.dcnt[i]) for i in range(NDS) if self.dcnt[i] > 0]
        for e in self.ENG:
            self._waits(e, toks)

    def arena_reset(self):
        self.barrier()
        self.top = 0

    def mark(self):
        return self.top

    def release(self, m):
        self.barrier()
        self.top = m

    def sb(self, shape, dt, name=None):
        self.nuniq += 1
        return self.es.enter_context(self.nc.sbuf_tensor(name or ("sb%d" % self.nuniq), list(shape), dt))

    def ps(self, shape, dt=F32, name=None):
        self.nuniq += 1
        return self.es.enter_context(self.nc.psum_tensor(name or ("ps%d" % self.nuniq), list(shape), dt))

    def dram(self, name, shape, dt, kind="Internal"):
        return self.nc.dram_tensor(name, list(shape), dt, kind=kind)

    def _collect(self, r, w):
        toks = []
        for x in r:
            toks.append(x.w)
        for x in w:
            toks.append(x.w)
            toks.extend(x.rs.values())
        return toks

    def _waits(self, eng, toks):
        need = {}
        seen = self.seen[eng]
        for t in toks:
            if t is None:
                continue
            k, h, v = t
            if seen.get(k, 0) >= v:
                continue
            if k not in need or need[k][1] < v:
                need[k] = (h, v)
        for k, (h, v) in need.items():
            seen[k] = v
            self.q[eng].append(lambda e, h=h, v=v: e.wait_ge(h, v))

    def _record(self, tok, r, w):
        k = tok[0]
        for x in r:
            old = x.rs.get(k)
            if old is None or old[2] < tok[2]:
                x.rs[k] = tok
        for x in w:
            x.w = tok
            x.rs = {}

    def op(self, eng, fn, r=(), w=(), pe_acc=False, inc=True):
        toks = self._collect(r, w)
        if pe_acc:
            toks = [t for t in toks if t is None or t[0] != eng]
        self._waits(eng, toks)
        h = self.sem[eng]
        if inc:
            self.cnt[eng] += 1
            n = self.cnt[eng]
            self.q[eng].append(lambda e: fn(e).then_inc(h, 1))
        else:
            n = self.cnt[eng] + 1
            self.q[eng].append(lambda e: fn(e))
        tok = (eng, h, n)
        self._record(tok, r, w)
        return tok

    def dma(self, qe, out, in_, r=(), w=(), fn=None, **kw):
        toks = self._collect(r, w)
        if qe == "pool":
            i = NDS_HW + self.dnext_sw
            self.dnext_sw = (self.dnext_sw + 1) % (NDS - NDS_HW)
        else:
            i = self.dnext
            self.dnext = (i + 1) % NDS_HW
        key = "d%d" % i
        h = self.dsems[i]
        if self.dcnt[i] > 0:
            toks.append((key, h, self.dcnt[i]))
        self._waits(qe, toks)
        self.dcnt[i] += 16
        v = self.dcnt[i]
        if fn is None:
            self.q[qe].append(lambda e: e.dma_start(out=out, in_=in_, **kw).then_inc(h, 16))
        else:
            self.q[qe].append(lambda e: fn(e).then_inc(h, 16))
        tok = (key, h, v)
        self._record(tok, r, w)
        return tok

    def wait_all_dma(self, eng="sp"):
        toks = [("d%d" % i, self.dsems[i], self.dcnt[i]) for i in range(NDS) if self.dcnt[i] > 0]
        self._waits(eng, toks)

    def build(self):
        self.wait_all_dma("sp")
        nc = self.nc
        q = self.q
        with nc.Block() as block:
            @block.tensor
            def _(e):
                for f in q["pe"]:
                    f(e)

            @block.scalar
            def _(e):
                for f in q["act"]:
                    f(e)

            @block.vector
            def _(e):
                for f in q["dve"]:
                    f(e)

            @block.gpsimd
            def _(e):
                for f in q["pool"]:
                    f(e)

            @block.sync
            def _(e):
                for f in q["sp"]:
                    f(e)
        self.es.close()
        return nc


D = 2048
EPS = 1e-6
NC = 8
NH = 16
G_KL, G_QL, G_QN, G_KN, G_FQ, G_FK, G_BF, G_IF = 0, 512, 1024, 1216, 1408, 1536, 1664, 1680
G_NV = 1712


def build_F(kb, S, io):
    NT = S // 128
    NU = NT // NC
    TO = NU * 128
    x, xo = io["x"], io["x_own"]
    V = lambda fn, r=(), w=(): kb.op("dve", fn, r, w)
    A = lambda fn, r=(), w=(): kb.op("act", fn, r, w)
    P = lambda fn, r=(), w=(): kb.op("pool", fn, r, w)
    T = lambda fn, r=(), w=(), inc=True: kb.op("pe", fn, r, w, pe_acc=True, inc=inc)
    pbk = kb.pbank
    R_pb = [Res("pb%d" % i) for i in range(8)]
    pT = pbk[7][:, :].bitcast(BF16)
    pTv = pT.rearrange("p (c t) -> p c t", c=8)
    R_pT = R_pb[7]
    pT2 = [pT, pbk[6][:, :].bitcast(BF16)]
    pT2v = [pTv, pT2[1].rearrange("p (c t) -> p c t", c=8)]
    R_pT2 = [R_pb[7], R_pb[6]]
    tcount = [0]
    ktn_d, ktp_d, ktf_d, vm_d, vf_d, ht_d = io["ktn_d"], io["ktp_d"], io["ktf_d"], io["vm_d"], io["vf_d"], io["ht_d"]
    qtn_d, qtp_d, qtf_d, gt_d = io["qtn_d"], io["qtp_d"], io["qtf_d"], io["gt_d"]
    R_ktn, R_ktp, R_ktf, R_vm, R_vf, R_htd = Res(), Res(), Res(), Res(), Res(), Res()
    R_qtn, R_qtp, R_qtf, R_gtd = Res(), Res(), Res(), Res()

    identf = kb.al([128, 128], F32)
    ident = kb.al([128, 128], BF16)
    trif = kb.al([128, 128], F32)
    onesf = kb.al([128, 128], F32)
    onesb = kb.al([128, 128], BF16)
    nhalf = kb.al([128, 16], F32)
    R_c = Res("consts")
    P(lambda e: e.memset(identf[:], 0.0), w=[R_c])
    P(lambda e: e.affine_select(out=identf[:], in_=identf[:], pattern=[[-1, 128]], compare_op=ALU.not_equal, fill=1.0, base=0, channel_multiplier=1), r=[R_c], w=[R_c])
    P(lambda e: e.memset(onesf[:], 1.0), w=[R_c])
    P(lambda e: e.affine_select(out=trif[:], in_=onesf[:], pattern=[[1, 128]], compare_op=ALU.is_ge, fill=0.0, base=0, channel_multiplier=-1), r=[R_c], w=[R_c])
    P(lambda e: e.memset(nhalf[:], -0.5), w=[R_c])
    epsc = kb.al([128, 1], F32)
    P(lambda e: e.memset(epsc[:], EPS), w=[R_c])
    V(lambda e: e.tensor_copy(out=ident[:], in_=identf[:]), r=[R_c], w=[R_c])
    V(lambda e: e.tensor_copy(out=onesb[:], in_=onesf[:]), r=[R_c], w=[R_c])
    gT = kb.al([128, 16], F32)
    gv = kb.al([128, G_NV], F32)
    R_g = Res("gains")
    kb.dma("sp", gT[:], io["gmix"].rearrange("(c p) -> p c", p=128), w=[R_g], allow_slow_non_contiguous=True)
    kb.dma("sp", gv[:], io["gvf"], w=[R_g])
    V(lambda e: e.tensor_scalar(out=gv[:, G_QN:G_QN + 192], in0=gv[:, G_QN:G_QN + 192], scalar1=192 ** -0.5, scalar2=None, op0=ALU.mult), r=[R_g], w=[R_g])
    V(lambda e: e.tensor_scalar(out=gv[:, G_FQ:G_FQ + 128], in0=gv[:, G_FQ:G_FQ + 128], scalar1=128 ** -0.5, scalar2=None, op0=ALU.mult), r=[R_g], w=[R_g])
    flog = kb.al([128, NH, NT], F32)
    cend = kb.al([128, NH, NT], F32)
    R_fl = Res("flog")
    st = kb.al([128, 64], F32)
    rs = kb.al([128, 64], F32)
    R_st = Res("st")
    iota_i = kb.al([128, 16], I32)
    iota_f = kb.al([128, 16], F32)
    gT2 = kb.al([128, 16], F32)
    R_gB = Res("gB")
    P(lambda e: e.iota(iota_i[:], pattern=[[1, 16]], base=0, channel_multiplier=0), w=[R_c])
    V(lambda e: e.tensor_copy(out=iota_f[:], in_=iota_i[:]), r=[R_c], w=[R_c])
    kb.dma("sp", gT2[:], io["gffn"].rearrange("(c p) -> p c", p=128), w=[R_gB], allow_slow_non_contiguous=True)
    m_base = kb.mark()

    def rstd(a, b, inv_n, S_=None, bias_ap=None, R_bias=None):
        st_, rs_, R_ = S_ if S_ is not None else (st, rs, R_st)
        bap = bias_ap if bias_ap is not None else epsc[:, 0:1]
        A(lambda e: e.activation(out=rs_[:, a:b], in_=st_[:, a:b], func=AF.Sqrt, scale=inv_n, bias=bap), r=[R_, R_c] + ([R_bias] if R_bias is not None else []), w=[R_])
        V(lambda e: e.reciprocal(out=rs_[:, a:b], in_=rs_[:, a:b]), r=[R_], w=[R_])

    def rope_tables(posd, n, sin_t, cos_t, R_rp):
        posi = kb.al([128, n], I32)
        posf = kb.al([128, n], F32)
        ang = kb.al([128, n, 32], F32)
        tq = kb.al([128, n, 32], F32)
        tki = kb.al([128, n, 32], I32)
        kb.dma("sp", posi[:], posd, w=[R_rp])
        V(lambda e: e.tensor_copy(out=posf[:], in_=posi[:]), r=[R_rp], w=[R_rp])
        V(lambda e: e.tensor_tensor(out=ang[:], in0=posf[:, :].unsqueeze(2).to_broadcast([128, n, 32]),
                                    in1=gv[:, G_IF:G_IF + 32].unsqueeze(1).to_broadcast([128, n, 32]), op=ALU.mult), r=[R_rp, R_g], w=[R_rp])
        for dst, shift in ((sin_t, 0.0), (cos_t, math.pi / 2)):
            V(lambda e, shift=shift: e.tensor_scalar(out=tq[:], in0=ang[:], scalar1=shift, scalar2=1.0 / (2 * math.pi), op0=ALU.add, op1=ALU.mult), r=[R_rp], w=[R_rp])
            V(lambda e: e.tensor_copy(out=tki[:], in_=tq[:]), r=[R_rp], w=[R_rp])
            V(lambda e: e.tensor_copy(out=tq[:], in_=tki[:]), r=[R_rp], w=[R_rp])
            V(lambda e: e.tensor_scalar(out=tq[:], in0=tq[:], scalar1=-2 * math.pi, scalar2=None, op0=ALU.mult), r=[R_rp], w=[R_rp])
            V(lambda e, shift=shift: e.scalar_tensor_tensor(out=tq[:], in0=ang[:], scalar=shift, in1=tq[:], op0=ALU.add, op1=ALU.add), r=[R_rp], w=[R_rp])
            V(lambda e, dst=dst: e.tensor_scalar(out=dst[:], in0=tq[:], scalar1=math.pi, scalar2=-2 * math.pi, op0=ALU.is_gt, op1=ALU.mult), r=[R_rp], w=[R_rp])
            V(lambda e, dst=dst: e.tensor_tensor(out=tq[:], in0=tq[:], in1=dst[:], op=ALU.add), r=[R_rp], w=[R_rp])
            V(lambda e: e.tensor_scalar(out=tq[:], in0=tq[:], scalar1=-3.1415925, scalar2=3.1415925, op0=ALU.max, op1=ALU.min), r=[R_rp], w=[R_rp])
            A(lambda e, dst=dst: e.activation(out=dst[:], in_=tq[:], func=AF.Sin), r=[R_rp], w=[R_rp])

    def load_w(dst, src, c0, c1, R):
        sv = src.rearrange("(c p) n -> p c n", p=128)
        n = c1 - c0
        step = 2048
        for c in range(0, 16, 4):
            for a in range(0, n, step):
                b = min(n, a + step)
                kb.dma("pool", dst[:, c:c + 4, a:b], sv[:, c:c + 4, c0 + a:c0 + b], w=[R])

    def x_to_hT(xt_b, R_xt_b, junk, R_junk, xn, R_xn, hT, R_hT, S_=None):
        st_, rs_, R_ = S_ if S_ is not None else (st, rs, R_st)
        A(lambda e: e.activation(out=junk[:], in_=xt_b[:], func=AF.Square, accum_out=st_[:, 0:1]), r=[R_xt_b], w=[R_junk, R_])
        rstd(0, 1, 1.0 / D, S_)
        V(lambda e: e.tensor_scalar(out=xn[:], in0=xt_b[:], scalar1=rs_[:, 0:1], scalar2=None, op0=ALU.mult), r=[R_xt_b, R_], w=[R_xn])
        for half in range(2):
            ti_ = tcount[0] % 2
            tcount[0] += 1
            for c in range(8):
                cc = half * 8 + c
                T(lambda e, c=c, cc=cc, ti_=ti_: e.transpose(out=pT2[ti_][:, c * 128:(c + 1) * 128], in_=xn[:, cc * 128:(cc + 1) * 128], identity=ident[:]), r=[R_xn, R_c], w=[R_pT2[ti_]], inc=(c == 7))
            V(lambda e, half=half, ti_=ti_: e.tensor_tensor(out=hT[:, half * 8:half * 8 + 8, :], in0=pT2v[ti_], in1=gT[:, half * 8:half * 8 + 8].unsqueeze(2).to_broadcast([128, 8, 128]), op=ALU.mult),
              r=[R_pT2[ti_], R_g], w=[R_hT])

    def head_norm(src, nh, dh, so, R_src, extra=None, R_extra=None):
        sq = kb_sq[:, 0:nh * dh].rearrange("p (h d) -> p h d", h=nh)
        A(lambda e: e.activation(out=sq, in_=src, func=AF.Square), r=[R_src], w=[R_sq])
        V(lambda e: e.tensor_reduce(out=st[:, so:so + nh], in_=sq, axis=AX.X, op=ALU.add), r=[R_sq], w=[R_st])
        if extra is not None:
            V(lambda e: e.tensor_scalar(out=st[:, so:so + nh], in0=st[:, so:so + nh], scalar1=extra, scalar2=None, op0=ALU.add), r=[R_st] + ([R_extra] if R_extra is not None else []), w=[R_st])

    def scale_heads(dst, src, nh, dh, so, goff, R_src, R_dst, tmp):
        V(lambda e: e.tensor_tensor(out=tmp, in0=src, in1=rs[:, so:so + nh].unsqueeze(2).to_broadcast([128, nh, dh]), op=ALU.mult), r=[R_src, R_st], w=[R_sq])
        V(lambda e: e.tensor_tensor(out=dst, in0=tmp, in1=gv[:, goff:goff + dh].unsqueeze(1).to_broadcast([128, nh, dh]), op=ALU.mult), r=[R_sq, R_g], w=[R_dst])

    def rope_heads(pe_in, pe_out, cs_row, sn_row, nh, R_in, R_out, R_rp):
        cs = cs_row.unsqueeze(1).to_broadcast([128, nh, 32])
        sn = sn_row.unsqueeze(1).to_broadcast([128, nh, 32])
        x1, x2 = pe_in[:, :, 0:32], pe_in[:, :, 32:64]
        t = [kb_pet[:, i, 0:nh, :] for i in range(4)]
        V(lambda e: e.tensor_tensor(out=t[0], in0=x1, in1=cs, op=ALU.mult), r=[R_in, R_rp], w=[R_pet])
        V(lambda e: e.tensor_tensor(out=t[1], in0=x2, in1=sn, op=ALU.mult), r=[R_in, R_rp], w=[R_pet])
        V(lambda e: e.tensor_tensor(out=t[2], in0=x1, in1=sn, op=ALU.mult), r=[R_in, R_rp], w=[R_pet])
        V(lambda e: e.tensor_tensor(out=t[3], in0=x2, in1=cs, op=ALU.mult), r=[R_in, R_rp], w=[R_pet])
        V(lambda e: e.tensor_tensor(out=pe_out[:, :, 0:32], in0=t[0], in1=t[1], op=ALU.subtract), r=[R_pet], w=[R_out])
        V(lambda e: e.tensor_tensor(out=pe_out[:, :, 32:64], in0=t[2], in1=t[3], op=ALU.add), r=[R_pet], w=[R_out])

    def transpose_out(src_flat, nblk, dst_fn, R_src, scale_col=None):
        for b0 in range(0, nblk, 8):
            b1 = min(nblk, b0 + 8)
            ti_ = tcount[0] % 2
            tcount[0] += 1
            for c in range(b1 - b0):
                T(lambda e, c=c, b0=b0, ti_=ti_: e.transpose(out=pT2[ti_][:, c * 128:(c + 1) * 128], in_=src_flat[:, (b0 + c) * 128:(b0 + c + 1) * 128], identity=ident[:]), r=[R_src, R_c], w=[R_pT2[ti_]], inc=(c == b1 - b0 - 1))
            sb_ = kb_stage_i[0] % 2
            kb_stage_i[0] += 1
            stg, R_stg = kb_stage[sb_], R_stage[sb_]
            if scale_col is None:
                A(lambda e, stg=stg, n=b1 - b0, ti_=ti_: e.copy(out=stg[:, 0:n, :], in_=pT2v[ti_][:, 0:n, :]), r=[R_pT2[ti_]], w=[R_stg])
            else:
                A(lambda e, stg=stg, n=b1 - b0, ti_=ti_: e.mul(out=stg[:, 0:n, :], in_=pT2v[ti_][:, 0:n, :], mul=scale_col[:, 0:1]), r=[R_pT2[ti_], R_g], w=[R_stg])
            dst_fn(b0, b1, stg, R_stg)

    w1 = kb.al([128, 16, 592], BF16)
    wkvu = kb.al([128, 4, 4096], BF16)
    R_w1, R_wkvu = Res(), Res()
    load_w(w1, io["w_kv1"], 0, 592, R_w1)
    wkv_v = io["w_kvup"].rearrange("(c p) n -> p c n", p=128)
    for c in range(4):
        for a in range(0, 4096, 2048):
            kb.dma("pool", wkvu[:, c, a:a + 2048], wkv_v[:, c, a:a + 2048], w=[R_wkvu])
    sin_a = kb.al([128, NT, 32], F32)
    cos_a = kb.al([128, NT, 32], F32)
    R_rpa = Res("ropeA")
    m_r = kb.mark()
    rope_tables(io["pos_all"], NT, sin_a, cos_a, R_rpa)
    kb.release(m_r)
    xt = [kb.al([128, D], F32) for _ in range(2)]
    R_xt = [Res(), Res()]
    junk = kb.al([128, D], BF16)
    R_junk = Res()
    xn = kb.al([128, D], BF16)
    R_xn = Res()
    hT = kb.al([128, 16, 128], BF16)
    R_hT = Res()
    kb_sq = kb.al([128, 2048], F32)
    R_sq = Res()
    kb_pet = kb.al([128, 4, 16, 32], F32)
    R_pet = Res()
    kb_stage = [kb.al([128, 8, 128], BF16) for _ in range(2)]
    R_stage = [Res(), Res()]
    kb_stage_i = [0]
    lat = kb.al([128, 512], BF16)
    R_lat = Res()
    latT = kb.al([128, 4, 128], BF16)
    R_latT = Res()
    kpe_f = kb.al([128, 64], F32)
    R_kpe = Res()
    kbuf = kb.al([128, 16, 128], BF16)
    R_kbuf = Res()
    pein = kb.al([128, 16, 64], F32)
    R_pein = Res()
    peout = kb.al([128, 16, 64], BF16)
    R_peout = Res()
    vbuf = kb.al([128, 16, 128], BF16)
    R_vbuf = Res()
    tmp8 = kb_sq

    gcol_kn = kb.al([128, 1], F32)
    kb.dma("sp", gcol_kn[:], io["gvf"][0:1, G_KN:G_KN + 128].rearrange("o d -> d o"), w=[R_g], allow_slow_non_contiguous=True)
    stA = kb.al([128, 8], F32)
    rsA = kb.al([128, 8], F32)
    R_stA = Res("stA")
    SA = (stA, rsA, R_stA)
    latT2 = [latT, kb.al([128, 4, 128], BF16)]
    R_latT2 = [R_latT, Res()]
    kpe2 = [kpe_f, kb.al([128, 64], F32)]
    R_kpe2 = [R_kpe, Res()]
    kb.dma("sp", xt[0][:], x[0:128, :], w=[R_xt[0]])

    def stageA(t):
        b = t % 2
        if t + 1 < NT:
            kb.dma("sp", xt[1 - b][:], x[(t + 1) * 128:(t + 2) * 128, :], w=[R_xt[1 - b]])
        x_to_hT(xt[b], R_xt[b], junk, R_junk, xn, R_xn, hT, R_hT, SA)
        kb.dma("sp", ht_d[t], hT[:], r=[R_hT], w=[R_htd])
        for c in range(16):
            T(lambda e, c=c: e.matmul(out=pbk[4][:], lhsT=hT[:, c, :], rhs=w1[:, c, 0:512], start=(c == 0), stop=(c == 15)), r=[R_hT, R_w1], w=[R_pb[4]], inc=(c == 15))
        for c in range(16):
            T(lambda e, c=c: e.matmul(out=pbk[5][:, 0:80], lhsT=hT[:, c, :], rhs=w1[:, c, 512:592], start=(c == 0), stop=(c == 15)), r=[R_hT, R_w1], w=[R_pb[5]], inc=(c == 15))
        A(lambda e: e.activation(out=junk[:, 0:512], in_=pbk[4][:], func=AF.Square, accum_out=stA[:, 0:1]), r=[R_pb[4]], w=[R_junk, R_stA])
        A(lambda e, b=b: e.activation(out=junk[:, 0:64], in_=pbk[5][:, 0:64], func=AF.Square, accum_out=stA[:, 2 + b:3 + b]), r=[R_pb[5]], w=[R_junk, R_stA])
        rstd(0, 1, 1.0 / 512, SA)
        V(lambda e, b=b: e.tensor_scalar(out=stA[:, 4 + b:5 + b], in0=stA[:, 2 + b:3 + b], scalar1=1.0 / 192, scalar2=EPS, op0=ALU.mult, op1=ALU.add), r=[R_stA], w=[R_stA])
        V(lambda e: e.scalar_tensor_tensor(out=lat[:], in0=pbk[4][:], scalar=rsA[:, 0:1], in1=gv[:, G_KL:G_KL + 512], op0=ALU.mult, op1=ALU.mult), r=[R_pb[4], R_stA, R_g], w=[R_lat])
        A(lambda e, b=b: e.copy(out=kpe2[b][:], in_=pbk[5][:, 0:64]), r=[R_pb[5]], w=[R_kpe2[b]])
        A(lambda e, t=t: e.copy(out=flog[:, :, t], in_=pbk[5][:, 64:80]), r=[R_pb[5]], w=[R_fl])
        ti_ = tcount[0] % 2
        tcount[0] += 1
        for c in range(4):
            T(lambda e, c=c, ti_=ti_: e.transpose(out=pT2[ti_][:, c * 128:(c + 1) * 128], in_=lat[:, c * 128:(c + 1) * 128], identity=ident[:]), r=[R_lat, R_c], w=[R_pT2[ti_]], inc=(c == 3))
        A(lambda e, b=b, ti_=ti_: e.copy(out=latT2[b][:], in_=pT2v[ti_][:, 0:4, :]), r=[R_pT2[ti_]], w=[R_latT2[b]])

    def stageB(t):
        b = t % 2
        for half in range(2):
            for c in range(4):
                for k in range(4):
                    T(lambda e, c=c, k=k, half=half, b=b: e.matmul(out=pbk[k][:], lhsT=latT2[b][:, c, :], rhs=wkvu[:, c, half * 2048 + k * 512:half * 2048 + (k + 1) * 512], start=(c == 0), stop=(c == 3)),
                      r=[R_latT2[b], R_wkvu], w=[R_pb[k]])
            for k in range(4):
                src = pbk[k][:, :].rearrange("p (h two d) -> p h two d", h=2, two=2)
                h0 = half * 8 + 2 * k
                head_norm(src[:, :, 0, :], 2, 128, 32 + h0, R_pb[k])
                A(lambda e, src=src, h0=h0: e.copy(out=vbuf[:, h0:h0 + 2, :], in_=src[:, :, 1, :]), r=[R_pb[k]], w=[R_vbuf])
            rstd(32 + half * 8, 32 + half * 8 + 8, 1.0 / 192, bias_ap=stA[:, 4 + b:5 + b], R_bias=R_stA)
            for k in range(4):
                src = pbk[k][:, :].rearrange("p (h two d) -> p h two d", h=2, two=2)
                h0 = half * 8 + 2 * k
                V(lambda e, src=src, h0=h0: e.tensor_tensor(out=kbuf[:, h0:h0 + 2, :], in0=src[:, :, 0, :], in1=rs[:, 32 + h0:32 + h0 + 2].unsqueeze(2).to_broadcast([128, 2, 128]), op=ALU.mult), r=[R_pb[k], R_st], w=[R_kbuf])
        transpose_out(kbuf[:, :, :].rearrange("p h d -> p (h d)"), 16,
                      lambda b0, b1, stg, R_stg, t=t: kb.dma("sp", ktn_d[b0:b1, :, t * 128:(t + 1) * 128].rearrange("b p s -> p b s"), stg[:, 0:b1 - b0, :], r=[R_stg], w=[R_ktn]), R_kbuf, scale_col=gcol_kn)
        kb.dma("sp", vm_d[t * 128:(t + 1) * 128, :], vbuf[:].rearrange("p h d -> p (h d)"), r=[R_vbuf], w=[R_vm])
        V(lambda e, b=b: e.tensor_tensor(out=pein[:], in0=kpe2[b][:, :].unsqueeze(1).to_broadcast([128, 16, 64]), in1=rs[:, 32:48].unsqueeze(2).to_broadcast([128, 16, 64]), op=ALU.mult), r=[R_kpe2[b], R_st], w=[R_pein])
        V(lambda e: e.tensor_tensor(out=pein[:], in0=pein[:], in1=gv[:, G_KN + 128:G_KN + 192].unsqueeze(1).to_broadcast([128, 16, 64]), op=ALU.mult), r=[R_pein, R_g], w=[R_pein])
        rope_heads(pein, peout, cos_a[:, t, :], sin_a[:, t, :], 16, R_pein, R_peout, R_rpa)
        transpose_out(peout[:, :, :].rearrange("p h d -> p (h d)"), 8,
                      lambda b0, b1, stg, R_stg, t=t: kb.dma("sp", ktp_d[b0:b1, :, t * 128:(t + 1) * 128].rearrange("b p s -> p b s"), stg[:, 0:b1 - b0, :], r=[R_stg], w=[R_ktp]), R_peout)

    stageA(0)
    for t in range(NT):
        if t + 1 < NT:
            stageA(t + 1)
        stageB(t)

    kb.release(m_base)
    wfk = kb.al([128, 16, 2048], BF16)
    wfv = kb.al([128, 16, 2048], BF16)
    R_wfk, R_wfv = Res(), Res()
    load_w(wfk, io["w_fk"], 0, 2048, R_wfk)
    load_w(wfv, io["w_fv"], 0, 2048, R_wfv)
    hTb = [kb.al([128, 16, 128], BF16) for _ in range(2)]
    R_hTb = [Res(), Res()]
    vbf = [kb.al([128, 2048], BF16) for _ in range(2)]
    R_vbf = [Res(), Res()]
    kb_sq = kb.al([128, 2048], F32)
    R_sq = Res()
    kb_stage = [kb.al([128, 8, 128], BF16) for _ in range(2)]
    R_stage = [Res(), Res()]
    kbuf2 = kb.al([128, 16, 128], BF16)
    R_kbuf2 = Res()
    tmp82 = kb_sq
    kb.dma("sp", hTb[0][:], ht_d[0], r=[R_htd], w=[R_hTb[0]])
    for t in range(NT):
        b = t % 2
        if t + 1 < NT:
            kb.dma("sp", hTb[1 - b][:], ht_d[t + 1], r=[R_htd], w=[R_hTb[1 - b]])
        for c in range(16):
            for k in range(4):
                T(lambda e, c=c, k=k, b=b: e.matmul(out=pbk[k][:], lhsT=hTb[b][:, c, :], rhs=wfk[:, c, k * 512:(k + 1) * 512], start=(c == 0), stop=(c == 15)), r=[R_hTb[b], R_wfk], w=[R_pb[k]])
        for k in range(4):
            src = pbk[k][:, :].rearrange("p (h d) -> p h d", h=4)
            head_norm(src, 4, 128, 16 + 4 * k, R_pb[k])
        rstd(16, 32, 1.0 / 128)
        for k in range(4):
            src = pbk[k][:, :].rearrange("p (h d) -> p h d", h=4)
            scale_heads(kbuf2[:, 4 * k:4 * k + 4, :], src, 4, 128, 16 + 4 * k, G_FK, R_pb[k], R_kbuf2, tmp82[:, 0:512].rearrange("p (h d) -> p h d", h=4))
        vbank = [4, 5, 6, 0]
        for c in range(16):
            for k in range(4):
                kk = vbank[k]
                T(lambda e, c=c, k=k, kk=kk, b=b: e.matmul(out=pbk[kk][:], lhsT=hTb[b][:, c, :], rhs=wfv[:, c, k * 512:(k + 1) * 512], start=(c == 0), stop=(c == 15)), r=[R_hTb[b], R_wfv], w=[R_pb[kk]])
        for k in range(4):
            kk = vbank[k]
            A(lambda e, k=k, kk=kk, b=b: e.copy(out=vbf[b][:, k * 512:(k + 1) * 512], in_=pbk[kk][:]), r=[R_pb[kk]], w=[R_vbf[b]])
        kb.dma("sp", vf_d[t * 128:(t + 1) * 128, :], vbf[b][:], r=[R_vbf[b]], w=[R_vf])
        transpose_out(kbuf2[:, :, :].rearrange("p h d -> p (h d)"), 16,
                      lambda b0, b1, stg, R_stg, t=t: kb.dma("sp", ktf_d[b0:b1, :, t * 128:(t + 1) * 128].rearrange("b p s -> p b s"), stg[:, 0:b1 - b0, :], r=[R_stg], w=[R_ktf]), R_kbuf2)

    kb.release(m_base)
    hTo = kb.al([128, 16, TO], BF16)
    R_hTo = Res()
    sin_o = kb.al([128, NU, 32], F32)
    cos_o = kb.al([128, NU, 32], F32)
    R_rpo = Res("ropeO")
    m_q = kb.mark()
    rope_tables(io["pos_own"], NU, sin_o, cos_o, R_rpo)
    kb.release(m_q)
    xt_q = [kb.al([128, D], F32) for _ in range(2)]
    R_xt_q = [Res(), Res()]
    junk_q = kb.al([128, D], BF16)
    R_junk_q = Res()
    xn_q = kb.al([128, D], BF16)
    R_xn_q = Res()
    hT1 = kb.al([128, 16, 128], BF16)
    R_hT1 = Res()
    for u in range(NU):
        b = u % 2
        kb.dma("sp", xt_q[b][:], xo[u * 128:(u + 1) * 128, :], w=[R_xt_q[b]])
        x_to_hT(xt_q[b], R_xt_q[b], junk_q, R_junk_q, xn_q, R_xn_q, hT1, R_hT1)
        V(lambda e, u=u: e.tensor_copy(out=hTo[:, :, u * 128:(u + 1) * 128], in_=hT1[:]), r=[R_hT1], w=[R_hTo])
    kb.release(m_q)
    wq1 = kb.al([128, 16, 512], BF16)
    wqu = kb.al([128, 4, 3072], BF16)
    wfq = kb.al([128, 16, 2048], BF16)
    R_wq1, R_wqu, R_wfq = Res(), Res(), Res()
    load_w(wq1, io["w_cq"], 0, 512, R_wq1)
    wqu_v = io["w_qup"].rearrange("(c p) n -> p c n", p=128)
    for c in range(4):
        for a in range(0, 3072, 1536):
            kb.dma("pool", wqu[:, c, a:a + 1536], wqu_v[:, c, a:a + 1536], w=[R_wqu])
    load_w(wfq, io["w_fq"], 0, 2048, R_wfq)
    junk_q = kb.al([128, D], BF16)
    R_junk_q = Res()
    kb_sq = kb.al([128, 2048], F32)
    R_sq = Res()
    kb_pet = kb.al([128, 4, 16, 32], F32)
    R_pet = Res()
    kb_stage = [kb.al([128, 8, 128], BF16) for _ in range(2)]
    R_stage = [Res(), Res()]
    lat_q = kb.al([128, 512], BF16)
    R_lat_q = Res()
    latT_q = kb.al([128, 4, 128], BF16)
    R_latT_q = Res()
    kbuf_q = kb.al([128, 16, 128], BF16)
    R_kbuf_q = Res()
    pein_q = kb.al([128, 16, 64], F32)
    R_pein_q = Res()
    peout_q = kb.al([128, 16, 64], BF16)
    R_peout_q = Res()
    tmp8 = kb_sq
    for u in range(NU):
        hTu = hTo[:, :, u * 128:(u + 1) * 128]
        for c in range(16):
            T(lambda e, c=c, hTu=hTu: e.matmul(out=pbk[6][:], lhsT=hTu[:, c, :], rhs=wq1[:, c, :], start=(c == 0), stop=(c == 15)), r=[R_hTo, R_wq1], w=[R_pb[6]])
        A(lambda e: e.activation(out=junk_q[:, 0:512], in_=pbk[6][:], func=AF.Square, accum_out=st[:, 0:1]), r=[R_pb[6]], w=[R_junk_q, R_st])
        rstd(0, 1, 1.0 / 512)
        V(lambda e: e.scalar_tensor_tensor(out=lat_q[:], in0=pbk[6][:], scalar=rs[:, 0:1], in1=gv[:, G_QL:G_QL + 512], op0=ALU.mult, op1=ALU.mult), r=[R_pb[6], R_st, R_g], w=[R_lat_q])
        for c in range(4):
            T(lambda e, c=c: e.transpose(out=pT[:, c * 128:(c + 1) * 128], in_=lat_q[:, c * 128:(c + 1) * 128], identity=ident[:]), r=[R_lat_q, R_c], w=[R_pT])
        A(lambda e: e.copy(out=latT_q[:], in_=pTv[:, 0:4, :]), r=[R_pT], w=[R_latT_q])
        for grp in range(8):
            kk = grp % 6
            for c in range(4):
                T(lambda e, c=c, grp=grp, kk=kk: e.matmul(out=pbk[kk][:, 0:384], lhsT=latT_q[:, c, :], rhs=wqu[:, c, grp * 384:(grp + 1) * 384], start=(c == 0), stop=(c == 3)), r=[R_latT_q, R_wqu], w=[R_pb[kk]])
            src = pbk[kk][:, 0:384].rearrange("p (h d) -> p h d", h=2)
            h0 = 2 * grp
            head_norm(src, 2, 192, 48, R_pb[kk])
            rstd(48, 50, 1.0 / 192)
            scale_heads(kbuf_q[:, h0:h0 + 2, :], src[:, :, 0:128], 2, 128, 48, G_QN, R_pb[kk], R_kbuf_q, tmp8[:, 0:256].rearrange("p (h d) -> p h d", h=2))
            scale_heads(pein_q[:, h0:h0 + 2, :], src[:, :, 128:192], 2, 64, 48, G_QN + 128, R_pb[kk], R_pein_q, tmp8[:, 256:384].rearrange("p (h d) -> p h d", h=2))
        transpose_out(kbuf_q[:, :, :].rearrange("p h d -> p (h d)"), 16,
                      lambda b0, b1, stg, R_stg, u=u: kb.dma("sp", qtn_d[b0:b1, :, u * 128:(u + 1) * 128].rearrange("b p s -> p b s"), stg[:, 0:b1 - b0, :], r=[R_stg], w=[R_qtn]), R_kbuf_q)
        rope_heads(pein_q, peout_q, cos_o[:, u, :], sin_o[:, u, :], 16, R_pein_q, R_peout_q, R_rpo)
        transpose_out(peout_q[:, :, :].rearrange("p h d -> p (h d)"), 8,
                      lambda b0, b1, stg, R_stg, u=u: kb.dma("sp", qtp_d[b0:b1, :, u * 128:(u + 1) * 128].rearrange("b p s -> p b s"), stg[:, 0:b1 - b0, :], r=[R_stg], w=[R_qtp]), R_peout_q)
        for c in range(16):
            for k in range(4):
                T(lambda e, c=c, k=k, hTu=hTu: e.matmul(out=pbk[k][:], lhsT=hTu[:, c, :], rhs=wfq[:, c, k * 512:(k + 1) * 512], start=(c == 0), stop=(c == 15)), r=[R_hTo, R_wfq], w=[R_pb[k]])
        for k in range(4):
            src = pbk[k][:, :].rearrange("p (h d) -> p h d", h=4)
            head_norm(src, 4, 128, 16 + 4 * k, R_pb[k])
        rstd(16, 32, 1.0 / 128)
        for k in range(4):
            src = pbk[k][:, :].rearrange("p (h d) -> p h d", h=4)
            scale_heads(kbuf_q[:, 4 * k:4 * k + 4, :], src, 4, 128, 16 + 4 * k, G_FQ, R_pb[k], R_kbuf_q, tmp8[:, 0:512].rearrange("p (h d) -> p h d", h=4))
        transpose_out(kbuf_q[:, :, :].rearrange("p h d -> p (h d)"), 16,
                      lambda b0, b1, stg, R_stg, u=u: kb.dma("sp", qtf_d[b0:b1, :, u * 128:(u + 1) * 128].rearrange("b p s -> p b s"), stg[:, 0:b1 - b0, :], r=[R_stg], w=[R_qtf]), R_kbuf_q)
    kb.release(m_q)
    wg2 = [kb.al([128, 16, 2048], BF16) for _ in range(2)]
    R_wg2 = [Res(), Res()]
    for gh in range(2):
        load_w(wg2[gh], io["w_gate"], gh * 2048, (gh + 1) * 2048, R_wg2[gh])
    bgb = kb.al([128, 2048], F32)
    R_bg = Res()
    gtmp = kb.al([128, 2048], F32)
    R_gtmp = Res()
    gsb = kb.al([128, 2048], BF16)
    R_gsb = Res()
    kb_stage = [kb.al([128, 8, 128], BF16) for _ in range(2)]
    R_stage = [Res(), Res()]
    for gh in range(2):
        wgh, R_wgh = wg2[gh], R_wg2[gh]
        kb.dma("sp", bgb[:], io["bg_b"][:, gh * 2048:(gh + 1) * 2048], w=[R_bg])
        for u in range(NU):
            hTu = hTo[:, :, u * 128:(u + 1) * 128]
            for c in range(16):
                for k in range(4):
                    T(lambda e, c=c, k=k, hTu=hTu, wgh=wgh: e.matmul(out=pbk[k][:], lhsT=hTu[:, c, :], rhs=wgh[:, c, k * 512:(k + 1) * 512], start=(c == 0), stop=(c == 15)), r=[R_hTo, R_wgh], w=[R_pb[k]])
            for k in range(4):
                V(lambda e, k=k: e.tensor_tensor(out=gtmp[:, k * 512:(k + 1) * 512], in0=pbk[k][:], in1=bgb[:, k * 512:(k + 1) * 512], op=ALU.add), r=[R_pb[k], R_bg], w=[R_gtmp])
            A(lambda e: e.activation(out=gsb[:], in_=gtmp[:], func=AF.Sigmoid), r=[R_gtmp], w=[R_gsb])
            transpose_out(gsb, 16,
                          lambda b0, b1, stg, R_stg, u=u, gh=gh: kb.dma("sp", gt_d[gh * 16 + b0:gh * 16 + b1, :, u * 128:(u + 1) * 128].rearrange("b p s -> p b s"), stg[:, 0:b1 - b0, :], r=[R_stg], w=[R_gtd]), R_gsb)

    kb.release(m_base)
    lzf = flog[:, :, :].rearrange("p h j -> p (h j)")
    cendf = cend[:, :, :].rearrange("p h j -> p (h j)")
    onesr = kb.al([128, NT], F32)
    csel = kb.al([128, NU, NT], F32)
    cown = kb.al([128, NH, NU], F32)
    ctmp = kb.al([128, NH, NT], F32)
    R_cs = Res("cums")
    kb.dma("sp", csel[:], io["csel"], w=[R_cs])
    V(lambda e: e.tensor_tensor(out=flog[:], in0=flog[:], in1=gv[:, G_BF:G_BF + 16].unsqueeze(2).to_broadcast([128, NH, NT]), op=ALU.add), r=[R_fl, R_g], w=[R_fl])
    A(lambda e: e.activation(out=flog[:], in_=flog[:], func=AF.Exp, scale=-1.0), r=[R_fl], w=[R_fl])
    A(lambda e: e.activation(out=flog[:], in_=flog[:], func=AF.Ln, bias=1.0), r=[R_fl], w=[R_fl])
    P(lambda e: e.memset(onesr[:], 1.0), w=[R_cs])
    W = NH * NT
    for c0 in range(0, W, 512):
        c1 = min(W, c0 + 512)
        T(lambda e, c0=c0, c1=c1: e.matmul(out=pbk[0][:, 0:c1 - c0], lhsT=trif[:], rhs=lzf[:, c0:c1], start=True, stop=True), r=[R_fl, R_c], w=[R_pb[0]])
        T(lambda e, c0=c0, c1=c1: e.matmul(out=pbk[1][:, 0:c1 - c0], lhsT=onesf[:], rhs=lzf[:, c0:c1], start=True, stop=True), r=[R_fl, R_c], w=[R_pb[1]])
        V(lambda e, c0=c0, c1=c1: e.tensor_copy(out=cendf[:, c0:c1], in_=pbk[1][:, 0:c1 - c0]), r=[R_pb[1]], w=[R_cs])
        V(lambda e, c0=c0, c1=c1: e.tensor_copy(out=ctmp[:, :, :].rearrange("p h j -> p (h j)")[:, c0:c1], in_=pbk[0][:, 0:c1 - c0]), r=[R_pb[0]], w=[R_cs])
    V(lambda e: e.tensor_copy(out=flog[:], in_=cend[:]), r=[R_cs, R_fl], w=[R_fl])
    for h in range(NH):
        V(lambda e, h=h: e.tensor_tensor_scan(out=cend[:, h, :], data0=onesr[:], data1=flog[:, h, :], initial=0.0, op0=ALU.mult, op1=ALU.add), r=[R_fl, R_cs], w=[R_cs])
    V(lambda e: e.tensor_tensor(out=flog[:], in0=cend[:], in1=flog[:], op=ALU.subtract), r=[R_cs, R_fl], w=[R_fl])
    V(lambda e: e.tensor_tensor(out=flog[:], in0=flog[:], in1=ctmp[:], op=ALU.add), r=[R_cs, R_fl], w=[R_fl])
    for u in range(NU):
        V(lambda e, u=u: e.tensor_tensor(out=ctmp[:], in0=cend[:], in1=csel[:, u, :].unsqueeze(1).to_broadcast([128, NH, NT]), op=ALU.mult), r=[R_cs], w=[R_cs])
        V(lambda e, u=u: e.tensor_reduce(out=cown[:, :, u], in_=ctmp[:], axis=AX.X, op=ALU.add), r=[R_cs], w=[R_cs])

    kposf = kb.al([128, NT], F32)
    kposi = kb.al([128, NT], I32)
    qposb = kb.al([128, TO], F32)
    R_pos = Res()
    P(lambda e: e.iota(kposi[:], pattern=[[128, NT]], base=0, channel_multiplier=1), w=[R_pos])
    V(lambda e: e.tensor_copy(out=kposf[:], in_=kposi[:]), r=[R_pos], w=[R_pos])
    kb.dma("sp", qposb[:], io["qpos_b"], w=[R_pos])
    cmask = kb.al([128, NT, 128], BF16)
    R_cm = Res("cmask")
    for b8 in range(NU):
        V(lambda e, b8=b8: e.tensor_tensor(out=cmask[:, b8 * NC:(b8 + 1) * NC, :], in0=qposb[:, b8 * 128:(b8 + 1) * 128].unsqueeze(1).to_broadcast([128, NC, 128]),
                                          in1=kposf[:, b8 * NC:(b8 + 1) * NC].unsqueeze(2).to_broadcast([128, NC, 128]), op=ALU.is_ge), r=[R_pos], w=[R_cm])
    knt = [kb.al([128, S], BF16) for _ in range(2)]
    kpp = [kb.al([128, S], BF16) for _ in range(2)]
    vth = [kb.al([128, NT, 128], BF16) for _ in range(2)]
    R_k, R_kp, R_v = [Res(), Res()], [Res(), Res()], [Res(), Res()]
    qn_t = [kb.al([128, TO], BF16) for _ in range(2)]
    qpp = [kb.al([128, TO], BF16) for _ in range(2)]
    g_t = [kb.al([128, TO], BF16) for _ in range(2)]
    R_q, R_qp, R_gt = [Res(), Res()], [Res(), Res()], [Res(), Res()]
    bt = [kb.al([128, NT, NU], F32) for _ in range(2)]
    R_bt = [Res(), Res()]
    CW = min(512, TO)
    NPASS = TO // CW
    ND = 4
    sadd = [kb.al([128, CW], F32) for _ in range(ND)]
    R_sadd = [Res() for _ in range(ND)]
    pt_t = [kb.al([128, CW], BF16) for _ in range(ND)]
    R_pt = [Res() for _ in range(ND)]
    rl = kb.al([128, CW], F32)
    yt = kb.al([128, CW], F32)
    R_y = Res()
    yb = [kb.al([128, CW], BF16) for _ in range(2)]
    R_yb = [Res(), Res()]
    mt_d = io["mt_d"]
    R_mtd = Res("mt_d")
    NHH = 2 * NH
    OB, LB = 4, 5

    def load_head(hh):
        hb = hh % 2
        mla = hh < NH
        h = hh % NH
        if mla:
            kb.dma("sp", knt[hb][:], ktn_d[h], r=[R_ktn], w=[R_k[hb]])
            if h % 2 == 0:
                pb_ = (h // 2) % 2
                kb.dma("sp", kpp[pb_][:], ktp_d[h // 2], r=[R_ktp], w=[R_kp[pb_]])
                kb.dma("sp", qpp[pb_][:], qtp_d[h // 2], r=[R_qtp], w=[R_qp[pb_]])
            vsrc = vm_d[:, h * 128:(h + 1) * 128].rearrange("(j p) d -> p j d", p=128)
            kb.dma("sp", qn_t[hb][:], qtn_d[h], r=[R_qtn], w=[R_q[hb]])
        else:
            kb.dma("sp", knt[hb][:], ktf_d[h], r=[R_ktf], w=[R_k[hb]])
            vsrc = vf_d[:, h * 128:(h + 1) * 128].rearrange("(j p) d -> p j d", p=128)
            kb.dma("sp", qn_t[hb][:], qtf_d[h], r=[R_qtf], w=[R_q[hb]])
            V(lambda e, h=h, hb=hb: e.tensor_tensor(out=bt[hb][:], in0=flog[:, h, :].unsqueeze(2).to_broadcast([128, NT, NU]),
                                                  in1=cown[:, h, :].unsqueeze(1).to_broadcast([128, NT, NU]), op=ALU.subtract), r=[R_fl, R_cs], w=[R_bt[hb]])
            V(lambda e, hb=hb: e.tensor_scalar(out=bt[hb][:], in0=bt[hb][:], scalar1=0.0, scalar2=None, op0=ALU.min), r=[R_bt[hb]], w=[R_bt[hb]])
        for j0 in range(0, NT, 4):
            j1 = min(NT, j0 + 4)
            kb.dma("sp", vth[hb][:, j0:j1, :], vsrc[:, j0:j1, :], r=[R_vm, R_vf], w=[R_v[hb]])
        kb.dma("sp", g_t[hb][:], gt_d[hh], r=[R_gtd], w=[R_gt[hb]])

    def geom(ps, j):
        a0 = ps * CW
        c0 = max(a0, (j // NC) * 128)
        return a0, c0, a0 + CW

    def emit_S(idx, hh, ps, j):
        hb, sb_ = hh % 2, idx % ND
        mla = hh < NH
        h = hh % NH
        a0, c0, a1 = geom(ps, j)
        pss, Rs = pbk[sb_], R_pb[sb_]
        T(lambda e: e.matmul(out=pss[:, c0 - a0:a1 - a0], lhsT=knt[hb][:, j * 128:(j + 1) * 128], rhs=qn_t[hb][:, c0:a1], start=True, stop=(not mla)),
          r=[R_k[hb], R_q[hb]], w=[Rs], inc=(not mla))
        if mla:
            pb_ = (h // 2) % 2
            r0 = (h % 2) * 64
            T(lambda e: e.matmul(out=pss[:, c0 - a0:a1 - a0], lhsT=kpp[pb_][r0:r0 + 64, j * 128:(j + 1) * 128], rhs=qpp[pb_][r0:r0 + 64, c0:a1], start=False, stop=True),
              r=[R_kp[pb_], R_qp[pb_]], w=[Rs])

    def emit_rest(idx, hh, ps, j, first, last):
        hb, sb_ = hh % 2, idx % ND
        mla = hh < NH
        a0, c0, a1 = geom(ps, j)
        pss, Rs = pbk[sb_], R_pb[sb_]
        ptile, Rp = pt_t[sb_], R_pt[sb_]
        lo, hi = c0 - a0, a1 - a0
        if mla:
            A(lambda e: e.activation(out=ptile[:, lo:hi], in_=pss[:, lo:hi], func=AF.Exp), r=[Rs], w=[Rp])
        else:
            nu = (hi - lo) // 128
            V(lambda e: e.tensor_tensor(out=sadd[sb_][:, lo:hi].rearrange("p (u q) -> p u q", u=nu), in0=pss[:, lo:hi].rearrange("p (u q) -> p u q", u=nu),
                                        in1=bt[hb][:, j, c0 // 128:a1 // 128].unsqueeze(2).to_broadcast([128, nu, 128]), op=ALU.add), r=[Rs, R_bt[hb]], w=[R_sadd[sb_]])
            A(lambda e: e.activation(out=ptile[:, lo:hi], in_=sadd[sb_][:, lo:hi], func=AF.Exp), r=[R_sadd[sb_]], w=[Rp])
        if c0 == (j // NC) * 128:
            P(lambda e: e.tensor_tensor(out=ptile[:, lo:lo + 128], in0=ptile[:, lo:lo + 128], in1=cmask[:, j, :], op=ALU.mult), r=[Rp, R_cm], w=[Rp])
        T(lambda e: e.matmul(out=pbk[OB][:, lo:hi], lhsT=vth[hb][:, j, :], rhs=ptile[:, lo:hi], start=first, stop=last), r=[R_v[hb], Rp], w=[R_pb[OB]], inc=False)
        T(lambda e: e.matmul(out=pbk[LB][:, lo:hi], lhsT=onesb[:], rhs=ptile[:, lo:hi], start=first, stop=last), r=[R_c, Rp], w=[R_pb[LB], R_pb[OB]])
        if last:
            V(lambda e: e.reciprocal(out=rl[:], in_=pbk[LB][:, 0:CW]), r=[R_pb[LB]], w=[R_y])
            V(lambda e: e.tensor_tensor(out=yt[:], in0=pbk[OB][:, 0:CW], in1=rl[:], op=ALU.mult), r=[R_pb[OB], R_y], w=[R_y])
            yi = (hh * NPASS + ps) % 2
            P(lambda e: e.tensor_tensor(out=yb[yi][:], in0=yt[:], in1=g_t[hb][:, a0:a1], op=ALU.mult), r=[R_y, R_gt[hb]], w=[R_yb[yi]])
            kb.dma("sp", mt_d[hh][:, a0:a1], yb[yi][:], r=[R_yb[yi]], w=[R_mtd])

    steps = []
    for hh in range(NHH):
        for ps in range(NPASS):
            nj = min(NT, ((ps + 1) * CW // 128) * NC)
            for j in range(nj):
                steps.append((hh, ps, j, j == 0, j == nj - 1))
    load_head(0)
    LOOK = ND - 1
    for i in range(min(LOOK, len(steps))):
        emit_S(i, *steps[i][:3])
    for idx, (hh, ps, j, first, last) in enumerate(steps):
        if first and ps == 0 and hh + 1 < NHH:
            load_head(hh + 1)
        if idx + LOOK < len(steps):
            emit_S(idx + LOOK, *steps[idx + LOOK][:3])
        emit_rest(idx, hh, ps, j, first, last)
    kb.release(m_base)
    return {"identf": identf, "ident": ident, "nhalf": nhalf, "iota_f": iota_f, "gT2": gT2, "R_c": R_c, "R_g": R_gB, "R_mtd": R_mtd}


F_IN = lambda S: {
    "x": ([S, D], F32), "x_own": ([S // 8, D], F32), "w_kv1": ([D, 592], F32), "w_kvup": ([512, 4096], F32), "w_fk": ([D, 2048], F32), "w_fv": ([D, 2048], F32),
    "w_cq": ([D, 512], F32), "w_qup": ([512, 3072], F32), "w_fq": ([D, 2048], F32), "w_gate": ([D, 4096], F32), "gmix": ([D], F32), "gvf": ([128, G_NV], F32),
    "pos_all": ([128, S // 128], I32), "pos_own": ([128, S // 1024], I32), "csel": ([128, S // 1024, S // 128], F32), "qpos_b": ([128, S // 8], F32), "bg_b": ([128, 4096], F32),
    "w_out": ([4096, D], F32), "gffn": ([D], F32), "gffn_b": ([128, D], F32), "w_pq": ([D, D], F32), "skeys": ([16, 128, 128], F32),
    "pu": ([16384, D], F32), "pv": ([16384, D], F32)}
F_SCR = lambda S: {
    "ktn_d": ([16, 128, S], BF16), "ktp_d": ([8, 128, S], BF16), "ktf_d": ([16, 128, S], BF16), "vm_d": ([S, 2048], BF16), "vf_d": ([S, 2048], BF16),
    "ht_d": ([S // 128, 128, 16, 128], BF16), "qtn_d": ([16, 128, S // 8], BF16), "qtp_d": ([8, 128, S // 8], BF16), "qtf_d": ([16, 128, S // 8], BF16),
    "gt_d": ([32, 128, S // 8], BF16), "mt_d": ([32, 128, S // 8], BF16), "x2d": ([S // 8, D], F32), "h2d": ([S // 8, D], BF16), "pout": ([S // 8, D], F32)}


def own_tiles(core, S):
    return [core + 8 * u for u in range(S // 1024)]


def prep_F(inp, core, S):
    f = np.float32
    NT, NU = S // 128, S // 1024
    w_in = inp["w_in"][0]
    sp = np.cumsum([0, 512, 512, 64, 2048, 2048, 2048, 16, 2048, 2048])
    tiles = own_tiles(core, S)
    rows = np.concatenate([np.arange(t * 128, (t + 1) * 128) for t in tiles])
    x = np.ascontiguousarray(inp["x"][0, :S])
    gvec = np.zeros(G_NV, f)
    gvec[G_KL:G_KL + 512] = inp["mla_kv_latent_g"][0]
    gvec[G_QL:G_QL + 512] = inp["mla_q_latent_g"][0]
    gvec[G_QN:G_QN + 192] = inp["mla_q_norm_g"][0]
    gvec[G_KN:G_KN + 192] = inp["mla_k_norm_g"][0]
    gvec[G_FQ:G_FQ + 128] = inp["fox_q_norm_g"][0]
    gvec[G_FK:G_FK + 128] = inp["fox_k_norm_g"][0]
    gvec[G_BF:G_BF + 16] = inp["b_forget"][0]
    gvec[G_IF:G_IF + 32] = (10000.0 ** (-np.arange(32, dtype=np.float32) / 32)).astype(f)
    pos = inp["positions"][0, :S].astype(np.int32)
    csel = np.zeros((NU, NT), f)
    for u, t in enumerate(tiles):
        csel[u, t] = 1.0
    bc = lambda a: np.ascontiguousarray(np.broadcast_to(a, (128,) + a.shape))
    return {
        "x": x, "x_own": np.ascontiguousarray(x[rows]),
        "w_kv1": np.ascontiguousarray(np.concatenate([w_in[:, sp[1]:sp[2]], w_in[:, sp[2]:sp[3]], w_in[:, sp[6]:sp[7]]], axis=1)),
        "w_kvup": np.ascontiguousarray(inp["w_kv_up"][0]), "w_fk": np.ascontiguousarray(w_in[:, sp[4]:sp[5]]), "w_fv": np.ascontiguousarray(w_in[:, sp[5]:sp[6]]),
        "w_cq": np.ascontiguousarray(w_in[:, sp[0]:sp[1]]), "w_qup": np.ascontiguousarray(inp["w_q_up"][0]), "w_fq": np.ascontiguousarray(w_in[:, sp[3]:sp[4]]),
        "w_gate": np.ascontiguousarray(w_in[:, sp[7]:sp[9]]), "gmix": np.ascontiguousarray(inp["mix_norm_g"][0]), "gvf": bc(gvec),
        "pos_all": np.ascontiguousarray(pos.reshape(NT, 128).T), "pos_own": np.ascontiguousarray(pos[rows].reshape(NU, 128).T),
        "csel": bc(csel), "qpos_b": bc(rows.astype(f)), "bg_b": bc(inp["b_gate"][0].astype(f)),
        "w_out": np.ascontiguousarray(inp["w_out"][0]), "gffn": np.ascontiguousarray(inp["ffn_norm_g"][0]), "gffn_b": bc(inp["ffn_norm_g"][0].astype(f)),
        "w_pq": np.ascontiguousarray(inp["w_peer_q"][0]), "skeys": np.ascontiguousarray(inp["peer_sub_keys"][0].reshape(16, 128, 128)),
        "pu": np.ascontiguousarray(inp["peer_u"][0]), "pv": np.ascontiguousarray(inp["peer_v"][0]),
    }


def build_B(kb, Tn, io, R_mtin=None, shared=None):
    NT = Tn // 128
    mtin, x_own, w_out, gffn, gffn_b, w_pq, skeys = io.get("mtin"), io["x_own"], io["w_out"], io["gffn"], io["gffn_b"], io["w_pq"], io["skeys"]
    pu, pv, out, x2d, h2d, pout = io["pu"], io["pv"], io["out"], io["x2d"], io["h2d"], io["pout"]
    if R_mtin is None:
        R_mtin = Res("mtin")
    R_x2d, R_h2d, R_pout = Res("x2d"), Res("h2d"), Res("pout")
    V = lambda fn, r=(), w=(): kb.op("dve", fn, r, w)
    A = lambda fn, r=(), w=(): kb.op("act", fn, r, w)
    P = lambda fn, r=(), w=(): kb.op("pool", fn, r, w)
    T = lambda fn, r=(), w=(), inc=True: kb.op("pe", fn, r, w, pe_acc=True, inc=inc)
    pbk = kb.pbank
    R_pb = [Res("pbB%d" % i) for i in range(8)]

    if shared is None:
        identf = kb.al([128, 128], F32)
        ident = kb.al([128, 128], BF16)
        nhalf = kb.al([128, 8], F32)
        iota_i = kb.al([128, 16], I32)
        iota_f = kb.al([128, 16], F32)
        R_c = Res("constsB")
        P(lambda e: e.memset(identf[:], 0.0), w=[R_c])
        P(lambda e: e.affine_select(out=identf[:], in_=identf[:], pattern=[[-1, 128]], compare_op=ALU.not_equal,
                                    fill=1.0, base=0, channel_multiplier=1), r=[R_c], w=[R_c])
        P(lambda e: e.memset(nhalf[:], -0.5), w=[R_c])
        P(lambda e: e.iota(iota_i[:], pattern=[[1, 16]], base=0, channel_multiplier=0), w=[R_c])
        V(lambda e: e.tensor_copy(out=ident[:], in_=identf[:]), r=[R_c], w=[R_c])
        V(lambda e: e.tensor_copy(out=iota_f[:], in_=iota_i[:]), r=[R_c], w=[R_c])
        gT2 = kb.al([128, 16], F32)
        R_g = Res("gB")
        kb.dma("sp", gT2[:], gffn.rearrange("(c p) -> p c", p=128), w=[R_g], allow_slow_non_contiguous=True)
    else:
        identf, ident, nhalf, iota_f, gT2, R_c, R_g = (shared[k] for k in ("identf", "ident", "nhalf", "iota_f", "gT2", "R_c", "R_g"))

    m0 = kb.mark()
    ar2 = kb.al([128, 32 * Tn], BF16)
    R_ar2 = Res("ar2")
    mT = ar2[:, 0:32 * Tn].rearrange("p (c t) -> p c t", c=32)
    for c in range(8):
        kb.dma("sp", mT[:, c * 4:(c + 1) * 4, :], mtin[c].rearrange("h d t -> d h t"), r=[R_mtin], w=[R_ar2])
    ar3 = kb.al([128, 16384], BF16)
    R_ar3 = Res("ar3")
    wo = ar3[:, :].rearrange("p (c n) -> p c n", c=32)
    w_out_v = w_out.rearrange("(c p) n -> p c n", p=128)
    xs = [kb.al([128, 512], F32) for _ in range(2)]
    R_xs = [Res("xs0"), Res("xs1")]
    n = 0
    for ds in range(4):
        for g in range(8):
            kb.dma("pool", wo[:, 4 * g:4 * g + 4, :], w_out_v[:, 4 * g:4 * g + 4, ds * 512:(ds + 1) * 512], w=[R_ar3])
        for tt in range(NT):
            b = n % 2
            n += 1
            kb.dma("sp", xs[b][:], x_own[tt * 128:(tt + 1) * 128, ds * 512:(ds + 1) * 512], w=[R_xs[b]])
            pk = pbk[b]
            for c in range(32):
                T(lambda e, pk=pk, c=c, tt=tt: e.matmul(out=pk[:], lhsT=mT[:, c, tt * 128:(tt + 1) * 128], rhs=wo[:, c, :], start=(c == 0), stop=(c == 31)),
                  r=[R_ar2, R_ar3], w=[R_pb[b]], inc=(c == 31))
            V(lambda e, b=b, pk=pk: e.tensor_tensor(out=xs[b][:], in0=xs[b][:], in1=pk[:], op=ALU.add), r=[R_xs[b], R_pb[b]], w=[R_xs[b]])
            kb.dma("sp", x2d[tt * 128:(tt + 1) * 128, ds * 512:(ds + 1) * 512], xs[b][:], r=[R_xs[b]], w=[R_x2d])

    kb.release(m0)
    h2T = kb.al([128, 16, Tn], BF16)
    R_h2T = Res("h2T")
    i1T = kb.al([128, Tn], BF16)
    i2T = kb.al([128, Tn], F32)
    gtT = kb.al([128, Tn], F32)
    R_eiT = Res("eiT")
    m1 = kb.mark()
    gb = kb.al([128, D], F32)
    kb.dma("sp", gb[:], gffn_b, w=[R_g])
    xt = [kb.al([128, D], F32) for _ in range(2)]
    R_xt = [Res("xtB0"), Res("xtB1")]
    junk = kb.al([128, D], BF16)
    R_junk = Res("junkB")
    xn = kb.al([128, D], BF16)
    R_xn = Res("xnB")
    hrow = kb.al([128, D], BF16)
    R_hrow = Res("hrow")
    st = kb.al([128, 8], F32)
    rs = kb.al([128, 8], F32)
    R_st = Res("stB")
    pT = pbk[7][:, :].bitcast(BF16)
    pTv = pT.rearrange("p (c t) -> p c t", c=8)
    R_pT = R_pb[7]
    for tt in range(NT):
        b = tt % 2
        kb.dma("sp", xt[b][:], x2d[tt * 128:(tt + 1) * 128, :], r=[R_x2d], w=[R_xt[b]])
        A(lambda e, b=b: e.activation(out=junk[:], in_=xt[b][:], func=AF.Square, accum_out=st[:, 0:1]), r=[R_xt[b]], w=[R_junk, R_st])
        V(lambda e: e.tensor_scalar(out=rs[:, 0:1], in0=st[:, 0:1], scalar1=1.0 / D, scalar2=EPS, op0=ALU.mult, op1=ALU.add), r=[R_st], w=[R_st])
        P(lambda e: e.tensor_tensor(out=rs[:, 0:1], in0=rs[:, 0:1], in1=nhalf[:, 0:1], op=ALU.pow), r=[R_st, R_c], w=[R_st])
        V(lambda e, b=b: e.tensor_scalar(out=xn[:], in0=xt[b][:], scalar1=rs[:, 0:1], scalar2=None, op0=ALU.mult), r=[R_xt[b], R_st], w=[R_xn])
        for half in range(2):
            for c in range(8):
                cc = half * 8 + c
                T(lambda e, c=c, cc=cc: e.transpose(out=pT[:, c * 128:(c + 1) * 128], in_=xn[:, cc * 128:(cc + 1) * 128], identity=ident[:]), r=[R_xn, R_c], w=[R_pT])
            V(lambda e, half=half, tt=tt: e.tensor_tensor(out=h2T[:, half * 8:half * 8 + 8, tt * 128:(tt + 1) * 128], in0=pTv,
                                                          in1=gT2[:, half * 8:half * 8 + 8].unsqueeze(2).to_broadcast([128, 8, 128]), op=ALU.mult),
              r=[R_pT, R_g], w=[R_h2T])
        V(lambda e: e.tensor_tensor(out=hrow[:], in0=xn[:], in1=gb[:], op=ALU.mult), r=[R_xn, R_g], w=[R_hrow])
        kb.dma("sp", h2d[tt * 128:(tt + 1) * 128, :], hrow[:], r=[R_hrow], w=[R_h2d])

    kb.release(m1)
    ar3 = kb.al([128, 16 * Tn], BF16)
    skT = kb.al([128, 16, 128], BF16)
    R_ar2, R_ar3 = Res("ar2b"), Res("ar3b")
    m2 = kb.mark()
    ar2 = kb.al([128, 32768], BF16)
    R_junk = Res("junkB2")
    wpq = ar2[:, :].rearrange("p (c n) -> p c n", c=16)
    w_pq_v = w_pq.rearrange("(c p) n -> p c n", p=128)
    for c in range(16):
        kb.dma("pool", wpq[:, c, :], w_pq_v[:, c, :], w=[R_ar2])
    qT = ar3[:, 0:16 * Tn].rearrange("p (c t) -> p c t", c=16)
    skb = kb.al([128, 16, 128], BF16)
    R_sk = Res("sk")
    kb.dma("pool", skb[:], skeys.rearrange("h k d -> k h d"), w=[R_sk])
    for half in range(2):
        for c in range(8):
            T(lambda e, c=c, half=half: e.transpose(out=pT[:, c * 128:(c + 1) * 128], in_=skb[:, half * 8 + c, :], identity=ident[:]), r=[R_sk, R_c], w=[R_pT])
        V(lambda e, half=half: e.tensor_copy(out=skT[:, half * 8:half * 8 + 8, :], in_=pTv), r=[R_pT], w=[R_sk])
    W = min(512, Tn)
    n = 0
    for hc in range(16):
        for c0 in range(0, Tn, W):
            b = n % 2
            n += 1
            pk = pbk[b]
            for c in range(16):
                T(lambda e, pk=pk, c=c, hc=hc, c0=c0: e.matmul(out=pk[:, 0:W], lhsT=wpq[:, c, hc * 128:(hc + 1) * 128], rhs=h2T[:, c, c0:c0 + W], start=(c == 0), stop=(c == 15)),
                  r=[R_ar2, R_h2T], w=[R_pb[b]], inc=(c == 15))
            A(lambda e, pk=pk, hc=hc, c0=c0: e.copy(out=qT[:, hc, c0:c0 + W], in_=pk[:, 0:W]), r=[R_pb[b]], w=[R_ar3])

    kb.release(m2)
    m3 = kb.mark()
    sc = kb.al([128, 16, 128], F32)
    R_sc = Res("sc")
    wk = kb.al([128, 256], F32)
    R_wk = Res("wk")
    tv = kb.al([128, 16, 16], F32)
    ti = kb.al([128, 16, 16], U32)
    tif = kb.al([128, 16, 16], F32)
    R_tv = Res("tv")
    cand = kb.al([128, 8, 16, 16], F32)
    R_cand = Res("cand")
    cv = kb.al([128, 8, 16], F32)
    cp = kb.al([128, 8, 16], U32)
    ci = kb.al([128, 8, 16], I32)
    ikf = kb.al([128, 8, 16], F32)
    jkf = kb.al([128, 8, 16], F32)
    R_cv = Res("cv")
    eq = kb.al([128, 8, 16, 16], F32)
    R_eq = Res("eq")
    i1s = kb.al([128, 8, 16], F32)
    i2s = kb.al([128, 8, 16], F32)
    ef = kb.al([128, 128], F32)
    gt = kb.al([128, 8, 16], F32)
    zs = kb.al([128, 8], F32)
    R_sel = Res("sel")
    tvv = tv[:, :, :].rearrange("p (h c) k -> p h c k", c=2)
    tifv = tif[:, :, :].rearrange("p (h c) k -> p h c k", c=2)
    pTf = pbk[6]
    for tt in range(NT):
        for hc in range(16):
            T(lambda e, hc=hc, tt=tt: e.matmul(out=pbk[hc // 4][:, (hc % 4) * 128:(hc % 4 + 1) * 128], lhsT=qT[:, hc, tt * 128:(tt + 1) * 128], rhs=skT[:, hc, :], start=True, stop=True),
              r=[R_ar3, R_sk], w=[R_pb[hc // 4]])
        for k in range(4):
            A(lambda e, k=k: e.copy(out=sc[:, 4 * k:4 * k + 4, :], in_=pbk[k][:, :].rearrange("p (a b) -> p a b", a=4)), r=[R_pb[k]], w=[R_sc])
        for hc in range(16):
            V(lambda e, hc=hc: e.max(out=tv[:, hc, 0:8], in_=sc[:, hc, :]), r=[R_sc], w=[R_tv])
            V(lambda e, hc=hc: e.max_index(out=ti[:, hc, 0:8], in_max=tv[:, hc, 0:8], in_values=sc[:, hc, :]), r=[R_sc, R_tv], w=[R_tv])
            V(lambda e, hc=hc: e.match_replace(out=wk[:, 0:128], in_to_replace=tv[:, hc, 0:8], in_values=sc[:, hc, :], imm_value=-1e30), r=[R_sc, R_tv], w=[R_wk])
            V(lambda e, hc=hc: e.max(out=tv[:, hc, 8:16], in_=wk[:, 0:128]), r=[R_wk], w=[R_tv])
            V(lambda e, hc=hc: e.max_index(out=ti[:, hc, 8:16], in_max=tv[:, hc, 8:16], in_values=wk[:, 0:128]), r=[R_wk, R_tv], w=[R_tv])
        V(lambda e: e.tensor_copy(out=tif[:], in_=ti[:]), r=[R_tv], w=[R_tv])
        V(lambda e: e.tensor_tensor(out=cand[:], in0=tvv[:, :, 0, :].unsqueeze(3).to_broadcast([128, 8, 16, 16]),
                                    in1=tvv[:, :, 1, :].unsqueeze(2).to_broadcast([128, 8, 16, 16]), op=ALU.add), r=[R_tv], w=[R_cand])
        for h in range(8):
            cf = cand[:, h, :, :].rearrange("p a b -> p (a b)")
            V(lambda e, h=h, cf=cf: e.max(out=cv[:, h, 0:8], in_=cf), r=[R_cand], w=[R_cv])
            V(lambda e, h=h, cf=cf: e.max_index(out=cp[:, h, 0:8], in_max=cv[:, h, 0:8], in_values=cf), r=[R_cand, R_cv], w=[R_cv])
            V(lambda e, h=h, cf=cf: e.match_replace(out=wk[:], in_to_replace=cv[:, h, 0:8], in_values=cf, imm_value=-1e30), r=[R_cand, R_cv], w=[R_wk])
            V(lambda e, h=h: e.max(out=cv[:, h, 8:16], in_=wk[:]), r=[R_wk], w=[R_cv])
            V(lambda e, h=h: e.max_index(out=cp[:, h, 8:16], in_max=cv[:, h, 8:16], in_values=wk[:]), r=[R_wk, R_cv], w=[R_cv])
        V(lambda e: e.tensor_tensor(out=gt[:], in0=cv[:], in1=cv[:, :, 0:1].to_broadcast([128, 8, 16]), op=ALU.subtract), r=[R_cv], w=[R_sel])
        A(lambda e: e.activation(out=gt[:], in_=gt[:], func=AF.Exp), r=[R_sel], w=[R_sel])
        V(lambda e: e.tensor_reduce(out=zs[:], in_=gt[:], axis=AX.X, op=ALU.add), r=[R_sel], w=[R_sel])
        V(lambda e: e.reciprocal(out=zs[:], in_=zs[:]), r=[R_sel], w=[R_sel])
        V(lambda e: e.tensor_tensor(out=gt[:], in0=gt[:], in1=zs[:, :].unsqueeze(2).to_broadcast([128, 8, 16]), op=ALU.mult), r=[R_sel], w=[R_sel])
        cpi = cp[:, :, :].bitcast(I32)
        V(lambda e, cpi=cpi: e.tensor_single_scalar(out=ci[:], in_=cpi, scalar=4, op=ALU.arith_shift_right), r=[R_cv], w=[R_cv])
        V(lambda e: e.tensor_copy(out=ikf[:], in_=ci[:]), r=[R_cv], w=[R_cv])
        V(lambda e, cpi=cpi: e.tensor_single_scalar(out=ci[:], in_=cpi, scalar=15, op=ALU.bitwise_and), r=[R_cv], w=[R_cv])
        V(lambda e: e.tensor_copy(out=jkf[:], in_=ci[:]), r=[R_cv], w=[R_cv])
        io16 = iota_f[:, :].unsqueeze(1).unsqueeze(1).to_broadcast([128, 8, 16, 16])
        for sel, kf, c in ((i1s, ikf, 0), (i2s, jkf, 1)):
            V(lambda e, kf=kf: e.tensor_tensor(out=eq[:], in0=io16, in1=kf[:, :, :].unsqueeze(3).to_broadcast([128, 8, 16, 16]), op=ALU.is_equal), r=[R_cv, R_c], w=[R_eq])
            V(lambda e, c=c: e.tensor_tensor(out=eq[:], in0=eq[:], in1=tifv[:, :, c, :].unsqueeze(2).to_broadcast([128, 8, 16, 16]), op=ALU.mult), r=[R_eq, R_tv], w=[R_eq])
            V(lambda e, sel=sel: e.tensor_reduce(out=sel[:], in_=eq[:], axis=AX.X, op=ALU.add), r=[R_eq], w=[R_sel])
        T(lambda e: e.transpose(out=pTf[:, 0:128], in_=i1s[:].rearrange("p h k -> p (h k)"), identity=identf[:]), r=[R_sel, R_c], w=[R_pb[6]])
        T(lambda e: e.transpose(out=pTf[:, 128:256], in_=i2s[:].rearrange("p h k -> p (h k)"), identity=identf[:]), r=[R_sel, R_c], w=[R_pb[6]])
        T(lambda e: e.transpose(out=pTf[:, 256:384], in_=gt[:].rearrange("p h k -> p (h k)"), identity=identf[:]), r=[R_sel, R_c], w=[R_pb[6]])
        V(lambda e, tt=tt: e.tensor_copy(out=i1T[:, tt * 128:(tt + 1) * 128], in_=pTf[:, 0:128]), r=[R_pb[6]], w=[R_eiT])
        V(lambda e, tt=tt: e.tensor_copy(out=i2T[:, tt * 128:(tt + 1) * 128], in_=pTf[:, 128:256]), r=[R_pb[6]], w=[R_eiT])
        V(lambda e, tt=tt: e.tensor_copy(out=gtT[:, tt * 128:(tt + 1) * 128], in_=pTf[:, 256:384]), r=[R_pb[6]], w=[R_eiT])

    import os
    _stop = int(os.environ.get("DENSE_STOP", "9"))
    if _stop == 0:
        return
    kb.release(m1)
    TH = min(512, Tn)
    NTH = TH // 128
    iob = kb.al([128, 128], BF16)
    ioi = kb.al([128, 128], I32)
    ioc = kb.al([128, 16], F32)
    R_io = Res("iota")
    P(lambda e: e.iota(ioi[:], pattern=[[1, 128]], base=0, channel_multiplier=0), w=[R_io])
    V(lambda e: e.tensor_copy(out=iob[:], in_=ioi[:]), r=[R_io], w=[R_io])
    V(lambda e: e.tensor_copy(out=ioc[:], in_=ioi[:, 0:16]), r=[R_io], w=[R_io])
    oacc = kb.al([128, NTH, D], F32)
    R_oacc = Res("oacc")
    Gc = [kb.al([128, 16, TH], BF16) for _ in range(2)]
    R_Gc = [Res("Gc0"), Res("Gc1")]
    i2c = kb.al([128, TH], F32)
    R_i2c = Res("i2c")
    ablk = [kb.al([128, 32, 128], BF16) for _ in range(2)]
    R_ablk = [Res(), Res()]
    btmp = kb.al([128, 64, 16], F32)
    bblk = kb.al([128, 64, 16], BF16)
    R_bblk = Res("bblk")
    ub = [kb.al([128, D], BF16) for _ in range(2)]
    R_ub = [Res(), Res()]
    uT = [kb.al([128, 16, 128], BF16) for _ in range(2)]
    R_uT = [Res(), Res()]
    vb = [[kb.al([128, D], BF16) for _ in range(4)] for _ in range(2)]
    R_vb = [[Res() for _ in range(4)] for _ in range(2)]
    wg = [[kb.al([128, TH], BF16) for _ in range(4)] for _ in range(2)]
    R_wg = [[Res() for _ in range(4)] for _ in range(2)]
    ga = [kb.al([128, TH], BF16) for _ in range(2)]
    R_ga = [Res(), Res()]
    pu_v = pu.rearrange("(i c) d -> c i d", c=128)
    pv_v = pv.rearrange("(i c) d -> c i d", c=128)
    pT5s = [pbk[7][:, :].bitcast(BF16), pbk[6][:, :].bitcast(BF16)]
    pT5vs = [x_.rearrange("p (c t) -> p c t", c=8) for x_ in pT5s]
    cnt5 = {"pg": 0, "ab": 0, "po": 0, "ev": 0}
    evb = [kb.al([128, 512], F32) for _ in range(2)]
    R_evb = [Res(), Res()]
    R_oacc_t = [Res() for _ in range(16)]

    def build_G(hf, cc):
        tk0, c0, gbuf = hf * TH, cc * 16, (hf * 8 + cc) % 2
        V(lambda e: e.tensor_scalar(out=i2c[:], in0=i2T[:, tk0:tk0 + TH], scalar1=float(-c0), scalar2=None, op0=ALU.add), r=[R_eiT], w=[R_i2c])
        for sb64 in range(TH // 64):
            t64 = sb64 * 64
            V(lambda e, t64=t64: e.tensor_tensor(out=btmp[:], in0=ioc[:, :].unsqueeze(1).to_broadcast([128, 64, 16]),
                                                in1=i2c[:, t64:t64 + 64].unsqueeze(2).to_broadcast([128, 64, 16]), op=ALU.is_equal), r=[R_i2c, R_io], w=[R_bblk])
            V(lambda e, t64=t64: e.tensor_tensor(out=bblk[:], in0=btmp[:], in1=gtT[:, tk0 + t64:tk0 + t64 + 64].unsqueeze(2).to_broadcast([128, 64, 16]), op=ALU.mult),
              r=[R_bblk, R_eiT], w=[R_bblk])
            for q32 in range(2):
                t0 = t64 + q32 * 32
                ab = cnt5["ab"] % 2
                cnt5["ab"] += 1
                V(lambda e, ab=ab, t0=t0: e.tensor_tensor(out=ablk[ab][:], in0=iob[:, :].unsqueeze(1).to_broadcast([128, 32, 128]),
                                                         in1=i1T[:, tk0 + t0:tk0 + t0 + 32].unsqueeze(2).to_broadcast([128, 32, 128]), op=ALU.is_equal), r=[R_eiT, R_io], w=[R_ablk[ab]])
                pg = pbk[4 + cnt5["pg"] % 2]
                R_pg = R_pb[4 + cnt5["pg"] % 2]
                cnt5["pg"] += 1
                for t in range(32):
                    T(lambda e, pg=pg, t=t, ab=ab, q32=q32: e.matmul(out=pg[:, t * 16:(t + 1) * 16], lhsT=ablk[ab][:, t, :], rhs=bblk[:, q32 * 32 + t, :], start=True, stop=True),
                      r=[R_ablk[ab], R_bblk], w=[R_pg], inc=(t == 31))
                A(lambda e, pg=pg, t0=t0, gbuf=gbuf: e.copy(out=Gc[gbuf][:, :, t0:t0 + 32].rearrange("p c t -> p t c"), in_=pg[:, :].rearrange("p (t c) -> p t c", c=16)), r=[R_pg], w=[R_Gc[gbuf]])

    def stage1(i, hf, c):
        ubf, gp, cj = i % 2, (i // 4) % 2, i % 4
        kb.dma("pool", ub[ubf][:], pu_v[c], w=[R_ub[ubf]])
        kb.dma("pool", vb[gp][cj][:], pv_v[c], w=[R_vb[gp][cj]])
        for half in range(2):
            for k in range(8):
                kk = half * 8 + k
                T(lambda e, k=k, kk=kk, ubf=ubf, half=half: e.transpose(out=pT5s[half][:, k * 128:(k + 1) * 128], in_=ub[ubf][:, kk * 128:(kk + 1) * 128], identity=ident[:]), r=[R_ub[ubf], R_c], w=[R_pb[7 - half]], inc=(k == 7))
            A(lambda e, half=half, ubf=ubf: e.copy(out=uT[ubf][:, half * 8:half * 8 + 8, :], in_=pT5vs[half]), r=[R_pb[7 - half]], w=[R_uT[ubf]])

    def stage2(i, hf, c):
        ubf, gp, cj = i % 2, (i // 4) % 2, i % 4
        tk0, gbuf, cl = hf * TH, (hf * 8 + c // 16) % 2, c % 16
        pa, R_pa = pbk[i % 2], R_pb[i % 2]
        for k in range(16):
            T(lambda e, pa=pa, k=k, ubf=ubf: e.matmul(out=pa[:, 0:TH], lhsT=uT[ubf][:, k, :], rhs=h2T[:, k, tk0:tk0 + TH], start=(k == 0), stop=(k == 15)),
              r=[R_uT[ubf], R_h2T], w=[R_pa], inc=(k == 15))
        A(lambda e, pa=pa, ubf=ubf: e.activation(out=ga[ubf][:], in_=pa[:, 0:TH], func=AF.Gelu), r=[R_pa], w=[R_ga[ubf]])
        V(lambda e, ubf=ubf, gp=gp, cj=cj, cl=cl, gbuf=gbuf: e.tensor_tensor(out=wg[gp][cj][:], in0=ga[ubf][:], in1=Gc[gbuf][:, cl, :], op=ALU.mult), r=[R_ga[ubf], R_Gc[gbuf]], w=[R_wg[gp][cj]])

    def stage3(i, hf):
        gp = (i // 4) % 2
        for tt in range(NTH):
            for ds in range(4):
                po = pbk[2 + cnt5["po"] % 4]
                R_po = R_pb[2 + cnt5["po"] % 4]
                cnt5["po"] += 1
                for cj in range(4):
                    T(lambda e, po=po, gp=gp, cj=cj, tt=tt, ds=ds: e.matmul(out=po[:], lhsT=wg[gp][cj][:, tt * 128:(tt + 1) * 128], rhs=vb[gp][cj][:, ds * 512:(ds + 1) * 512], start=(cj == 0), stop=(cj == 3)),
                      r=[R_wg[gp][cj], R_vb[gp][cj]], w=[R_po], inc=(cj == 3))
                R_oa = R_oacc_t[tt * 4 + ds]
                if (tt * 4 + ds) % 2 == 0:
                    V(lambda e, po=po, tt=tt, ds=ds: e.tensor_tensor(out=oacc[:, tt, ds * 512:(ds + 1) * 512], in0=oacc[:, tt, ds * 512:(ds + 1) * 512], in1=po[:], op=ALU.add), r=[R_po, R_oa], w=[R_oa])
                else:
                    ei = cnt5["ev"] % 2
                    cnt5["ev"] += 1
                    A(lambda e, po=po, ei=ei: e.copy(out=evb[ei][:], in_=po[:]), r=[R_po], w=[R_evb[ei]])
                    P(lambda e, tt=tt, ds=ds, ei=ei: e.tensor_tensor(out=oacc[:, tt, ds * 512:(ds + 1) * 512], in0=oacc[:, tt, ds * 512:(ds + 1) * 512], in1=evb[ei][:], op=ALU.add), r=[R_evb[ei], R_oa], w=[R_oa])

    items = [(hf, c) for hf in range(Tn // TH) for c in range(128)]
    for tt in range(NTH):
        kb.dma("sp", oacc[:, tt, :], x2d[tt * 128:(tt + 1) * 128, :], r=[R_x2d], w=R_oacc_t[tt * 4:tt * 4 + 4])
    build_G(0, 0)
    stage1(0, *items[0])
    for i, (hf, c) in enumerate(items):
        if i + 1 < len(items):
            stage1(i + 1, *items[i + 1])
        stage2(i, hf, c)
        if c % 16 == 7 and not (hf == Tn // TH - 1 and c // 16 == 7):
            nhf, ncc = (hf, c // 16 + 1) if c // 16 < 7 else (hf + 1, 0)
            build_G(nhf, ncc)
        if i % 4 == 3:
            stage3(i, hf)
        if c == 127:
            tk0 = hf * TH
            for tt in range(NTH):
                kb.dma("sp", out[tk0 + tt * 128:tk0 + (tt + 1) * 128, :], oacc[:, tt, :], r=R_oacc_t[tt * 4:tt * 4 + 4])
            if hf + 1 < Tn // TH:
                for tt in range(NTH):
                    kb.dma("sp", oacc[:, tt, :], x2d[tk0 + TH + tt * 128:tk0 + TH + (tt + 1) * 128, :], r=[R_x2d], w=R_oacc_t[tt * 4:tt * 4 + 4])


def wout_perm():
    idx = []
    for i in range(8):
        for hh in range(4):
            base = (0 if hh < 2 else 2048) + (2 * i + hh % 2) * 128
            idx.extend(range(base, base + 128))
    return np.array(idx)


def prep_B(inp, core, Tn, S):
    f = np.float32
    t0 = core * Tn
    return {
        "x_own": np.ascontiguousarray(inp["x"][0, t0:t0 + Tn]),
        "w_out": np.ascontiguousarray(inp["w_out"][0][wout_perm()]),
        "gffn": np.ascontiguousarray(inp["ffn_norm_g"][0]),
        "gffn_b": np.ascontiguousarray(np.broadcast_to(inp["ffn_norm_g"][0], (128, D))),
        "w_pq": np.ascontiguousarray(inp["w_peer_q"][0]),
        "skeys": np.ascontiguousarray(inp["peer_sub_keys"][0].reshape(16, 128, 128)),
        "pu": np.ascontiguousarray(inp["peer_u"][0]),
        "pv": np.ascontiguousarray(inp["peer_v"][0]),
    }


S_FULL = 8192


def _build_fused(S):
    kb = KB()
    io = {}
    for k, (sh, dt) in F_IN(S).items():
        io[k] = kb.dram(k, sh, dt, kind="ExternalInput").ap()
    for k, (sh, dt) in F_SCR(S).items():
        io[k] = kb.dram(k, sh, dt).ap()
    io["out"] = kb.dram("out", [S // 8, 2048], F32, kind="ExternalOutput").ap()
    shared = build_F(kb, S, io)
    io["mtin"] = io["mt_d"].rearrange("(c h) d t -> c h d t", c=8)
    kb.arena_reset()
    build_B(kb, S // 8, io, R_mtin=shared["R_mtd"], shared=None)
    return kb.build()


def kernel(**inputs):
    inp = {k: np.asarray(v) for k, v in inputs.items()}
    S = S_FULL
    nc = _build_fused(S)
    ins = [prep_F(inp, c, S) for c in range(8)]
    res = run_bass_kernel_spmd(nc, ins, core_ids=list(range(8)))
    out = np.zeros((S, 2048), np.float32)
    for c in range(8):
        rows = np.concatenate([np.arange(t * 128, (t + 1) * 128) for t in own_tiles(c, S)])
        out[rows] = np.asarray(res.results[c]["out"])
    return out.reshape(1, S, 2048)
```
